# Optimizing a Trainium2 kernel written in Bass

```python
import math
import jax, jax.numpy as jnp
from jax import lax
import numpy as np

D_MODEL = 1024
BATCH = 2
SEQ = 8192
DEPTH = 1

HEAD_DIM = 64
FOX_HEADS = 8
NSA_HEADS = 8
NSA_KV_HEADS = 2
CMP_BLOCK = 32
CMP_STRIDE = 16
CMP_HIDDEN = 256
SLC_BLOCK = 64
SLC_TOPK = 16
WINDOW = 512
Q_BLOCK = 128
N_GROUPS = 4
EXPERTS_PER_GROUP = 4
N_EXPERTS = N_GROUPS * EXPERTS_PER_GROUP
TOP_K_IN_GROUP = 2
D_EXPERT = 512
RMS_EPS = 1e-6
NEG_INF = -1e30
FORCE_SCORE = 1e4

FOX_W = FOX_HEADS * HEAD_DIM
NSA_W = NSA_HEADS * HEAD_DIM
NSA_KV_W = NSA_KV_HEADS * HEAD_DIM
IN_COLS = (FOX_W, FOX_W, FOX_W, FOX_HEADS,
           NSA_W, NSA_KV_W, NSA_KV_W, NSA_KV_W, NSA_KV_W, NSA_KV_W, NSA_KV_W, 3 * NSA_HEADS,
           D_MODEL, D_MODEL)
D_IN = sum(IN_COLS)

kernel_name = "hybrid_fox_nsa_hiermoe_block"


def rms_norm(x, g):
    xf = x.astype(jnp.float32)
    y = xf * lax.rsqrt(jnp.mean(xf * xf, axis=-1, keepdims=True) + RMS_EPS)
    return (y * g.astype(jnp.float32)).astype(x.dtype)


def split_cols(z):
    outs, off = [], 0
    for c in IN_COLS:
        outs.append(z[..., off:off + c])
        off += c
    return outs


def to_heads(t, n):
    B, S, _ = t.shape
    return t.reshape(B, S, n, HEAD_DIM).transpose(0, 2, 1, 3)


def from_heads(t):
    B, n, S, dh = t.shape
    return t.transpose(0, 2, 1, 3).reshape(B, S, n * dh)


def alibi_slopes(n):
    return jnp.asarray([2.0 ** (-8.0 * (h + 1) / n) for h in range(n)], jnp.float32)


def fox_attention(q, k, v, log_f):
    B, H, S, dh = q.shape
    cum = jnp.cumsum(log_f, axis=-1)
    scale = dh ** -0.5
    kpos = jnp.arange(S)

    def block(i):
        q0 = i * Q_BLOCK
        qpos = q0 + jnp.arange(Q_BLOCK)
        qb = lax.dynamic_slice_in_dim(q, q0, Q_BLOCK, axis=2)
        cb = lax.dynamic_slice_in_dim(cum, q0, Q_BLOCK, axis=2)
        s = jnp.einsum('bhqd,bhkd->bhqk', qb, k, preferred_element_type=jnp.float32) * scale
        s = s + cb[..., :, None] - cum[..., None, :]
        s = jnp.where(kpos[None, :] <= qpos[:, None], s, NEG_INF)
        p = jax.nn.softmax(s, axis=-1)
        return jnp.einsum('bhqk,bhkd->bhqd', p.astype(v.dtype), v)

    o = lax.map(block, jnp.arange(S // Q_BLOCK))
    return jnp.moveaxis(o, 0, 2).reshape(B, H, S, dh)


def compress_blocks(t, w1, w2, pos):
    B, S, G, dh = t.shape
    n_cmp = (S - CMP_BLOCK) // CMP_STRIDE + 1
    idx = jnp.arange(n_cmp)[:, None] * CMP_STRIDE + jnp.arange(CMP_BLOCK)[None, :]
    blk = t[:, idx] + pos[None, None, :, None, :]
    blk = jnp.moveaxis(blk, 3, 1).reshape(B, G, n_cmp, CMP_BLOCK * dh)
    return jax.nn.gelu(blk @ w1) @ w2


def nsa_attention(q, k_cmp, v_cmp, k_slc, v_slc, k_win, v_win, gates):
    B, H, S, dh = q.shape
    G = k_slc.shape[1]
    R = H // G
    n_cmp = k_cmp.shape[2]
    n_slc = S // SLC_BLOCK
    top_n = min(SLC_TOPK, n_slc)
    scale = dh ** -0.5
    slopes = alibi_slopes(H).reshape(G, R)[None, :, :, None, None]

    cmp_start = jnp.arange(n_cmp) * CMP_STRIDE
    cmp_end = cmp_start + CMP_BLOCK - 1
    slc_start = jnp.arange(n_slc) * SLC_BLOCK
    ov = jnp.minimum(cmp_start[:, None] + CMP_BLOCK, slc_start[None, :] + SLC_BLOCK) - \
        jnp.maximum(cmp_start[:, None], slc_start[None, :])
    overlap = jnp.clip(ov, 0, None).astype(jnp.float32) / CMP_BLOCK
    slc_ids = jnp.arange(n_slc)

    k_sb = k_slc.reshape(B, G, n_slc, SLC_BLOCK, dh)
    v_sb = v_slc.reshape(B, G, n_slc, SLC_BLOCK, dh)
    pad = ((0, 0), (0, 0), (WINDOW, 0), (0, 0))
    k_wp = jnp.pad(k_win, pad)
    v_wp = jnp.pad(v_win, pad)
    gather = jax.vmap(jax.vmap(lambda blocks, idx: blocks[idx]))

    def block(i):
        q0 = i * Q_BLOCK
        qpos = q0 + jnp.arange(Q_BLOCK)
        qb = lax.dynamic_slice_in_dim(q, q0, Q_BLOCK, axis=2).reshape(B, G, R, Q_BLOCK, dh)

        s = jnp.einsum('bgrqd,bgnd->bgrqn', qb, k_cmp, preferred_element_type=jnp.float32) * scale
        dist = (qpos[:, None] - cmp_end[None, :]).astype(jnp.float32)
        valid = dist >= 0
        s = jnp.where(valid, s - slopes * dist, NEG_INF)
        p_cmp = jax.nn.softmax(s, axis=-1) * jnp.any(valid, axis=-1)[:, None].astype(jnp.float32)
        o_cmp = jnp.einsum('bgrqn,bgnd->bgrqd', p_cmp.astype(v_cmp.dtype), v_cmp)

        imp = jnp.einsum('bgrqn,nj->bgqj', p_cmp, overlap)
        qblk = qpos // SLC_BLOCK
        forced = (slc_ids[None, :] == 0) | (slc_ids[None, :] == qblk[:, None]) | \
            (slc_ids[None, :] == qblk[:, None] - 1)
        future = slc_ids[None, :] * SLC_BLOCK > qpos[:, None]
        score = jnp.where(future, -1.0, jnp.where(forced, FORCE_SCORE, imp))
        _, idx = lax.top_k(score, top_n)
        n_tok = top_n * SLC_BLOCK
        ks_g = gather(k_sb, idx).reshape(B, G, Q_BLOCK, n_tok, dh)
        vs_g = gather(v_sb, idx).reshape(B, G, Q_BLOCK, n_tok, dh)
        spos = (idx[..., None] * SLC_BLOCK + jnp.arange(SLC_BLOCK)).reshape(B, G, Q_BLOCK, n_tok)
        dist = (qpos[:, None] - spos).astype(jnp.float32)[:, :, None]
        s = jnp.einsum('bgrqd,bgqkd->bgrqk', qb, ks_g, preferred_element_type=jnp.float32) * scale
        s = jnp.where(dist >= 0, s - slopes * dist, NEG_INF)
        o_slc = jnp.einsum('bgrqk,bgqkd->bgrqd', jax.nn.softmax(s, axis=-1).astype(vs_g.dtype), vs_g)

        kw_b = lax.dynamic_slice_in_dim(k_wp, q0, WINDOW + Q_BLOCK, axis=2)
        vw_b = lax.dynamic_slice_in_dim(v_wp, q0, WINDOW + Q_BLOCK, axis=2)
        wpos = q0 - WINDOW + jnp.arange(WINDOW + Q_BLOCK)
        dist = (qpos[:, None] - wpos[None, :]).astype(jnp.float32)
        valid = (dist >= 0) & (dist < WINDOW) & (wpos[None, :] >= 0)
        s = jnp.einsum('bgrqd,bgkd->bgrqk', qb, kw_b, preferred_element_type=jnp.float32) * scale
        s = jnp.where(valid, s - slopes * dist, NEG_INF)
        o_win = jnp.einsum('bgrqk,bgkd->bgrqd', jax.nn.softmax(s, axis=-1).astype(vw_b.dtype), vw_b)

        gb = lax.dynamic_slice_in_dim(gates, q0, Q_BLOCK, axis=2).reshape(B, G, R, Q_BLOCK, 3)
        o = gb[..., 0:1] * o_cmp + gb[..., 1:2] * o_slc + gb[..., 2:3] * o_win
        return o.reshape(B, H, Q_BLOCK, dh)

    o = lax.map(block, jnp.arange(S // Q_BLOCK))
    return jnp.moveaxis(o, 0, 2).reshape(B, H, S, dh)


def hierarchical_moe(x, w_group, b_group, w_router, b_router, w_gate, w_up, w_down):
    B, S, D = x.shape
    t = x.reshape(B * S, D)
    g_logits = (t @ w_group).astype(jnp.float32) + b_group.astype(jnp.float32)
    p_g, g_top = lax.top_k(jax.nn.softmax(g_logits, axis=-1), 1)
    e_logits = ((t @ w_router).astype(jnp.float32) + b_router.astype(jnp.float32)).reshape(-1, N_GROUPS, EXPERTS_PER_GROUP)
    e_in = jnp.take_along_axis(e_logits, g_top[:, :, None], axis=1)[:, 0]
    top_p, top_i = lax.top_k(jax.nn.softmax(e_in, axis=-1), TOP_K_IN_GROUP)
    w = top_p / jnp.sum(top_p, axis=-1, keepdims=True) * p_g
    expert_id = g_top * EXPERTS_PER_GROUP + top_i
    combine = jnp.sum(jax.nn.one_hot(expert_id, N_EXPERTS, dtype=jnp.float32) * w[..., None], axis=1)
    hid = jax.nn.silu(jnp.einsum('td,edf->etf', t, w_gate)) * jnp.einsum('td,edf->etf', t, w_up)
    hid = hid * combine.T.astype(hid.dtype)[:, :, None]
    y = jnp.einsum('etf,efd->td', hid, w_down)
    return y.reshape(B, S, D)


def setup_inputs(seed: int = 0) -> dict:
    key = jax.random.key(seed)
    ks = jax.random.split(key, 26)
    L = DEPTH

    def nrm(k, shape, scale):
        return jax.random.normal(k, shape, jnp.float32) * scale

    def gain(k, shape):
        return 1.0 + 0.02 * jax.random.normal(k, shape, jnp.float32)

    cmp_in = CMP_BLOCK * HEAD_DIM
    return {
        "x": nrm(ks[0], (BATCH, SEQ, D_MODEL), 1.0),
        "norm_mix_g": gain(ks[1], (L, D_MODEL)),
        "w_in": nrm(ks[2], (L, D_MODEL, D_IN), D_MODEL ** -0.5),
        "b_forget": jnp.linspace(1.0, 6.0, FOX_HEADS, dtype=jnp.float32)[None, :] + nrm(ks[3], (L, FOX_HEADS), 0.1),
        "fox_q_g": gain(ks[4], (L, HEAD_DIM)),
        "fox_k_g": gain(ks[5], (L, HEAD_DIM)),
        "nsa_q_g": gain(ks[6], (L, HEAD_DIM)),
        "nsa_k_g": gain(ks[7], (L, HEAD_DIM)),
        "cmp_k_w1": nrm(ks[8], (L, cmp_in, CMP_HIDDEN), cmp_in ** -0.5),
        "cmp_k_w2": nrm(ks[9], (L, CMP_HIDDEN, HEAD_DIM), CMP_HIDDEN ** -0.5),
        "cmp_k_pos": nrm(ks[10], (L, CMP_BLOCK, HEAD_DIM), 0.1),
        "cmp_v_w1": nrm(ks[11], (L, cmp_in, CMP_HIDDEN), cmp_in ** -0.5),
        "cmp_v_w2": nrm(ks[12], (L, CMP_HIDDEN, HEAD_DIM), CMP_HIDDEN ** -0.5),
        "cmp_v_pos": nrm(ks[13], (L, CMP_BLOCK, HEAD_DIM), 0.1),
        "w_fox_up": nrm(ks[14], (L, FOX_W, D_MODEL), FOX_W ** -0.5),
        "w_nsa_up": nrm(ks[15], (L, NSA_W, D_MODEL), NSA_W ** -0.5),
        "w_out": nrm(ks[16], (L, D_MODEL, D_MODEL), D_MODEL ** -0.5),
        "norm_ffn_g": gain(ks[17], (L, D_MODEL)),
        "w_group": nrm(ks[18], (L, D_MODEL, N_GROUPS), D_MODEL ** -0.5),
        "b_group": nrm(ks[19], (L, N_GROUPS), 0.01),
        "w_router": nrm(ks[20], (L, D_MODEL, N_EXPERTS), D_MODEL ** -0.5),
        "b_router": nrm(ks[21], (L, N_EXPERTS), 0.01),
        "w_gate": nrm(ks[22], (L, N_EXPERTS, D_MODEL, D_EXPERT), D_MODEL ** -0.5),
        "w_up": nrm(ks[23], (L, N_EXPERTS, D_MODEL, D_EXPERT), D_MODEL ** -0.5),
        "w_down": nrm(ks[24], (L, N_EXPERTS, D_EXPERT, D_MODEL), D_EXPERT ** -0.5),
    }


def reference(x, norm_mix_g, w_in, b_forget, fox_q_g, fox_k_g, nsa_q_g, nsa_k_g,
              cmp_k_w1, cmp_k_w2, cmp_k_pos, cmp_v_w1, cmp_v_w2, cmp_v_pos,
              w_fox_up, w_nsa_up, w_out, norm_ffn_g, w_group, b_group, w_router, b_router,
              w_gate, w_up, w_down):
    B, S, D = x.shape
    for l in range(DEPTH):
        h = rms_norm(x, norm_mix_g[l])
        (f_q, f_k, f_v, f_f, n_q, k_c, v_c, k_s, v_s, k_w, v_w, n_g, g_a, g_b) = split_cols(h @ w_in[l])

        qa = rms_norm(to_heads(f_q, FOX_HEADS), fox_q_g[l])
        ka = rms_norm(to_heads(f_k, FOX_HEADS), fox_k_g[l])
        va = to_heads(f_v, FOX_HEADS)
        log_f = jax.nn.log_sigmoid(f_f.astype(jnp.float32) + b_forget[l].astype(jnp.float32)).transpose(0, 2, 1)
        out_a = from_heads(fox_attention(qa, ka, va, log_f)) @ w_fox_up[l]

        qb = rms_norm(to_heads(n_q, NSA_HEADS), nsa_q_g[l])
        kc = compress_blocks(k_c.reshape(B, S, NSA_KV_HEADS, HEAD_DIM), cmp_k_w1[l], cmp_k_w2[l], cmp_k_pos[l])
        kc = rms_norm(kc, nsa_k_g[l])
        vc = compress_blocks(v_c.reshape(B, S, NSA_KV_HEADS, HEAD_DIM), cmp_v_w1[l], cmp_v_w2[l], cmp_v_pos[l])
        ks_ = rms_norm(to_heads(k_s, NSA_KV_HEADS), nsa_k_g[l])
        vs_ = to_heads(v_s, NSA_KV_HEADS)
        kw_ = rms_norm(to_heads(k_w, NSA_KV_HEADS), nsa_k_g[l])
        vw_ = to_heads(v_w, NSA_KV_HEADS)
        gates = jax.nn.sigmoid(n_g.reshape(B, S, NSA_HEADS, 3)).transpose(0, 2, 1, 3)
        out_b = from_heads(nsa_attention(qb, kc, vc, ks_, vs_, kw_, vw_, gates)) @ w_nsa_up[l]

        mix = jax.nn.sigmoid(g_a) * out_a + jax.nn.sigmoid(g_b) * out_b
        x = x + mix @ w_out[l]

        x = x + hierarchical_moe(rms_norm(x, norm_ffn_g[l]), w_group[l], b_group[l], w_router[l], b_router[l],
                                 w_gate[l], w_up[l], w_down[l])
    return x
```

```python
import contextlib
import numpy as np
import concourse.bass as bass
import concourse.mybir as mybir
from concourse.bass_utils import run_bass_kernel_spmd

F32 = mybir.dt.float32
BF16 = mybir.dt.bfloat16
AF = mybir.ActivationFunctionType
ALU = mybir.AluOpType
AX = mybir.AxisListType

S_LEN = 8192
D = 1024
NT = 64
NEGM = -30000.0
DIN = 4896


class _Op:
    __slots__ = ("id", "eng", "fn", "deps", "dma", "needs_inc", "sem", "val")

    def __init__(self, id, eng, fn, dma):
        self.id = id
        self.eng = eng
        self.fn = fn
        self.deps = []
        self.dma = dma
        self.needs_inc = False
        self.sem = None
        self.val = 0


class Sched:
    ENGS = ("pe", "act", "dve", "pool", "sp")
    NDMA = {"sp": 12, "pool": 8, "act": 2, "pe": 1, "dve": 1}

    def __init__(self, nc):
        self.nc = nc
        self.ops = {e: [] for e in self.ENGS}
        self.last_w = {}
        self.readers = {}
        self.n = 0
        self.dma_hist = {e: [] for e in self.ENGS}

    def op(self, eng, fn, r=(), w=(), dma=False):
        o = _Op(self.n, eng, fn, dma)
        self.n += 1
        deps = {}
        for res in r:
            lw = self.last_w.get(res)
            if lw is not None:
                deps[lw.id] = lw
        for res in w:
            lw = self.last_w.get(res)
            if lw is not None:
                deps[lw.id] = lw
            for rd in self.readers.get(res, ()):
                deps[rd.id] = rd
        if dma:
            hist = self.dma_hist[eng]
            n = self.NDMA[eng]
            if len(hist) >= n:
                p = hist[len(hist) - n]
                deps[p.id] = p
            hist.append(o)
        for d in deps.values():
            if d is o:
                continue
            if d.eng == eng and eng == "pe" and not d.dma and not dma:
                continue
            if not d.dma:
                d.needs_inc = True
            o.deps.append(d)
        for res in r:
            self.readers.setdefault(res, []).append(o)
        for res in w:
            self.last_w[res] = o
            self.readers[res] = []
        self.ops[eng].append(o)
        return o

    def barrier(self):
        lasts = []
        for e in self.ENGS:
            comp = [o for o in self.ops[e] if not o.dma and o.fn is not None]
            if comp:
                lasts.append(comp[-1])
            lasts.extend(self.dma_hist[e][-self.NDMA[e]:])
        for e in self.ENGS:
            o = _Op(self.n, e, None, False)
            self.n += 1
            for d in lasts:
                if d.eng == e and e == "pe" and not d.dma:
                    continue
                if not d.dma:
                    d.needs_inc = True
                o.deps.append(d)
            self.ops[e].append(o)
        self.last_w = {}
        self.readers = {}

    def emit(self):
        nc = self.nc
        with contextlib.ExitStack() as st:
            esem = {e: st.enter_context(nc.semaphore(f"s_{e}")) for e in self.ENGS}
            dsem = {e: [st.enter_context(nc.semaphore(f"d_{e}{i}")) for i in range(self.NDMA[e])]
                    for e in self.ENGS}
            for e in self.ENGS:
                cnt = 0
                dcnt = [0] * self.NDMA[e]
                k = 0
                for o in self.ops[e]:
                    if o.dma:
                        i = k % self.NDMA[e]
                        k += 1
                        dcnt[i] += 16
                        o.sem = dsem[e][i]
                        o.val = dcnt[i]
                    elif o.needs_inc:
                        cnt += 1
                        o.sem = esem[e]
                        o.val = cnt
            block = st.enter_context(nc.Block())

            def run(engobj, ops):
                waited = {}
                for o in ops:
                    for d in sorted(o.deps, key=lambda d: d.id):
                        key = id(d.sem)
                        if waited.get(key, 0) < d.val:
                            engobj.wait_ge(d.sem, d.val)
                            waited[key] = d.val
                    if o.fn is None:
                        continue
                    ins = o.fn(engobj)
                    if o.dma:
                        ins.then_inc(o.sem, 16)
                    elif o.needs_inc:
                        ins.then_inc(o.sem, 1)

            if self.ops["pe"]:
                @block.tensor
                def _(e):
                    run(e, self.ops["pe"])
            if self.ops["act"]:
                @block.scalar
                def _(e):
                    run(e, self.ops["act"])
            if self.ops["dve"]:
                @block.vector
                def _(e):
                    run(e, self.ops["dve"])
            if self.ops["pool"]:
                @block.gpsimd
                def _(e):
                    run(e, self.ops["pool"])
            if self.ops["sp"]:
                @block.sync
                def _(e):
                    run(e, self.ops["sp"])


class Arena:
    def __init__(self, nc, base=18560, limit=229376):
        self.nc = nc
        self.off = base
        self.limit = limit
        self.k = 0

    def alloc(self, name, shape, dt):
        nb = int(np.prod(shape[1:])) * (4 if dt == F32 else 2)
        nb = (nb + 63) // 64 * 64
        assert self.off + nb <= self.limit, f"SBUF overflow at {name}: {self.off}+{nb}"
        self.k += 1
        t = self.nc.alloc_sbuf_tensor_at(f"{name}_{self.k}", list(shape), dt, offset=self.off)
        self.off += nb
        return t.ap()

    def mark(self):
        return self.off

    def release(self, m):
        self.off = m


def host_consts(j):
    c = {}
    f = np.float32
    c["c_ident"] = np.eye(128, dtype=f)
    blk = np.zeros((128, 128), f)
    blk[:64, :64] = 1.0
    blk[64:, 64:] = 1.0
    c["c_blk"] = blk
    p = np.arange(128)
    c["c_utri"] = (p[:, None] <= p[None, :]).astype(f)
    c["c_ones"] = np.ones((128, 128), f)
    q0 = np.array([512 * (4 * s + j) for s in range(4)])
    t64 = np.arange(64)
    oh = np.zeros((128, 4, 64), f)
    badd = np.zeros((128, 4, 64), f)
    for s in range(4):
        oh[:, s, 4 * (4 * s + j)] = 1.0
        badd[:, s, 16 * s + 4 * (j + 1):16 * s + 16] = NEGM
    c["c_oh"] = oh
    c["c_badd"] = badd
    am = np.zeros((128, 4, 128), f)
    am[:, j, :] = np.eye(128, dtype=f)
    c["c_am"] = am
    ql = np.arange(512)
    dg = np.zeros((128, 4, 512), f)
    for tl in range(4):
        dg[:, tl, :] = np.where(128 * tl + p[:, None] <= ql[None, :], 0.0, NEGM)
    c["c_dg"] = dg
    wm = np.zeros((128, 8, 512), f)
    for t in range(8):
        dist = ql[None, :] - (128 * t + p[:, None]) + 512
        wm[:, t, :] = np.where((dist >= 0) & (dist < 512), 0.0, NEGM)
    c["c_wm"] = wm
    cm = np.zeros((128, 4, 4, 512), f)
    for s in range(4):
        for cc in range(4):
            n = 128 * cc + p[:, None]
            q = q0[s] + ql[None, :]
            cm[:, s, cc, :] = np.where((16 * n + 31 <= q) & (n < 511), 0.0, NEGM)
    c["c_cm"] = cm
    slopes = np.array([2.0 ** (-(h + 1)) for h in range(8)], np.float64)
    kbs = np.zeros((128, 8, 4, 64), f)
    kbw = np.zeros((128, 8, 4, 8), f)
    kbc = np.zeros((128, 8, 4, 4), f)
    for h in range(8):
        for s in range(4):
            kpos = 128 * t64[None, :] + p[:, None]
            kbs[:, h, s, :] = slopes[h] * (kpos - q0[s]) + badd[:, s, :]
            wpos = q0[s] - 512 + 128 * np.arange(8)[None, :] + p[:, None]
            kbw[:, h, s, :] = np.where(wpos >= 0, slopes[h] * (wpos - q0[s]), NEGM)
            cend = 16 * (128 * np.arange(4)[None, :] + p[:, None]) + 31
            kbc[:, h, s, :] = slopes[h] * (cend - q0[s])
    c["c_kbs"] = kbs.reshape(128, -1)
    c["c_kbw"] = kbw.reshape(128, -1)
    c["c_kbc"] = kbc.reshape(128, -1)
    asel = np.zeros((128, 4, 4, 128), f)
    bsel = np.zeros((128, 4, 4, 128), f)
    jb = np.arange(128)
    for s in range(4):
        for sub in range(4):
            q = q0[s] + 128 * sub + p[:, None]
            qb = q // 64
            future = 64 * jb[None, :] > q
            forced = (jb[None, :] == 0) | (jb[None, :] == qb) | (jb[None, :] == qb - 1)
            asel[:, s, sub, :] = np.where(future | forced, 0.0, 1.0)
            bsel[:, s, sub, :] = np.where(future, -1.0, np.where(forced, 1e4, 0.0))
    c["c_asel"] = asel.reshape(128, -1)
    c["c_bsel"] = bsel.reshape(128, -1)
    ov = np.zeros((128, 4, 128), f)
    for cc in range(4):
        for pp in range(128):
            n = 128 * cc + pp
            if n >= 511:
                continue
            a0, a1 = 16 * n, 16 * n + 32
            for b_ in range(a0 // 64, (a1 - 1) // 64 + 1):
                ovl = min(a1, 64 * b_ + 64) - max(a0, 64 * b_)
                ov[pp, cc, b_] = ovl / 32.0
    c["c_ov"] = ov.reshape(128, -1)
    e_all = np.zeros((128, S_LEN), f)
    for b_ in range(128):
        e_all[b_, 64 * b_:64 * b_ + 64] = 1.0
    c["c_eall"] = e_all
    c["c_onerow"] = np.ones((1, S_LEN), f)
    qaug = np.zeros((8, 2, 2048), f)
    qq = np.arange(2048) % 512
    for h in range(8):
        qaug[h, 0] = -slopes[h] * (qq % 256)
        qaug[h, 1] = -slopes[h] * 256 * (qq // 256)
    c["c_qaug"] = qaug
    selg = np.zeros((64, 24, 128), f)
    for hb in range(24):
        selg[hb, hb, :] = 1.0
        selg[32 + hb, hb, :] = 1.0
    c["c_selg"] = selg.reshape(64, -1)
    return c


CONST_SHAPES = {
    "c_ident": [128, 128], "c_blk": [128, 128], "c_utri": [128, 128], "c_ones": [128, 128],
    "c_oh": [128, 4, 64], "c_badd": [128, 4, 64], "c_am": [128, 4, 128], "c_dg": [128, 4, 512],
    "c_wm": [128, 8, 512], "c_cm": [128, 4, 4, 512], "c_kbs": [128, 2048], "c_kbw": [128, 256],
    "c_kbc": [128, 128], "c_asel": [128, 2048], "c_bsel": [128, 2048], "c_ov": [128, 512],
    "c_eall": [128, S_LEN], "c_onerow": [1, S_LEN], "c_qaug": [8, 2, 2048], "c_selg": [64, 24 * 128],
}


def build(debug=None, stop_after=99):
    nc = bass.Bass("TRN2", target_bir_lowering=False)
    S = Sched(nc)
    A = Arena(nc)
    debug = debug or set()

    def din(name, shape):
        return nc.dram_tensor(name, list(shape), F32, kind="ExternalInput").ap()

    def scratch(name, shape, dt):
        kind = "ExternalOutput" if name in debug else "Internal"
        return nc.dram_tensor(name, list(shape), dt, kind=kind).ap()

    xb = din("xb", [S_LEN, D])
    xq = din("xq", [2048, D])
    xw = din("xw", [4096, D])
    w_in = din("w_in", [D, DIN])
    C = {k: din(k, v) for k, v in CONST_SHAPES.items()}
    gmix_d = din("gmix", [128, 8])
    gffn_d = din("gffn", [128, 8])
    gk2_d = din("gk2", [128, 4])
    bfb_d = din("bfb", [128, 8])
    nbfc_d = din("nbfc", [128, 1])
    w1k_d = din("cmp_k_w1", [2048, 256])
    w1v_d = din("cmp_v_w1", [2048, 256])
    w2k_d = din("cmp_k_w2", [256, 64])
    w2v_d = din("cmp_v_w2", [256, 64])
    posk_d = din("poskT", [128, 32])
    posv_d = din("posvT", [128, 32])
    wfu_d = din("w_fox_up", [512, D])
    wnu_d = din("w_nsa_up", [512, D])
    wout_d = din("w_out", [D, D])
    wr_d = din("w_rt", [D, 20])
    br_d = din("b_rt", [128, 20])
    wg_d = din("w_gate", [16, D, 512])
    wu_d = din("w_up", [16, D, 512])
    wd_d = din("w_down", [16, 512, D])
    out_d = nc.dram_tensor("out", [2048, D], F32, kind="ExternalOutput").ap()

    KT_fox = scratch("KT_fox", [8, 64, S_LEN], BF16)
    V_fox = scratch("V_fox", [8, 128, NT, 128], BF16)
    KT_s = scratch("KT_s", [2, 64, S_LEN], BF16)
    KT_w = scratch("KT_w", [2, 64, 4096], BF16)
    V_s = scratch("V_s", [2, 128, NT, 128], BF16)
    V_w = scratch("V_w", [2, 128, 32, 128], BF16)
    OA_scr = scratch("OA_scr", [4, 128, 2048], BF16)
    OB_scr = scratch("OB_scr", [4, 128, 2048], BF16)
    dbgA = scratch("dbgA", [128, 4096], F32)
    dbgB = scratch("dbgB", [128, 4096], F32)
    dbgB_big = scratch("dbgB_big", [128, 8192], F32)

    ps = [nc.alloc_psum_tensor(f"ps{i}", [128, 512], F32).ap() for i in range(8)]
    ps7b = ps[7].bitcast(BF16)

    def dma(eng, out, in_, r=(), w=()):
        return S.op(eng, lambda e: e.dma_start(out=out, in_=in_), r=r, w=w, dma=True)

    def mm(out, lhsT, rhs, start, stop, r=(), w=()):
        return S.op("pe", lambda e: e.matmul(out, lhsT=lhsT, rhs=rhs, start=start, stop=stop,
                                             skip_group_check=True), r=r, w=w)

    def tr(out, in_, ident, r=(), w=()):
        return S.op("pe", lambda e: e.transpose(out=out, in_=in_, identity=ident), r=r, w=w)

    def act(out, in_, func, r=(), w=(), bias=None, scale=1.0, accum_out=None):
        def f(e):
            kw = {}
            if bias is not None:
                kw["bias"] = bias
            if accum_out is not None:
                kw["accum_out"] = accum_out
            return e.activation(out=out, in_=in_, func=func, scale=scale, **kw)
        return S.op("act", f, r=r, w=w)

    def ts(eng, out, in0, s1, s2, op0, op1=None, r=(), w=()):
        def f(e):
            if op1 is None:
                return e.tensor_scalar(out=out, in0=in0, scalar1=s1, scalar2=None, op0=op0)
            return e.tensor_scalar(out=out, in0=in0, scalar1=s1, scalar2=s2, op0=op0, op1=op1)
        return S.op(eng, f, r=r, w=w)

    def tt(eng, out, in0, in1, op, r=(), w=()):
        return S.op(eng, lambda e: e.tensor_tensor(out=out, in0=in0, in1=in1, op=op), r=r, w=w)

    def stt(out, in0, scalar, in1, op0, op1, r=(), w=()):
        return S.op("dve", lambda e: e.scalar_tensor_tensor(out=out, in0=in0, scalar=scalar, in1=in1,
                                                            op0=op0, op1=op1), r=r, w=w)

    def cp(eng, out, in_, r=(), w=()):
        if eng == "act":
            return act(out, in_, AF.Copy, r=r, w=w)
        return S.op(eng, lambda e: e.tensor_copy(out=out, in_=in_), r=r, w=w)

    def recip(out, in_, r=(), w=()):
        return S.op("dve", lambda e: e.reciprocal(out=out, in_=in_), r=r, w=w)

    def memset(eng, ap, val, w=()):
        return S.op(eng, lambda e: e.memset(ap, val), w=w)

    def finish():
        S.barrier()
        S.emit()
        return nc

    ident_f = A.alloc("ident_f", [128, 128], F32)
    ident_b = A.alloc("ident_b", [128, 128], BF16)
    blk = A.alloc("blk", [128, 128], BF16)
    epsb = A.alloc("epsb", [128, 1], F32)
    lnq = A.alloc("lnq", [128, 1], F32)
    gmix = A.alloc("gmix", [128, 8], F32)
    gk2 = A.alloc("gk2", [128, 4], F32)
    dma("sp", ident_f[:], C["c_ident"][:], w=["ident_f"])
    dma("pool", ident_b[:], C["c_ident"][:], w=["ident_b"])
    dma("pool", blk[:], C["c_blk"][:], w=["blk"])
    dma("sp", gmix[:], gmix_d[:], w=["gmix"])
    dma("sp", gk2[:], gk2_d[:], w=["gk2"])
    memset("pool", epsb[:], 1e-6, w=["epsb"])
    memset("pool", lnq[:], float(np.log(0.125)), w=["lnq"])

    m_base = A.mark()
    LF = A.alloc("LF", [128, NT * 8], F32)
    KC = [A.alloc(f"KC{g}", [128, 512], BF16) for g in range(2)]
    VC = [A.alloc(f"VC{g}", [128, 4, 128], BF16) for g in range(2)]
    BKB = A.alloc("BKB", [128, 2048], F32)

    kraw = [A.alloc(f"kraw{i}", [128, 512], F32) for i in range(2)]
    ksq = [A.alloc(f"ksq{i}", [128, 512], BF16) for i in range(2)]
    klv = [A.alloc(f"klv{i}", [128, 512], F32) for i in range(2)]
    nrm_ctr = [0]

    def qknorm(pb, pr, P, N, gcol, out_ap, out_res, qscale=False, psum_idx=None, split=None):
        i2 = nrm_ctr[0] % 2
        nrm_ctr[0] += 1
        kr, kq, kl = kraw[i2], ksq[i2], klv[i2]
        cp("act", kr[:P, :N], pb, r=[pr], w=[f"kraw{i2}"])
        tt("dve", kq[:P, :N], kr[:P, :N], kr[:P, :N], ALU.mult, r=[f"kraw{i2}"], w=[f"ksq{i2}"])
        bi = psum_idx if psum_idx is not None else 4 + i2
        pb2 = ps[bi]
        mm(pb2[:P, :N], blk[:P, :P], kq[:P, :N], True, True, r=["blk", f"ksq{i2}"], w=[f"ps{bi}"])
        act(kl[:P, :N], pb2[:P, :N], AF.Ln, r=[f"ps{bi}", "epsb"], w=[f"klv{i2}"], bias=epsb[:P, :], scale=1.0 / 64)
        if qscale:
            act(kl[:P, :N], kl[:P, :N], AF.Exp, r=[f"klv{i2}", "lnq"], w=[f"klv{i2}"], scale=-0.5, bias=lnq[:P, :])
        else:
            act(kl[:P, :N], kl[:P, :N], AF.Exp, r=[f"klv{i2}"], w=[f"klv{i2}"], scale=-0.5)
        if split is not None:
            (oa, ra, ob, rb) = split
            stt(oa, kr[0:64, :N], gcol[0:64, :], kl[0:64, :N], ALU.mult, ALU.mult,
                r=[f"kraw{i2}", f"klv{i2}", "gk2"], w=[ra])
            stt(ob, kr[64:128, :N], gcol[64:128, :], kl[64:128, :N], ALU.mult, ALU.mult,
                r=[f"kraw{i2}", f"klv{i2}", "gk2"], w=[rb])
            return
        stt(out_ap, kr[:P, :N], gcol, kl[:P, :N], ALU.mult, ALU.mult,
            r=[f"kraw{i2}", f"klv{i2}", "gk2"], w=[out_res])

    def norm_transpose(src_rows, xt, rx, hT_dst, rh, ss, rs, rss, gvec, gres, evac_ctr):
        dma("sp", xt[:], src_rows.rearrange("(s p) d -> p s d", p=128), w=[rx])
        for sub in range(4):
            act(junk[:], xt[:, sub, :], AF.Square, r=[rx], w=["junk", rss], accum_out=ss[:, sub:sub + 1])
        ts("dve", rs[:], ss[:], 1.0 / D, 1e-6, ALU.mult, ALU.add, r=[rss], w=[rss + "r"])
        act(rs[:], rs[:], AF.Sqrt, r=[rss + "r"], w=[rss + "r"])
        recip(rs[:], rs[:], r=[rss + "r"], w=[rss + "r"])
        for sub in range(4):
            ts("dve", xt[:, sub, :], xt[:, sub, :], rs[:, sub:sub + 1], None, ALU.mult, r=[rx, rss + "r"], w=[rx])
        for kc in range(8):
            pb = ps[kc % 2]
            pr = f"ps{kc % 2}"
            for sub in range(4):
                tr(pb[:, sub * 128:(sub + 1) * 128], xt[:, sub, kc * 128:(kc + 1) * 128], ident_f[:],
                   r=[rx, "ident_f"], w=[pr])
            if kc % 2 == 0:
                S.op("act", lambda e, o=hT_dst[:, kc, :], i=pb[:], s=gvec[:, kc:kc + 1]:
                     e.activation(out=o, in_=i, func=AF.Copy, scale=s), r=[pr, gres], w=[rh])
            else:
                ts("dve", hT_dst[:, kc, :], pb[:], gvec[:, kc:kc + 1], None, ALU.mult, r=[pr, gres], w=[rh])

    m1 = A.mark()
    TCk = A.alloc("TCk", [128, S_LEN], BF16)
    TCv = A.alloc("TCv", [128, S_LEN], BF16)
    m1b = A.mark()
    WK = A.alloc("WK", [128, 8, 1800], BF16)
    w_in_v = w_in.rearrange("(kc p) n -> p kc n", p=128)
    wk_cols = [(512, 1024, 0), (2312, 2440, 512), (2568, 2696, 640), (2056, 2184, 768), (2184, 2312, 896),
               (1024, 1536, 1024), (2440, 2568, 1536), (2696, 2824, 1664), (1536, 1544, 1792)]
    for (c0, c1, d0) in wk_cols:
        dma("pool", WK[:, :, d0:d0 + (c1 - c0)], w_in_v[:, :, c0:c1], w=["WK"])
    bfb = A.alloc("bfb", [128, 8], F32)
    dma("sp", bfb[:], bfb_d[:], w=["bfb"])
    xbuf = [A.alloc(f"xbuf{i}", [128, 4, D], F32) for i in range(2)]
    hbuf = [A.alloc(f"hbuf{i}", [128, 8, 512], BF16) for i in range(2)]
    junk = A.alloc("junk", [128, D], BF16)
    ssb = [A.alloc(f"ss{i}", [128, 4], F32) for i in range(2)]
    rsb = [A.alloc(f"rs{i}", [128, 4], F32) for i in range(2)]
    kn = [A.alloc(f"kn{i}", [128, 512], BF16) for i in range(2)]
    VA = [A.alloc(f"VA{i}", [128, 8, 128], BF16) for i in range(2)]
    VAn = [A.alloc(f"VAn{i}", [128, 4, 128], BF16) for i in range(2)]
    zt = A.alloc("zt", [128, 32], F32)
    for i in range(2):
        memset("pool", VA[i][:, :, 64:128], 1.0, w=[f"VA{i}"])
        memset("pool", VAn[i][:, :, 64:128], 1.0, w=[f"VAn{i}"])

    KTf_rows = KT_fox.rearrange("h p t -> (h p) t")
    KTs_rows = KT_s.rearrange("g p t -> (g p) t")
    KTw_rows = KT_w.rearrange("g p t -> (g p) t")
    Vf_v = V_fox.rearrange("h p t c -> p h t c")
    Vs_v = V_s.rearrange("g p t c -> p g t c")
    Vw_v = V_w.rearrange("g p t c -> p g t c")

    npair = [0]
    gctr = [0]

    def kv_group(src_rows, G, window):
        gi = gctr[0] % 2
        gctr[0] += 1
        xt, hT, ss, rs = xbuf[gi], hbuf[gi], ssb[gi], rsb[gi]
        rx, rh = f"xbuf{gi}", f"hbuf{gi}"
        norm_transpose(src_rows, xt, rx, hT, rh, ss, rs, f"ss{gi}", gmix, "gmix", None)
        pairs = [5] if window else [0, 1, 2, 3, 4, 6, 7]
        for pi in pairs:
            c0 = pi * 128
            bi = 2 + npair[0] % 2
            pb, pr = ps[bi], f"ps{bi}"
            npair[0] += 1
            for kc in range(8):
                mm(pb[:], WK[:, kc, c0:c0 + 128], hT[:, kc, :], kc == 0, kc == 7, r=["WK", rh], w=[pr])
            if pi >= 6:
                dst = TCk if pi == 6 else TCv
                cp("act", dst[:, G * 512:(G + 1) * 512], pb[:], r=[pr], w=["TC"])
                continue
            kno = kn[npair[0] % 2]
            kres = f"kn{npair[0] % 2}"
            gcol = gk2[:, 0:1] if pi < 4 else gk2[:, 1:2]
            qknorm(pb[:], pr, 128, 512, gcol, kno[:], kres)
            if pi < 4:
                dst = KTf_rows[pi * 128:(pi + 1) * 128, G * 512:(G + 1) * 512]
            elif pi == 4:
                dst = KTs_rows[:, G * 512:(G + 1) * 512]
            else:
                dst = KTw_rows[:, G * 512:(G + 1) * 512]
            dma("sp", dst, kno[:], r=[kres], w=["KT_scr"])
        for sub in range(4):
            tile = G * 4 + sub
            bi = 6 + sub % 2
            pb, pr = ps[bi], f"ps{bi}"
            if not window:
                for kc in range(8):
                    mm(pb[:], hT[:, kc, sub * 128:(sub + 1) * 128], WK[:, kc, 1024:1536], kc == 0, kc == 7,
                       r=["WK", rh], w=[pr])
                va = VA[tile % 2]
                cp("dve" if sub % 2 == 0 else "act", va[:, :, 0:64], pb[:].rearrange("p (h c) -> p h c", c=64),
                   r=[pr], w=[f"VA{tile % 2}"])
                dma("sp", Vf_v[:, :, tile, :], va[:], r=[f"VA{tile % 2}"], w=["V_scr"])
            c0, c1 = (1664, 1792) if window else (1536, 1664)
            for kc in range(8):
                mm(pb[:, 0:128], hT[:, kc, sub * 128:(sub + 1) * 128], WK[:, kc, c0:c1], kc == 0, kc == 7,
                   r=["WK", rh], w=[pr])
            va = VAn[tile % 2]
            cp("act" if sub % 2 == 0 else "dve", va[:, 0:2, 0:64], pb[:, 0:128].rearrange("p (h c) -> p h c", c=64),
               r=[pr], w=[f"VAn{tile % 2}"])
            dma("sp", (Vw_v if window else Vs_v)[:, :, tile, :], va[:, 0:2, :], r=[f"VAn{tile % 2}"], w=["V_scr"])
        if window:
            return
        pb = ps[2]
        for sub in range(4):
            for kc in range(8):
                mm(pb[:, sub * 8:(sub + 1) * 8], hT[:, kc, sub * 128:(sub + 1) * 128], WK[:, kc, 1792:1800],
                   kc == 0, kc == 7, r=["WK", rh], w=["ps2"])
        tt("dve", zt[:].rearrange("p (s h) -> p s h", h=8), pb[:, 0:32].rearrange("p (s h) -> p s h", h=8),
           bfb[:].unsqueeze(1).to_broadcast([128, 4, 8]), ALU.add, r=["ps2", "bfb"], w=["zt"])
        act(zt[:], zt[:], AF.Exp, r=["zt"], w=["zt"], scale=-1.0)
        ts("dve", zt[:], zt[:], 1.0, None, ALU.add, r=["zt"], w=["zt"])
        act(zt[:], zt[:], AF.Ln, r=["zt"], w=["zt"])
        ts("dve", LF[:, G * 32:(G + 1) * 32], zt[:], -1.0, None, ALU.mult, r=["zt"], w=["LF"])

    for G in range(16):
        kv_group(xb[G * 512:(G + 1) * 512, :], G, False)
    for G in range(8):
        kv_group(xw[G * 512:(G + 1) * 512, :], G, True)
    S.barrier()
    A.release(m1b)
    if stop_after <= 1:
        return finish()

    utri = A.alloc("utri", [128, 128], F32)
    onesm = A.alloc("onesm", [128, 128], F32)
    oh = A.alloc("oh", [128, 4, 64], F32)
    badd = A.alloc("badd", [128, 4, 64], F32)
    dma("sp", utri[:], C["c_utri"][:], w=["utri"])
    dma("sp", onesm[:], C["c_ones"][:], w=["onesm"])
    dma("sp", oh[:], C["c_oh"][:], w=["oh"])
    dma("sp", badd[:], C["c_badd"][:], w=["badd"])
    CKW = A.alloc("CKW", [128, 512], F32)
    TOT = A.alloc("TOT", [128, 512], F32)
    INCL = A.alloc("INCL", [128, 512], F32)
    RS = A.alloc("RS", [128, 32], F32)
    prod = A.alloc("prod", [128, 512], F32)
    mm(ps[0][:], utri[:], LF[:], True, True, r=["utri", "LF"], w=["ps0"])
    mm(ps[1][:], onesm[:], LF[:], True, True, r=["onesm", "LF"], w=["ps1"])
    cp("act", CKW[:], ps[0][:], r=["ps0"], w=["CKW"])
    cp("dve", TOT[:], ps[1][:], r=["ps1"], w=["TOT"])
    TOT3 = TOT[:].rearrange("p (t h) -> p t h", h=8)
    INCL3 = INCL[:].rearrange("p (t h) -> p t h", h=8)
    CKW3 = CKW[:].rearrange("p (t h) -> p t h", h=8)
    for h in range(8):
        S.op("dve", lambda e, h=h: e.tensor_tensor_scan(out=INCL3[:, :, h], data0=onesm[:, 0:64], data1=TOT3[:, :, h],
                                                        initial=0.0, op0=ALU.mult, op1=ALU.add),
             r=["TOT", "onesm"], w=["INCL"])
    tt("dve", INCL[:], INCL[:], TOT[:], ALU.subtract, r=["INCL", "TOT"], w=["INCL"])
    tt("dve", CKW[:], CKW[:], INCL[:], ALU.add, r=["CKW", "INCL"], w=["CKW"])
    for s in range(4):
        tt("dve", prod[:].rearrange("p (t h) -> p t h", h=8), INCL3, oh[:, s, :].unsqueeze(2).to_broadcast([128, 64, 8]),
           ALU.mult, r=["INCL", "oh"], w=["prod"])
        S.op("dve", lambda e, s=s: e.tensor_reduce(out=RS[:, s * 8:(s + 1) * 8],
                                                   in_=prod[:].rearrange("p (t h) -> p h t", h=8),
                                                   axis=AX.X, op=ALU.add), r=["prod"], w=["RS"])
        for h in range(8):
            o = BKB[:, (s * 8 + h) * 64:(s * 8 + h + 1) * 64]
            stt(o, CKW3[:, :, h], -1.0, badd[:, s, :], ALU.mult, ALU.add, r=["CKW", "badd"], w=["BKB"])
            ts("dve", o, o, RS[:, s * 8 + h:s * 8 + h + 1], None, ALU.add, r=["BKB", "RS"], w=["BKB"])
    if "dbgA" in debug and stop_after == 2:
        dma("sp", dbgA[:, 0:2048], BKB[:], r=["BKB"], w=["dbgA"])
        dma("sp", dbgA[:, 2048:2560], CKW[:], r=["CKW"], w=["dbgA"])

    W1 = A.alloc("W1", [128, 32, 256], BF16)
    W2 = A.alloc("W2", [128, 2, 64], BF16)
    posT = A.alloc("posT", [128, 32], BF16)
    posb = A.alloc("posb", [128, 2], F32)
    Ucm = A.alloc("Ucm", [128, 512], F32)
    Tcm = A.alloc("Tcm", [128, 512], F32)
    HD = [A.alloc(f"HD{i}", [128, 512], BF16) for i in range(2)]
    for g in range(2):
        memset("pool", KC[g][:], 0.0, w=[f"KC{g}"])
        dma("pool", KC[g][64:65, :], C["c_onerow"][:, 0:512], w=[f"KC{g}"])
        dma("pool", KC[g][96:97, :], C["c_onerow"][:, 0:512], w=[f"KC{g}"])
        memset("pool", VC[g][:], 0.0, w=[f"VC{g}"])
        memset("pool", VC[g][:, :, 64:128], 1.0, w=[f"VC{g}"])
    for i in range(2):
        memset("pool", HD[i][:], 0.0, w=[f"HD{i}"])
    GC = 1.5957691216057308
    for kv in range(2):
        w1d = w1k_d if kv == 0 else w1v_d
        w2d = w2k_d if kv == 0 else w2v_d
        TC = TCk if kv == 0 else TCv
        TC3 = TC[:].rearrange("p (n s) -> p n s", s=16)
        w1v = w1d.rearrange("(j d) n -> d j n", d=64)
        for half in range(2):
            for jq in range(4):
                dma("pool", W1[64 * half:64 * half + 64, jq * 8:(jq + 1) * 8, :], w1v[:, jq * 8:(jq + 1) * 8, :], w=["W1"])
        dma("pool", W2[:], w2d.rearrange("(hc p) n -> p hc n", p=128), w=["W2"])
        dma("pool", posT[:], (posk_d if kv == 0 else posv_d)[:], w=["posT"])
        for hc in range(2):
            for jj in range(32):
                mm(ps[0][:, hc:hc + 1], W1[0:64, jj, hc * 128:(hc + 1) * 128], posT[0:64, jj:jj + 1], jj == 0, jj == 31,
                   r=["W1", "posT"], w=["ps0"])
        cp("dve", posb[:], ps[0][:, 0:2], r=["ps0"], w=["posb"])
        for g in range(2):
            b0 = 64 * g
            for hc in range(2):
                pb, pr = ps[2 + hc], f"ps{2 + hc}"
                for jj in range(32):
                    rhs = TC3[b0:b0 + 64, 0:511, jj] if jj < 16 else TC3[b0:b0 + 64, 1:512, jj - 16]
                    mm(pb[:, 0:511], W1[b0:b0 + 64, jj, hc * 128:(hc + 1) * 128], rhs, jj == 0, jj == 31,
                       r=["W1", "TC"], w=[pr])
                act(Ucm[:, 0:511], pb[:, 0:511], AF.Identity, r=[pr, "posb"], w=["Ucm"], bias=posb[:, hc:hc + 1])
                tt("dve", Tcm[:, 0:511], Ucm[:, 0:511], Ucm[:, 0:511], ALU.mult, r=["Ucm"], w=["Tcm"])
                ts("dve", Tcm[:, 0:511], Tcm[:, 0:511], 0.044715, 1.0, ALU.mult, ALU.add, r=["Tcm"], w=["Tcm"])
                tt("dve", Tcm[:, 0:511], Tcm[:, 0:511], Ucm[:, 0:511], ALU.mult, r=["Tcm", "Ucm"], w=["Tcm"])
                act(Tcm[:, 0:511], Tcm[:, 0:511], AF.Exp, r=["Tcm"], w=["Tcm"], scale=-GC)
                ts("dve", Tcm[:, 0:511], Tcm[:, 0:511], 1.0, None, ALU.add, r=["Tcm"], w=["Tcm"])
                recip(Tcm[:, 0:511], Tcm[:, 0:511], r=["Tcm"], w=["Tcm"])
                tt("dve", HD[hc][:, 0:511], Ucm[:, 0:511], Tcm[:, 0:511], ALU.mult, r=["Tcm", "Ucm"], w=[f"HD{hc}"])
            if kv == 0:
                for hc in range(2):
                    mm(ps[6][0:64, 0:511], W2[:, hc, :], HD[hc][:, 0:511], hc == 0, hc == 1, r=["W2", f"HD{hc}"], w=["ps6"])
                qknorm(ps[6][0:64, 0:511], "ps6", 64, 511, gk2[0:64, 1:2], KC[g][0:64, 0:511], f"KC{g}")
            else:
                for cc in range(4):
                    for hc in range(2):
                        mm(ps[6][:, cc * 64:(cc + 1) * 64], HD[hc][:, cc * 128:(cc + 1) * 128], W2[:, hc, :], hc == 0, hc == 1,
                           r=["W2", f"HD{hc}"], w=["ps6"])
                cp("dve", VC[g][:, :, 0:64], ps[6][:, 0:256].rearrange("p (c d) -> p c d", d=64), r=["ps6"], w=[f"VC{g}"])
    if "dbgB" in debug and stop_after == 2:
        for g in range(2):
            cp("dve", Ucm[:, :], KC[g][:, :], r=[f"KC{g}"], w=["Ucm"])
            dma("sp", dbgB[:, g * 512:(g + 1) * 512], Ucm[:], r=["Ucm"], w=["dbgB"])
            cp("dve", Tcm[:, :], VC[g][:].rearrange("p c d -> p (c d)"), r=[f"VC{g}"], w=["Tcm"])
            dma("sp", dbgB[:, 1024 + g * 512:1024 + (g + 1) * 512], Tcm[:], r=["Tcm"], w=["dbgB"])
    S.barrier()
    A.release(m1)
    if stop_after <= 2:
        return finish()

    HQ = A.alloc("HQ", [128, 8, 2048], BF16)
    WQn = A.alloc("WQn", [128, 8, 536], BF16)
    dma("pool", WQn[:, :, 0:512], w_in_v[:, :, 1544:2056], w=["WQn"])
    dma("pool", WQn[:, :, 512:536], w_in_v[:, :, 2824:2848], w=["WQn"])
    SGHL = A.alloc("SGHL", [64, 2048], BF16)
    memset("pool", SGHL[:], 0.0, w=["SGHL"])
    QT = [A.alloc(f"QT{i}", [128, 2048], BF16) for i in range(2)]
    Pb = [A.alloc(f"P{i}", [128, 512], BF16) for i in range(4)]
    AM = A.alloc("AM", [128, 4, 128], BF16)
    DG = A.alloc("DG", [128, 4, 512], BF16)
    etmp = [A.alloc(f"etmp{i}", [128, 512], F32) for i in range(2)]
    et2 = A.alloc("et2", [128, 512], F32)
    dma("pool", AM[:], C["c_am"][:], w=["AM"])
    dma("pool", DG[:], C["c_dg"][:], w=["DG"])
    for i in range(2):
        memset("pool", QT[i][64:128, :], 0.0, w=[f"QT{i}"])
    mF = A.mark()
    AQH = A.alloc("AQH", [8, 2048], BF16)
    AQL = A.alloc("AQL", [8, 2048], BF16)
    WQf = A.alloc("WQf", [128, 8, 520], BF16)
    dma("pool", WQf[:, :, 0:512], w_in_v[:, :, 0:512], w=["WQf"])
    dma("pool", WQf[:, :, 512:520], w_in_v[:, :, 1536:1544], w=["WQf"])
    mT = A.mark()
    nbfc = A.alloc("nbfc", [128, 1], F32)
    dma("sp", nbfc[:], nbfc_d[:], w=["nbfc"])
    AQ = A.alloc("AQ", [8, 2048], F32)
    onesf = A.alloc("onesf", [128, 512], F32)
    memset("pool", onesf[:], 1.0, w=["onesf"])
    xq_t = A.alloc("xq_t", [128, 4, D], F32)
    junk = A.alloc("junk2", [128, D], BF16)
    ssq = A.alloc("ssq", [128, 4], F32)
    rsq = A.alloc("rsq", [128, 4], F32)
    zq = A.alloc("zq", [32, 512], F32)
    for s in range(4):
        norm_transpose(xq[s * 512:(s + 1) * 512, :], xq_t, "xq_t", HQ[:, :, s * 512:(s + 1) * 512], "HQ", ssq, rsq, "ssq",
                       gmix, "gmix", None)
    for s in range(4):
        sl = slice(s * 512, (s + 1) * 512)
        pb = ps[6]
        for kc in range(8):
            mm(pb[0:8, :], WQf[:, kc, 512:520], HQ[:, kc, sl], kc == 0, kc == 7, r=["WQf", "HQ"], w=["ps6"])
        act(zq[0:8, :], pb[0:8, :], AF.Exp, r=["ps6", "nbfc"], w=["zq"], scale=-1.0, bias=nbfc[0:8, :])
        ts("dve", zq[0:8, :], zq[0:8, :], 1.0, None, ALU.add, r=["zq"], w=["zq"])
        act(zq[0:8, :], zq[0:8, :], AF.Ln, r=["zq"], w=["zq"])
        S.op("dve", lambda e, sl=sl: e.tensor_tensor_scan(out=AQ[0:8, sl], data0=onesf[0:8, :], data1=zq[0:8, :],
                                                          initial=0.0, op0=ALU.mult, op1=ALU.subtract),
             r=["zq", "onesf"], w=["AQ"])
        pb = ps[7]
        for kc in range(8):
            mm(pb[0:24, :], WQn[:, kc, 512:536], HQ[:, kc, sl], kc == 0, kc == 7, r=["WQn", "HQ"], w=["ps7"])
        act(zq[0:24, :], pb[0:24, :], AF.Exp, r=["ps7"], w=["zq"], scale=-1.0)
        ts("dve", zq[0:24, :], zq[0:24, :], 1.0, None, ALU.add, r=["zq"], w=["zq"])
        recip(zq[0:24, :], zq[0:24, :], r=["zq"], w=["zq"])
        cp("dve", SGHL[0:24, sl], zq[0:24, :], r=["zq"], w=["SGHL"])
        tt("dve", SGHL[32:56, sl], zq[0:24, :], SGHL[0:24, sl], ALU.subtract, r=["zq", "SGHL"], w=["SGHL"])
    cp("dve", AQH[:], AQ[:], r=["AQ"], w=["AQH"])
    tt("dve", AQL[:], AQ[:], AQH[:], ALU.subtract, r=["AQ", "AQH"], w=["AQL"])
    S.barrier()
    A.release(mT)

    tile_ctr = [0]
    slot_ctr = [0]

    def attn(qt, qres, tiles, oi, after_exps=None):
        n = len(tiles)
        base = tile_ctr[0]
        tile_ctr[0] += n

        def qk(i):
            t = tiles[i]
            b = (base + i) % 3
            ex = t["extras"]
            mm(ps[b][:], t["k"], qt, True, len(ex) == 0, r=[t["kres"], qres], w=[f"ps{b}"])
            for ei, (l, rr, res) in enumerate(ex):
                mm(ps[b][:], l, rr, False, ei == len(ex) - 1, r=res, w=[f"ps{b}"])

        LA = 2
        for i in range(min(LA, n)):
            qk(i)
        for i in range(n):
            if i + LA < n:
                qk(i + LA)
            t = tiles[i]
            b = (base + i) % 3
            pi = (base + i) % 4
            act(Pb[pi][:], ps[b][:], AF.Exp, r=[f"ps{b}", t["bres"]], w=[f"P{pi}"], bias=t["bias"])
            mm(ps[oi][:], t["v"], Pb[pi][:], i == 0, i == n - 1, r=[t["vres"], f"P{pi}"], w=[f"ps{oi}"])
        if after_exps is not None:
            after_exps([Pb[(base + i) % 4] for i in range(n)], [f"P{(base + i) % 4}" for i in range(n)])

    def qproj_pair(Wt, wres, c0, gcol, qa, ra, qb_, rb):
        for s in range(4):
            sl = slice(s * 512, (s + 1) * 512)
            for kc in range(8):
                mm(ps[6][:], Wt[:, kc, c0:c0 + 128], HQ[:, kc, sl], kc == 0, kc == 7, r=[wres, "HQ"], w=["ps6"])
            qknorm(ps[6][:], "ps6", 128, 512, gcol, None, None, qscale=True, psum_idx=7,
                   split=(qa[0:64, sl], ra, qb_[0:64, sl], rb))

    KB = [A.alloc(f"KB{i}", [128, S_LEN], BF16) for i in range(2)]
    VB = [A.alloc(f"VB{i}", [128, NT, 128], BF16) for i in range(2)]
    OST = A.alloc("OST", [128, 2048], BF16)
    for i in range(2):
        memset("pool", KB[i][64:128, :], 0.0, w=[f"KBa{i}"])
        dma("pool", KB[i][64:65, :], C["c_onerow"][:, :], r=[f"KBa{i}"], w=[f"KBa{i}"])
        dma("pool", KB[i][96:97, :], C["c_onerow"][:, :], r=[f"KBa{i}"], w=[f"KBa{i}"])
    for i in range(4):
        qproj_pair(WQf, "WQf", i * 128, gk2[:, 2:3], QT[0], "QT0", QT[1], "QT1")
        for par in range(2):
            qt, qr = QT[par], f"QT{par}"
            h = 2 * i + par
            dma("sp", qt[64:65, :], AQH[h:h + 1, :], r=["AQH"], w=[qr])
            dma("sp", qt[96:97, :], AQL[h:h + 1, :], r=["AQL"], w=[qr])
            kb, vb = KB[h % 2], VB[h % 2]
            for qd in range(4):
                dma("sp", kb[0:64, qd * 2048:(qd + 1) * 2048], KT_fox[h, :, qd * 2048:(qd + 1) * 2048],
                    r=["KT_scr"], w=[f"KB{h % 2}_{qd}"])
                dma("sp", vb[:, qd * 16:(qd + 1) * 16, :], V_fox[h, :, qd * 16:(qd + 1) * 16, :],
                    r=["V_scr"], w=[f"VB{h % 2}_{qd}"])
            for s in range(4):
                sl = slice(s * 512, (s + 1) * 512)
                tiles = []
                for t in range(16 * s + 16):
                    ex = []
                    if t >= 16 * s:
                        m_, tl = (t - 16 * s) // 4, (t - 16 * s) % 4
                        ex = [(AM[:, m_, :], DG[:, tl, :], ["AM", "DG"])]
                    tiles.append(dict(k=kb[0:97, t * 128:(t + 1) * 128], kres=f"KB{h % 2}_{t // 16}", extras=ex,
                                      bias=BKB[:, (s * 8 + h) * 64 + t:(s * 8 + h) * 64 + t + 1], bres="BKB",
                                      v=vb[:, t, :], vres=f"VB{h % 2}_{t // 16}"))
                oi = 3 + slot_ctr[0] % 2
                et = etmp[slot_ctr[0] % 2]
                er = f"etmp{slot_ctr[0] % 2}"
                slot_ctr[0] += 1
                attn(qt[0:97, sl], qr, tiles, oi)
                recip(et[0:64, :], ps[oi][64:128, :], r=[f"ps{oi}"], w=[er])
                tt("dve", OST[par * 64:(par + 1) * 64, sl], ps[oi][0:64, :], et[0:64, :], ALU.mult,
                   r=[f"ps{oi}", er], w=["OST"])
        dma("sp", OA_scr[i], OST[:], r=["OST"], w=["OA_scr"])
    S.barrier()
    A.release(mF)
    if stop_after <= 3:
        return finish()

    dma("sp", BKB[:], C["c_kbs"][:], w=["BKB"])
    OBT = A.alloc("OBT", [128, 4, 2048], BF16)
    NST = [A.alloc(f"NST{g}", [128, 4, 512], BF16) for g in range(2)]
    SELG = A.alloc("SELG", [64, 24, 128], BF16)
    OV = A.alloc("OV", [128, 4, 128], BF16)
    KBW = A.alloc("KBW", [128, 256], F32)
    KBC = A.alloc("KBC", [128, 128], F32)
    X16 = A.alloc("X16", [128, S_LEN], BF16)
    dma("pool", SELG[:], C["c_selg"][:].rearrange("p (a b) -> p a b", b=128), w=["SELG"])
    dma("pool", OV[:], C["c_ov"][:].rearrange("p (a b) -> p a b", b=128), w=["OV"])
    dma("sp", KBW[:], C["c_kbw"][:], w=["KBW"])
    dma("sp", KBC[:], C["c_kbc"][:], w=["KBC"])
    CMv = X16[:].rearrange("p (s c q) -> p s c q", s=4, c=4)
    cm_flat = C["c_cm"].rearrange("p s c q -> p (s c q)")
    for qd in range(4):
        dma("pool", X16[:, qd * 2048:(qd + 1) * 2048], cm_flat[:, qd * 2048:(qd + 1) * 2048], w=["X16"])

    def nsa_qpair(i):
        qproj_pair(WQn, "WQn", i * 128, gk2[:, 3:4], QT[0], "QT0", QT[1], "QT1")
        for par in range(2):
            h = 2 * i + par
            dma("pool", QT[par][64:65, :], C["c_qaug"][h, 0:1, :], w=[f"QT{par}"])
            dma("pool", QT[par][96:97, :], C["c_qaug"][h, 1:2, :], w=[f"QT{par}"])

    def nsa_epilogue(h, br, s, oi):
        i, par = h // 2, h % 2
        sl = slice(s * 512, (s + 1) * 512)
        et = etmp[slot_ctr[0] % 2]
        er = f"etmp{slot_ctr[0] % 2}"
        lo, hi = par * 64, par * 64 + 64
        ts("dve", et[0:64, :], ps[oi][64:128, :], 1e-30, None, ALU.max, r=[f"ps{oi}"], w=[er])
        recip(et[0:64, :], et[0:64, :], r=[er], w=[er])
        tt("dve", et2[lo:hi, :], ps[oi][0:64, :], et[0:64, :], ALU.mult, r=[f"ps{oi}", er], w=["et2"])
        mm(ps[5][:], SELG[:, h * 3 + br, :], SGHL[0:64, sl], True, True, r=["SELG", "SGHL"], w=["ps5"])
        if br == 0:
            tt("dve", OBT[lo:hi, i, sl], et2[lo:hi, :], ps[5][lo:hi, :], ALU.mult, r=["et2", "ps5"], w=["OBT"])
        else:
            tt("dve", et2[lo:hi, :], et2[lo:hi, :], ps[5][lo:hi, :], ALU.mult, r=["et2", "ps5"], w=["et2"])
            tt("dve", OBT[lo:hi, i, sl], OBT[lo:hi, i, sl], et2[lo:hi, :], ALU.add, r=["et2", "OBT"], w=["OBT"])

    mC = A.mark()
    IMP = A.alloc("IMP", [128, 4, 512], F32)
    ASEL = A.alloc("ASEL", [128, 2048], BF16)
    BSEL = A.alloc("BSEL", [128, 2048], BF16)
    SC = A.alloc("SC", [128, 512], F32)
    SC2 = A.alloc("SC2", [128, 128], F32)
    tmpU = A.alloc("tmpU", [128, 512], F32)
    l4 = A.alloc("l4", [128, 4], F32)
    m8 = A.alloc("m8", [128, 16], F32)
    nsel = [A.alloc(f"nsel{i}", [128, 128], BF16) for i in range(2)]
    dma("pool", ASEL[:], C["c_asel"][:], w=["ASEL"])
    dma("pool", BSEL[:], C["c_bsel"][:], w=["BSEL"])
    for i in range(4):
        g = i // 2
        nsa_qpair(i)
        for par in range(2):
            h = 2 * i + par
            first_of_g = (h % 4 == 0)
            qt, qr = QT[par], f"QT{par}"
            for s in range(4):
                sl = slice(s * 512, (s + 1) * 512)
                tiles = [dict(k=KC[g][0:97, c * 128:(c + 1) * 128], kres=f"KC{g}",
                              extras=[(ident_b[:], CMv[:, s, c, :], ["ident_b", "X16"])],
                              bias=KBC[:, (h * 4 + s) * 4 + c:(h * 4 + s) * 4 + c + 1], bres="KBC",
                              v=VC[g][:, c, :], vres=f"VC{g}") for c in range(4)]
                oi = 3 + slot_ctr[0] % 2

                def imp_fn(Ps, Pres, s=s, first_of_g=first_of_g):
                    for sub in range(4):
                        for c in range(4):
                            mm(ps[6][:, sub * 128:(sub + 1) * 128], Ps[c][:, sub * 128:(sub + 1) * 128], OV[:, c, :],
                               c == 0, c == 3, r=[Pres[c], "OV"], w=["ps6"])
                    U3 = ps[6][:].rearrange("p (a j) -> p a j", j=128)
                    S.op("dve", lambda e: e.tensor_reduce(out=l4[:], in_=U3, axis=AX.X, op=ALU.add), r=["ps6"], w=["l4"])
                    ts("dve", l4[:], l4[:], 1e-30, None, ALU.max, r=["l4"], w=["l4"])
                    recip(l4[:], l4[:], r=["l4"], w=["l4"])
                    rlb = l4[:].unsqueeze(2).to_broadcast([128, 4, 128])
                    I3 = IMP[:, s, :].rearrange("p (a j) -> p a j", j=128)
                    if first_of_g:
                        tt("dve", I3, U3, rlb, ALU.mult, r=["ps6", "l4"], w=["IMP"])
                    else:
                        tt("dve", tmpU[:].rearrange("p (a j) -> p a j", j=128), U3, rlb, ALU.mult, r=["ps6", "l4"], w=["tmpU"])
                        tt("dve", IMP[:, s, :], IMP[:, s, :], tmpU[:], ALU.add, r=["tmpU", "IMP"], w=["IMP"])

                attn(qt[0:97, sl], qr, tiles, oi, after_exps=imp_fn)
                nsa_epilogue(h, 0, s, oi)
                slot_ctr[0] += 1
        if i % 2 == 1:
            for s in range(4):
                tt("dve", SC[:], IMP[:, s, :], ASEL[:, s * 512:(s + 1) * 512], ALU.mult, r=["IMP", "ASEL"], w=["SC"])
                tt("dve", SC[:], SC[:], BSEL[:, s * 512:(s + 1) * 512], ALU.add, r=["SC", "BSEL"], w=["SC"])
                for sub in range(4):
                    scs = SC[:, sub * 128:(sub + 1) * 128]
                    ns = nsel[sub % 2]
                    S.op("dve", lambda e, scs=scs: e.max(out=m8[:, 0:8], in_=scs), r=["SC"], w=["m8"])
                    S.op("dve", lambda e, scs=scs: e.match_replace(out=SC2[:], in_to_replace=m8[:, 0:8], in_values=scs,
                                                                  imm_value=-1e9), r=["SC", "m8"], w=["SC2"])
                    S.op("dve", lambda e: e.max(out=m8[:, 8:16], in_=SC2[:]), r=["SC2"], w=["m8"])
                    ts("dve", ns[:], scs, m8[:, 15:16], NEGM, ALU.is_lt, ALU.mult, r=["SC", "m8"], w=[f"nsel{sub % 2}"])
                    tr(ps7b[:, sub * 128:(sub + 1) * 128], ns[:], ident_b[:], r=[f"nsel{sub % 2}", "ident_b"], w=["ps7"])
                cp("act", NST[g][:, s, :], ps7b[:, 0:512], r=["ps7"], w=[f"NST{g}"])
    if "dbgA" in debug and stop_after == 4:
        for g in range(2):
            for s in range(4):
                cp("dve", SC[:], NST[g][:, s, :], r=[f"NST{g}"], w=["SC"])
                dma("sp", dbgA[:, (g * 4 + s) * 512:(g * 4 + s + 1) * 512], SC[:], r=["SC"], w=["dbgA"])
    S.barrier()
    A.release(mC)
    if stop_after <= 4:
        if "dbgB" in debug:
            for i in range(4):
                cp("dve", etmp[0][:], OBT[:, i, 0:512], r=["OBT"], w=["etmp0"])
                dma("sp", dbgB[:, i * 512:(i + 1) * 512], etmp[0][:], r=["etmp0"], w=["dbgB"])
        return finish()

    for qd in range(4):
        dma("pool", X16[:, qd * 2048:(qd + 1) * 2048], C["c_eall"][:, qd * 2048:(qd + 1) * 2048], w=["X16"])
    WM = A.alloc("WM", [128, 8, 512], BF16)
    dma("pool", WM[:], C["c_wm"][:], w=["WM"])
    KS = A.alloc("KS", [128, S_LEN], BF16)
    VS = A.alloc("VS", [128, NT, 128], BF16)
    KW = A.alloc("KW", [128, 4096], BF16)
    VW = A.alloc("VW", [128, 32, 128], BF16)
    for (kbuf, n, nm) in [(KS, S_LEN, "KSa"), (KW, 4096, "KWa")]:
        memset("pool", kbuf[64:128, :], 0.0, w=[nm])
        dma("pool", kbuf[64:65, :], C["c_onerow"][:, 0:n], r=[nm], w=[nm])
        dma("pool", kbuf[96:97, :], C["c_onerow"][:, 0:n], r=[nm], w=[nm])
    for g in range(2):
        for qd in range(4):
            dma("sp", KS[0:64, qd * 2048:(qd + 1) * 2048], KT_s[g, :, qd * 2048:(qd + 1) * 2048], r=["KT_scr"], w=[f"KS_{qd}"])
            dma("sp", VS[:, qd * 16:(qd + 1) * 16, :], V_s[g, :, qd * 16:(qd + 1) * 16, :], r=["V_scr"], w=[f"VS_{qd}"])
        dma("sp", KW[0:64, :], KT_w[g], r=["KT_scr"], w=["KW"])
        dma("sp", VW[:], V_w[g], r=["V_scr"], w=["VW"])
        for ip in range(2):
            i = 2 * g + ip
            nsa_qpair(i)
            for par in range(2):
                h = 2 * i + par
                qt, qr = QT[par], f"QT{par}"
                for s in range(4):
                    sl = slice(s * 512, (s + 1) * 512)
                    tiles = []
                    for t in range(16 * s + 16):
                        ex = [(X16[:, t * 128:(t + 1) * 128], NST[g][:, s, :], ["X16", f"NST{g}"])]
                        if t >= 16 * s:
                            m_, tl = (t - 16 * s) // 4, (t - 16 * s) % 4
                            ex.append((AM[:, m_, :], DG[:, tl, :], ["AM", "DG"]))
                        bi = (h * 4 + s) * 64 + t
                        tiles.append(dict(k=KS[0:97, t * 128:(t + 1) * 128], kres=f"KS_{t // 16}", extras=ex,
                                          bias=BKB[:, bi:bi + 1], bres="BKB", v=VS[:, t, :], vres=f"VS_{t // 16}"))
                    oi = 3 + slot_ctr[0] % 2
                    attn(qt[0:97, sl], qr, tiles, oi)
                    nsa_epilogue(h, 1, s, oi)
                    slot_ctr[0] += 1
                for s in range(4):
                    sl = slice(s * 512, (s + 1) * 512)
                    tiles = []
                    for t in range(8):
                        bi = (h * 4 + s) * 8 + t
                        tiles.append(dict(k=KW[0:97, (8 * s + t) * 128:(8 * s + t + 1) * 128], kres="KW",
                                          extras=[(ident_b[:], WM[:, t, :], ["ident_b", "WM"])],
                                          bias=KBW[:, bi:bi + 1], bres="KBW", v=VW[:, 8 * s + t, :], vres="VW"))
                    oi = 3 + slot_ctr[0] % 2
                    attn(qt[0:97, sl], qr, tiles, oi)
                    nsa_epilogue(h, 2, s, oi)
                    slot_ctr[0] += 1
    if "dbgB" in debug and stop_after == 5:
        for i in range(4):
            for s in range(4):
                cp("dve", etmp[0][:], OBT[:, i, s * 512:(s + 1) * 512], r=["OBT"], w=["etmp0"])
                dma("sp", dbgB_big[:, (i * 4 + s) * 512:(i * 4 + s + 1) * 512], etmp[0][:], r=["etmp0"], w=["dbgB_big"])
    for i in range(4):
        dma("sp", OB_scr[i], OBT[:, i, :], r=["OBT"], w=["OB_scr"])
    S.barrier()
    A.release(m_base)
    if stop_after <= 5:
        return finish()

    ACC = A.alloc("ACC", [128, 16, D], F32)
    TT = A.alloc("TT", [128, 8, 2048], BF16)
    COMB = A.alloc("COMB", [128, 16, 16], F32)
    gffn = A.alloc("gffn", [128, 8], F32)
    dma("sp", gffn[:], gffn_d[:], w=["gffn"])
    m5 = A.mark()
    WA = A.alloc("WA", [128, 4, D], BF16)
    WB = A.alloc("WB", [128, 4, D], BF16)
    WO = A.alloc("WO", [128, 8, D], BF16)
    dma("pool", WA[:], wfu_d.rearrange("(c p) n -> p c n", p=128), w=["WA"])
    dma("pool", WB[:], wnu_d.rearrange("(c p) n -> p c n", p=128), w=["WB"])
    dma("pool", WO[:], wout_d.rearrange("(c p) n -> p c n", p=128), w=["WO"])
    WRf = A.alloc("WRf", [128, 8, 20], F32)
    WRh = A.alloc("WRh", [128, 8, 20], BF16)
    WRl = A.alloc("WRl", [128, 8, 20], BF16)
    brt = A.alloc("brt", [128, 20], F32)
    dma("sp", WRf[:], wr_d.rearrange("(c p) n -> p c n", p=128), w=["WRf"])
    dma("sp", brt[:], br_d[:], w=["brt"])
    cp("dve", WRh[:], WRf[:], r=["WRf"], w=["WRh"])
    tt("dve", WRl[:], WRf[:], WRh[:], ALU.subtract, r=["WRf", "WRh"], w=["WRl"])
    WGs = [A.alloc(f"WGs{i}", [128, 8, 128], BF16) for i in range(4)]
    xt3 = A.alloc("xt3", [128, 4, D], F32)
    junk = A.alloc("junk3", [128, D], BF16)
    ss3 = A.alloc("ss3", [128, 4], F32)
    rs3 = A.alloc("rs3", [128, 4], F32)
    HQs = A.alloc("HQs", [128, 8, 512], BF16)
    OAs = A.alloc("OAs", [128, 4, 512], BF16)
    OBs = A.alloc("OBs", [128, 4, 512], BF16)
    MIX = A.alloc("MIX", [128, 8, 512], BF16)
    sgt = [A.alloc(f"sgt{i}", [128, 512], F32) for i in range(2)]
    mxa = A.alloc("mxa", [128, 512], F32)
    tlo = A.alloc("tlo", [128, 8, 512], BF16)
    RT = A.alloc("RT", [128, 64], F32)
    wg_ctr = [0]
    for s in range(4):
        sl = slice(s * 512, (s + 1) * 512)
        norm_transpose(xq[sl, :], xt3, "xt3", HQs, "HQs", ss3, rs3, "ss3", gmix, "gmix", None)
        dma("sp", OAs[:], OA_scr.rearrange("c p t -> p c t")[:, :, sl], r=["OA_scr"], w=["OAs"])
        dma("sp", OBs[:], OB_scr.rearrange("c p t -> p c t")[:, :, sl], r=["OB_scr"], w=["OBs"])
        dma("sp", xt3[:], xq[sl, :].rearrange("(u p) d -> p u d", p=128), r=["HQs"], w=["xt3"])
        for m in range(8):
            for ab in range(2):
                wgb = WGs[wg_ctr[0] % 4]
                wgr = f"WGs{wg_ctr[0] % 4}"
                wg_ctr[0] += 1
                c0 = 2848 + ab * 1024 + m * 128
                dma("pool", wgb[:], w_in_v[:, :, c0:c0 + 128], w=[wgr])
                pg, pgr = ps[2 * ab], f"ps{2 * ab}"
                po, por = ps[2 * ab + 1], f"ps{2 * ab + 1}"
                for kc in range(8):
                    mm(pg[:], wgb[:, kc, :], HQs[:, kc, :], kc == 0, kc == 7, r=[wgr, "HQs"], w=[pgr])
                Wx, wxr, Ox, oxr = (WA, "WA", OAs, "OAs") if ab == 0 else (WB, "WB", OBs, "OBs")
                for c in range(4):
                    mm(po[:], Wx[:, c, m * 128:(m + 1) * 128], Ox[:, c, :], c == 0, c == 3, r=[wxr, oxr], w=[por])
                act(sgt[ab][:], pg[:], AF.Sigmoid, r=[pgr], w=[f"sgt{ab}"])
                if ab == 0:
                    tt("dve", mxa[:], po[:], sgt[0][:], ALU.mult, r=[por, "sgt0"], w=["mxa"])
                else:
                    tt("dve", sgt[1][:], po[:], sgt[1][:], ALU.mult, r=[por, "sgt1"], w=["sgt1"])
                    tt("dve", MIX[:, m, :], mxa[:], sgt[1][:], ALU.add, r=["mxa", "sgt1"], w=["MIX"])
        for u in range(4):
            for hf in range(2):
                bi = 4 + (u * 2 + hf) % 2
                for kc in range(8):
                    mm(ps[bi][:], MIX[:, kc, u * 128:(u + 1) * 128], WO[:, kc, hf * 512:(hf + 1) * 512], kc == 0, kc == 7,
                       r=["MIX", "WO"], w=[f"ps{bi}"])
                tt("dve", ACC[:, s * 4 + u, hf * 512:(hf + 1) * 512], ps[bi][:], xt3[:, u, hf * 512:(hf + 1) * 512], ALU.add,
                   r=[f"ps{bi}", "xt3"], w=["ACC"])
        for u in range(4):
            act(junk[:], ACC[:, s * 4 + u, :], AF.Square, r=["ACC"], w=["junk", "ss3"], accum_out=ss3[:, u:u + 1])
        ts("dve", rs3[:], ss3[:], 1.0 / D, 1e-6, ALU.mult, ALU.add, r=["ss3"], w=["ss3r"])
        act(rs3[:], rs3[:], AF.Sqrt, r=["ss3r"], w=["ss3r"])
        recip(rs3[:], rs3[:], r=["ss3r"], w=["ss3r"])
        for u in range(4):
            ts("dve", xt3[:, u, :], ACC[:, s * 4 + u, :], rs3[:, u:u + 1], None, ALU.mult, r=["ACC", "ss3r"], w=["xt3"])
        for kc in range(8):
            pb, pr = ps[6 + kc % 2], f"ps{6 + kc % 2}"
            for u in range(4):
                tr(pb[:, u * 128:(u + 1) * 128], xt3[:, u, kc * 128:(kc + 1) * 128], ident_f[:], r=["xt3", "ident_f"], w=[pr])
            S.op("act", lambda e, o=TT[:, kc, sl], i=pb[:], sc=gffn[:, kc:kc + 1]:
                 e.activation(out=o, in_=i, func=AF.Copy, scale=sc), r=[pr, "gffn"], w=["TT"])
            stt(tlo[:, kc, :], pb[:], gffn[:, kc:kc + 1], TT[:, kc, sl], ALU.mult, ALU.subtract, r=[pr, "gffn", "TT"], w=["tlo"])
        for u in range(4):
            tok = slice(s * 512 + u * 128, s * 512 + (u + 1) * 128)
            pl = ps[u % 2]
            plr = f"ps{u % 2}"
            k = 0
            for (lh, lr, wr_, wrr) in [("TT", None, WRh, "WRh"), ("tlo", None, WRh, "WRh"), ("TT", None, WRl, "WRl")]:
                for kc in range(8):
                    lhsT = TT[:, kc, tok] if lh == "TT" else tlo[:, kc, u * 128:(u + 1) * 128]
                    mm(pl[:, 0:20], lhsT, wr_[:, kc, :], k == 0, k == 23, r=[lh, wrr], w=[plr])
                    k += 1
            L = RT[:, 0:20]
            tt("dve", L, pl[:, 0:20], brt[:], ALU.add, r=[plr, "brt"], w=["RT"])
            gl, el = RT[:, 0:4], RT[:, 4:20]
            gmax, ngmax, sumg, pgc = RT[:, 20:21], RT[:, 21:22], RT[:, 22:23], RT[:, 23:24]
            ohg, ein, msk, e2 = RT[:, 24:28], RT[:, 28:32], RT[:, 32:36], RT[:, 36:40]
            m1, nm1, m2, den = RT[:, 40:41], RT[:, 41:42], RT[:, 42:43], RT[:, 43:44]
            ex, exg = RT[:, 44:48], RT[:, 48:52]
            rr = ["RT"]
            S.op("dve", lambda e, gl=gl, gmax=gmax: e.tensor_reduce(out=gmax, in_=gl, axis=AX.X, op=ALU.max), r=rr, w=rr)
            ts("dve", ngmax, gmax, -1.0, None, ALU.mult, r=rr, w=rr)
            ts("dve", ohg, gl, gmax, None, ALU.is_ge, r=rr, w=rr)
            act(exg, gl, AF.Exp, r=rr, w=rr, bias=ngmax, accum_out=sumg)
            recip(pgc, sumg, r=rr, w=rr)
            ts("dve", ein, el[:, 0:4], ohg[:, 0:1], None, ALU.mult, r=rr, w=rr)
            for g in range(1, 4):
                stt(ein, el[:, 4 * g:4 * g + 4], ohg[:, g:g + 1], ein, ALU.mult, ALU.add, r=rr, w=rr)
            S.op("dve", lambda e, ein=ein, m1=m1: e.tensor_reduce(out=m1, in_=ein, axis=AX.X, op=ALU.max), r=rr, w=rr)
            ts("dve", msk, ein, m1, -1e30, ALU.is_ge, ALU.mult, r=rr, w=rr)
            tt("dve", e2, msk, ein, ALU.add, r=rr, w=rr)
            S.op("dve", lambda e, e2=e2, m2=m2: e.tensor_reduce(out=m2, in_=e2, axis=AX.X, op=ALU.max), r=rr, w=rr)
            ts("dve", msk, ein, m2, None, ALU.is_ge, r=rr, w=rr)
            ts("dve", nm1, m1, -1.0, None, ALU.mult, r=rr, w=rr)
            act(ex, ein, AF.Exp, r=rr, w=rr, bias=nm1)
            tt("dve", ex, ex, msk, ALU.mult, r=rr, w=rr)
            S.op("dve", lambda e, ex=ex, den=den: e.tensor_reduce(out=den, in_=ex, axis=AX.X, op=ALU.add), r=rr, w=rr)
            recip(den, den, r=rr, w=rr)
            ts("dve", ex, ex, den, pgc, ALU.mult, ALU.mult, r=rr, w=rr)
            for g in range(4):
                ts("dve", COMB[:, s * 4 + u, 4 * g:4 * g + 4], ex, ohg[:, g:g + 1], None, ALU.mult, r=rr, w=["COMB"])
    if "dbgB_big" in debug and stop_after == 6:
        for u in range(8):
            dma("sp", dbgB_big[:, u * 1024:(u + 1) * 1024], ACC[:, u, :], r=["ACC"], w=["dbgB_big"])
        dma("sp", dbgA[:, 0:256], COMB[:].rearrange("p a b -> p (a b)"), r=["COMB"], w=["dbgA"])
    S.barrier()
    A.release(m5)
    if stop_after <= 6:
        return finish()

    WGb = [A.alloc(f"WGb{i}", [128, 8, 512], BF16) for i in range(2)]
    WUb = [A.alloc(f"WUb{i}", [128, 8, 512], BF16) for i in range(2)]
    WDb = [A.alloc(f"WDb{i}", [128, 4, D], BF16) for i in range(2)]
    hid = [A.alloc(f"hid{i}", [128, 512], BF16) for i in range(8)]
    sgm = [A.alloc(f"sgm{i}", [128, 512], F32) for i in range(2)]
    hctr = [0]
    for e_ in range(16):
        bi = e_ % 2
        wgv = wg_d[e_].rearrange("(c p) n -> p c n", p=128)
        wuv = wu_d[e_].rearrange("(c p) n -> p c n", p=128)
        wdv = wd_d[e_].rearrange("(c p) n -> p c n", p=128)
        for hlf in range(2):
            dma("pool", WGb[bi][:, hlf * 4:(hlf + 1) * 4, :], wgv[:, hlf * 4:(hlf + 1) * 4, :], w=[f"WGb{bi}"])
            dma("pool", WUb[bi][:, hlf * 4:(hlf + 1) * 4, :], wuv[:, hlf * 4:(hlf + 1) * 4, :], w=[f"WUb{bi}"])
            dma("pool", WDb[bi][:, hlf * 2:(hlf + 1) * 2, :], wdv[:, hlf * 2:(hlf + 1) * 2, :], w=[f"WDb{bi}"])
        for tg in range(4):
            tsl = slice(tg * 512, (tg + 1) * 512)
            hs = []
            for fc in range(4):
                pgi, pui = (fc % 2) * 2, (fc % 2) * 2 + 1
                for kc in range(8):
                    mm(ps[pgi][:], WGb[bi][:, kc, fc * 128:(fc + 1) * 128], TT[:, kc, tsl], kc == 0, kc == 7,
                       r=[f"WGb{bi}", "TT"], w=[f"ps{pgi}"])
                for kc in range(8):
                    mm(ps[pui][:], WUb[bi][:, kc, fc * 128:(fc + 1) * 128], TT[:, kc, tsl], kc == 0, kc == 7,
                       r=[f"WUb{bi}", "TT"], w=[f"ps{pui}"])
                sg_ = sgm[fc % 2]
                hh_ = hid[hctr[0] % 8]
                hr = f"hid{hctr[0] % 8}"
                hctr[0] += 1
                act(sg_[:], ps[pgi][:], AF.Silu, r=[f"ps{pgi}"], w=[f"sgm{fc % 2}"])
                tt("dve", hh_[:], ps[pui][:], sg_[:], ALU.mult, r=[f"ps{pui}", f"sgm{fc % 2}"], w=[hr])
                hs.append((hh_, hr))
            for u in range(4):
                for hf in range(2):
                    yi = 4 + (u * 2 + hf) % 4
                    for fc in range(4):
                        mm(ps[yi][:], hs[fc][0][:, u * 128:(u + 1) * 128], WDb[bi][:, fc, hf * 512:(hf + 1) * 512],
                           fc == 0, fc == 3, r=[hs[fc][1], f"WDb{bi}"], w=[f"ps{yi}"])
                    a_ = ACC[:, tg * 4 + u, hf * 512:(hf + 1) * 512]
                    stt(a_, ps[yi][:], COMB[:, tg * 4 + u, e_:e_ + 1], a_, ALU.mult, ALU.add,
                        r=[f"ps{yi}", "COMB", "ACC"], w=["ACC"])
    for u in range(16):
        dma("sp", out_d[u * 128:(u + 1) * 128, :], ACC[:, u, :], r=["ACC"], w=["out"])
    return finish()


def make_in_maps(inputs):
    f = np.float32
    g = lambda k: np.asarray(inputs[k], f)
    x = g("x")
    maps = []
    shared = {}
    shared["w_in"] = np.ascontiguousarray(g("w_in")[0])
    shared["gmix"] = np.ascontiguousarray(g("norm_mix_g")[0].reshape(8, 128).T)
    shared["gffn"] = np.ascontiguousarray(g("norm_ffn_g")[0].reshape(8, 128).T)
    shared["gk2"] = np.ascontiguousarray(np.stack([np.tile(g("fox_k_g")[0], 2), np.tile(g("nsa_k_g")[0], 2),
                                                    np.tile(g("fox_q_g")[0], 2), np.tile(g("nsa_q_g")[0], 2)], 1))
    shared["bfb"] = np.ascontiguousarray(np.tile(g("b_forget")[0][None, :], (128, 1)))
    nb = np.zeros((128, 1), f)
    nb[:8, 0] = -g("b_forget")[0]
    shared["nbfc"] = nb
    shared["cmp_k_w1"] = np.ascontiguousarray(g("cmp_k_w1")[0])
    shared["cmp_v_w1"] = np.ascontiguousarray(g("cmp_v_w1")[0])
    shared["cmp_k_w2"] = np.ascontiguousarray(g("cmp_k_w2")[0])
    shared["cmp_v_w2"] = np.ascontiguousarray(g("cmp_v_w2")[0])
    shared["poskT"] = np.ascontiguousarray(np.tile(g("cmp_k_pos")[0].T, (2, 1)))
    shared["posvT"] = np.ascontiguousarray(np.tile(g("cmp_v_pos")[0].T, (2, 1)))
    shared["w_fox_up"] = np.ascontiguousarray(g("w_fox_up")[0])
    shared["w_nsa_up"] = np.ascontiguousarray(g("w_nsa_up")[0])
    shared["w_out"] = np.ascontiguousarray(g("w_out")[0])
    shared["w_rt"] = np.ascontiguousarray(np.concatenate([g("w_group")[0], g("w_router")[0]], 1))
    shared["b_rt"] = np.ascontiguousarray(np.tile(np.concatenate([g("b_group")[0], g("b_router")[0]])[None, :], (128, 1)))
    shared["w_gate"] = np.ascontiguousarray(g("w_gate")[0])
    shared["w_up"] = np.ascontiguousarray(g("w_up")[0])
    shared["w_down"] = np.ascontiguousarray(g("w_down")[0])
    consts = [host_consts(j) for j in range(4)]
    for c in range(8):
        b, j = c // 4, c % 4
        m = dict(shared)
        m["xb"] = np.ascontiguousarray(x[b])
        m["xq"] = np.ascontiguousarray(np.concatenate([x[b, 512 * (4 * s + j):512 * (4 * s + j + 1)] for s in range(4)], 0))
        xw = np.zeros((4096, D), f)
        for s in range(4):
            q0 = 512 * (4 * s + j)
            lo = q0 - 512
            if lo >= 0:
                xw[1024 * s:1024 * (s + 1)] = x[b, lo:lo + 1024]
            else:
                xw[1024 * s + 512:1024 * (s + 1)] = x[b, 0:512]
        m["xw"] = xw
        for k, v in consts[j].items():
            m[k] = np.ascontiguousarray(v.reshape(CONST_SHAPES[k]))
        maps.append(m)
    return maps


def kernel(**inputs):
    nc = build()
    maps = make_in_maps(inputs)
    res = run_bass_kernel_spmd(nc, maps, core_ids=list(range(8)))
    out = np.zeros((2, S_LEN, D), np.float32)
    for c in range(8):
        b, j = c // 4, c % 4
        o = res.results[c]["out"]
        for s in range(4):
            out[b, 512 * (4 * s + j):512 * (4 * s + j + 1)] = o[512 * s:512 * (s + 1)]
    return out
```

```python
import contextlib
import numpy as np
import concourse.bass as bass
import concourse.mybir as mybir
from concourse.bass_utils import run_bass_kernel_spmd

F32 = mybir.dt.float32
BF16 = mybir.dt.bfloat16
AF = mybir.ActivationFunctionType
ALU = mybir.AluOpType
AX = mybir.AxisListType

S_LEN = 8192
D = 1024
NT = 64
NEGM = -30000.0
DIN = 4896


class _Op:
    __slots__ = ("id", "eng", "fn", "deps", "dma", "needs_inc", "sem", "val")

    def __init__(self, id, eng, fn, dma):
        self.id = id
        self.eng = eng
        self.fn = fn
        self.deps = []
        self.dma = dma
        self.needs_inc = False
        self.sem = None
        self.val = 0


class Sched:
    ENGS = ("pe", "act", "dve", "pool", "sp")
    NDMA = {"sp": 12, "pool": 8, "act": 2, "pe": 1, "dve": 1}

    def __init__(self, nc):
        self.nc = nc
        self.ops = {e: [] for e in self.ENGS}
        self.last_w = {}
        self.readers = {}
        self.n = 0
        self.dma_hist = {e: [] for e in self.ENGS}

    def op(self, eng, fn, r=(), w=(), dma=False):
        o = _Op(self.n, eng, fn, dma)
        self.n += 1
        deps = {}
        for res in r:
            lw = self.last_w.get(res)
            if lw is not None:
                deps[lw.id] = lw
        for res in w:
            lw = self.last_w.get(res)
            if lw is not None:
                deps[lw.id] = lw
            for rd in self.readers.get(res, ()):
                deps[rd.id] = rd
        if dma:
            hist = self.dma_hist[eng]
            n = self.NDMA[eng]
            if len(hist) >= n:
                p = hist[len(hist) - n]
                deps[p.id] = p
            hist.append(o)
        for d in deps.values():
            if d is o:
                continue
            if d.eng == eng and eng == "pe" and not d.dma and not dma:
                continue
            if not d.dma:
                d.needs_inc = True
            o.deps.append(d)
        for res in r:
            self.readers.setdefault(res, []).append(o)
        for res in w:
            self.last_w[res] = o
            self.readers[res] = []
        self.ops[eng].append(o)
        return o

    def barrier(self):
        lasts = []
        for e in self.ENGS:
            comp = [o for o in self.ops[e] if not o.dma and o.fn is not None]
            if comp:
                lasts.append(comp[-1])
            lasts.extend(self.dma_hist[e][-self.NDMA[e]:])
        for e in self.ENGS:
            o = _Op(self.n, e, None, False)
            self.n += 1
            for d in lasts:
                if d.eng == e and e == "pe" and not d.dma:
                    continue
                if not d.dma:
                    d.needs_inc = True
                o.deps.append(d)
            self.ops[e].append(o)
        self.last_w = {}
        self.readers = {}

    def emit(self):
        nc = self.nc
        with contextlib.ExitStack() as st:
            esem = {e: st.enter_context(nc.semaphore(f"s_{e}")) for e in self.ENGS}
            dsem = {e: [st.enter_context(nc.semaphore(f"d_{e}{i}")) for i in range(self.NDMA[e])]
                    for e in self.ENGS}
            for e in self.ENGS:
                cnt = 0
                dcnt = [0] * self.NDMA[e]
                k = 0
                for o in self.ops[e]:
                    if o.dma:
                        i = k % self.NDMA[e]
                        k += 1
                        dcnt[i] += 16
                        o.sem = dsem[e][i]
                        o.val = dcnt[i]
                    elif o.needs_inc:
                        cnt += 1
                        o.sem = esem[e]
                        o.val = cnt
            block = st.enter_context(nc.Block())

            def run(engobj, ops):
                waited = {}
                for o in ops:
                    need = {}
                    for d in o.deps:
                        key = id(d.sem)
                        if d.val > need.get(key, (0, None))[0]:
                            need[key] = (d.val, d.sem)
                    for key, (val, sem) in need.items():
                        if waited.get(key, 0) < val:
                            engobj.wait_ge(sem, val)
                            waited[key] = val
                    if o.fn is None:
                        continue
                    ins = o.fn(engobj)
                    if o.dma:
                        ins.then_inc(o.sem, 16)
                    elif o.needs_inc:
                        ins.then_inc(o.sem, 1)

            if self.ops["pe"]:
                @block.tensor
                def _(e):
                    run(e, self.ops["pe"])
            if self.ops["act"]:
                @block.scalar
                def _(e):
                    run(e, self.ops["act"])
            if self.ops["dve"]:
                @block.vector
                def _(e):
                    run(e, self.ops["dve"])
            if self.ops["pool"]:
                @block.gpsimd
                def _(e):
                    run(e, self.ops["pool"])
            if self.ops["sp"]:
                @block.sync
                def _(e):
                    run(e, self.ops["sp"])


class Arena:
    def __init__(self, nc, base=18560, limit=229376):
        self.nc = nc
        self.off = base
        self.limit = limit
        self.k = 0

    def alloc(self, name, shape, dt):
        nb = int(np.prod(shape[1:])) * (4 if dt == F32 else 2)
        nb = (nb + 63) // 64 * 64
        assert self.off + nb <= self.limit, f"SBUF overflow at {name}: {self.off}+{nb}"
        self.k += 1
        t = self.nc.alloc_sbuf_tensor_at(f"{name}_{self.k}", list(shape), dt, offset=self.off)
        self.off += nb
        self.hw = max(getattr(self, "hw", 0), self.off)
        return t.ap()

    def mark(self):
        return self.off

    def release(self, m):
        self.off = m


def host_consts(j):
    c = {}
    f = np.float32
    c["c_ident"] = np.eye(128, dtype=f)
    blk = np.zeros((128, 128), f)
    blk[:64, :64] = 1.0
    blk[64:, 64:] = 1.0
    c["c_blk"] = blk
    p = np.arange(128)
    c["c_utri"] = (p[:, None] <= p[None, :]).astype(f)
    c["c_ones"] = np.ones((128, 128), f)
    q0 = np.array([512 * (4 * s + j) for s in range(4)])
    t64 = np.arange(64)
    oh = np.zeros((128, 4, 64), f)
    badd = np.zeros((128, 4, 64), f)
    for s in range(4):
        oh[:, s, 4 * (4 * s + j)] = 1.0
        badd[:, s, 16 * s + 4 * (j + 1):16 * s + 16] = NEGM
    c["c_oh"] = oh
    c["c_badd"] = badd
    am = np.zeros((128, 4, 128), f)
    am[:, j, :] = np.eye(128, dtype=f)
    c["c_am"] = am
    ql = np.arange(512)
    dg = np.zeros((128, 4, 512), f)
    for tl in range(4):
        dg[:, tl, :] = np.where(128 * tl + p[:, None] <= ql[None, :], 0.0, NEGM)
    c["c_dg"] = dg
    wm = np.zeros((128, 8, 512), f)
    for t in range(8):
        dist = ql[None, :] - (128 * t + p[:, None]) + 512
        wm[:, t, :] = np.where((dist >= 0) & (dist < 512), 0.0, NEGM)
    c["c_wm"] = wm
    cm = np.zeros((128, 4, 4, 512), f)
    for s in range(4):
        for cc in range(4):
            n = 128 * cc + p[:, None]
            q = q0[s] + ql[None, :]
            cm[:, s, cc, :] = np.where((16 * n + 31 <= q) & (n < 511), 0.0, NEGM)
    c["c_cm"] = cm
    slopes = np.array([2.0 ** (-(h + 1)) for h in range(8)], np.float64)
    kbs = np.zeros((128, 8, 4, 64), f)
    kbw = np.zeros((128, 8, 4, 8), f)
    kbc = np.zeros((128, 8, 4, 4), f)
    for h in range(8):
        for s in range(4):
            kpos = 128 * t64[None, :] + p[:, None]
            kbs[:, h, s, :] = slopes[h] * (kpos - q0[s]) + badd[:, s, :]
            wpos = q0[s] - 512 + 128 * np.arange(8)[None, :] + p[:, None]
            kbw[:, h, s, :] = np.where(wpos >= 0, slopes[h] * (wpos - q0[s]), NEGM)
            cend = 16 * (128 * np.arange(4)[None, :] + p[:, None]) + 31
            kbc[:, h, s, :] = slopes[h] * (cend - q0[s])
    c["c_kbs"] = kbs.reshape(128, -1)
    c["c_kbw"] = kbw.reshape(128, -1)
    c["c_kbc"] = kbc.reshape(128, -1)
    asel = np.zeros((128, 4, 4, 128), f)
    bsel = np.zeros((128, 4, 4, 128), f)
    jb = np.arange(128)
    for s in range(4):
        for sub in range(4):
            q = q0[s] + 128 * sub + p[:, None]
            qb = q // 64
            future = 64 * jb[None, :] > q
            forced = (jb[None, :] == 0) | (jb[None, :] == qb) | (jb[None, :] == qb - 1)
            asel[:, s, sub, :] = np.where(future | forced, 0.0, 1.0)
            bsel[:, s, sub, :] = np.where(future, -1.0, np.where(forced, 1e4, 0.0))
    c["c_asel"] = asel.reshape(128, -1)
    c["c_bsel"] = bsel.reshape(128, -1)
    ov = np.zeros((128, 4, 128), f)
    for cc in range(4):
        for pp in range(128):
            n = 128 * cc + pp
            if n >= 511:
                continue
            a0, a1 = 16 * n, 16 * n + 32
            for b_ in range(a0 // 64, (a1 - 1) // 64 + 1):
                ovl = min(a1, 64 * b_ + 64) - max(a0, 64 * b_)
                ov[pp, cc, b_] = ovl / 32.0
    c["c_ov"] = ov.reshape(128, -1)
    e_all = np.zeros((128, S_LEN), f)
    for b_ in range(128):
        e_all[b_, 64 * b_:64 * b_ + 64] = 1.0
    c["c_eall"] = e_all
    c["c_onerow"] = np.ones((1, S_LEN), f)
    qaug = np.zeros((8, 2, 2048), f)
    qq = np.arange(2048) % 512
    for h in range(8):
        qaug[h, 0] = -slopes[h] * (qq % 256)
        qaug[h, 1] = -slopes[h] * 256 * (qq // 256)
    c["c_qaug"] = qaug
    selg = np.zeros((64, 24, 128), f)
    for hb in range(24):
        selg[hb, hb, :] = 1.0
        selg[32 + hb, hb, :] = 1.0
    c["c_selg"] = selg.reshape(64, -1)
    return c


CONST_SHAPES = {
    "c_ident": [128, 128], "c_blk": [128, 128], "c_utri": [128, 128], "c_ones": [128, 128],
    "c_oh": [128, 4, 64], "c_badd": [128, 4, 64], "c_am": [128, 4, 128], "c_dg": [128, 4, 512],
    "c_wm": [128, 8, 512], "c_cm": [128, 4, 4, 512], "c_kbs": [128, 2048], "c_kbw": [128, 256],
    "c_kbc": [128, 128], "c_asel": [128, 2048], "c_bsel": [128, 2048], "c_ov": [128, 512],
    "c_eall": [128, S_LEN], "c_onerow": [1, S_LEN], "c_qaug": [8, 2, 2048], "c_selg": [64, 24 * 128],
}


def build(debug=None, stop_after=99):
    nc = bass.Bass("TRN2", target_bir_lowering=False)
    S = Sched(nc)
    A = Arena(nc)
    debug = debug or set()

    def din(name, shape):
        return nc.dram_tensor(name, list(shape), F32, kind="ExternalInput").ap()

    def scratch(name, shape, dt):
        kind = "ExternalOutput" if name in debug else "Internal"
        return nc.dram_tensor(name, list(shape), dt, kind=kind).ap()

    xb = din("xb", [S_LEN, D])
    xq = din("xq", [2048, D])
    xw = din("xw", [4096, D])
    w_in = din("w_in", [D, DIN])
    C = {k: din(k, v) for k, v in CONST_SHAPES.items()}
    gmix_d = din("gmix", [128, 8])
    gffn_d = din("gffn", [128, 8])
    gk2_d = din("gk2", [128, 4])
    bfb_d = din("bfb", [128, 8])
    nbfc_d = din("nbfc", [128, 1])
    w1k_d = din("cmp_k_w1", [2048, 256])
    w1v_d = din("cmp_v_w1", [2048, 256])
    w2k_d = din("cmp_k_w2", [256, 64])
    w2v_d = din("cmp_v_w2", [256, 64])
    posk_d = din("poskT", [128, 32])
    posv_d = din("posvT", [128, 32])
    wfu_d = din("w_fox_up", [512, D])
    wnu_d = din("w_nsa_up", [512, D])
    wout_d = din("w_out", [D, D])
    wr_d = din("w_rt", [D, 20])
    br_d = din("b_rt", [128, 20])
    wg_d = din("w_gate", [16, D, 512])
    wu_d = din("w_up", [16, D, 512])
    wd_d = din("w_down", [16, 512, D])
    out_d = nc.dram_tensor("out", [2048, D], F32, kind="ExternalOutput").ap()

    KT_fox = scratch("KT_fox", [8, 64, S_LEN], BF16)
    V_fox = scratch("V_fox", [8, 128, NT, 128], BF16)
    KT_s = scratch("KT_s", [2, 64, S_LEN], BF16)
    KT_w = scratch("KT_w", [2, 64, 4096], BF16)
    V_s = scratch("V_s", [2, 128, NT, 128], BF16)
    V_w = scratch("V_w", [2, 128, 32, 128], BF16)
    OA_scr = scratch("OA_scr", [4, 128, 2048], BF16)
    OB_scr = scratch("OB_scr", [4, 128, 2048], BF16)
    dbgA = scratch("dbgA", [128, 4096], F32)
    dbgB = scratch("dbgB", [128, 4096], F32)
    dbgB_big = scratch("dbgB_big", [128, 8192], F32)

    ps = [nc.alloc_psum_tensor(f"ps{i}", [128, 512], F32).ap() for i in range(8)]
    ps7b = ps[7].bitcast(BF16)

    def dma(eng, out, in_, r=(), w=()):
        return S.op(eng, lambda e: e.dma_start(out=out, in_=in_), r=r, w=w, dma=True)

    def mm(out, lhsT, rhs, start, stop, r=(), w=()):
        return S.op("pe", lambda e: e.matmul(out, lhsT=lhsT, rhs=rhs, start=start, stop=stop,
                                             skip_group_check=True), r=r, w=w)

    def tr(out, in_, ident, r=(), w=()):
        return S.op("pe", lambda e: e.transpose(out=out, in_=in_, identity=ident), r=r, w=w)

    def act(out, in_, func, r=(), w=(), bias=None, scale=1.0, accum_out=None):
        def f(e):
            kw = {}
            if bias is not None:
                kw["bias"] = bias
            if accum_out is not None:
                kw["accum_out"] = accum_out
            return e.activation(out=out, in_=in_, func=func, scale=scale, **kw)
        return S.op("act", f, r=r, w=w)

    def ts(eng, out, in0, s1, s2, op0, op1=None, r=(), w=()):
        def f(e):
            if op1 is None:
                return e.tensor_scalar(out=out, in0=in0, scalar1=s1, scalar2=None, op0=op0)
            return e.tensor_scalar(out=out, in0=in0, scalar1=s1, scalar2=s2, op0=op0, op1=op1)
        return S.op(eng, f, r=r, w=w)

    def tt(eng, out, in0, in1, op, r=(), w=()):
        return S.op(eng, lambda e: e.tensor_tensor(out=out, in0=in0, in1=in1, op=op), r=r, w=w)

    def stt(out, in0, scalar, in1, op0, op1, r=(), w=()):
        return S.op("dve", lambda e: e.scalar_tensor_tensor(out=out, in0=in0, scalar=scalar, in1=in1,
                                                            op0=op0, op1=op1), r=r, w=w)

    def cp(eng, out, in_, r=(), w=()):
        if eng == "act":
            return act(out, in_, AF.Copy, r=r, w=w)
        return S.op(eng, lambda e: e.tensor_copy(out=out, in_=in_), r=r, w=w)

    def recip(out, in_, r=(), w=()):
        return S.op("dve", lambda e: e.reciprocal(out=out, in_=in_), r=r, w=w)

    def memset(eng, ap, val, w=()):
        return S.op(eng, lambda e: e.memset(ap, val), w=w)

    def finish():
        if debug:
            print("arena high water", A.hw - 18560, "bytes of", A.limit - 18560)
        S.barrier()
        S.emit()
        return nc

    ident_f = A.alloc("ident_f", [128, 128], F32)
    ident_b = A.alloc("ident_b", [128, 128], BF16)
    blk = A.alloc("blk", [128, 128], BF16)
    epsb = A.alloc("epsb", [128, 1], F32)
    lnq = A.alloc("lnq", [128, 1], F32)
    gmix = A.alloc("gmix", [128, 8], F32)
    gk2 = A.alloc("gk2", [128, 4], F32)
    dma("sp", ident_f[:], C["c_ident"][:], w=["ident_f"])
    dma("pool", ident_b[:], C["c_ident"][:], w=["ident_b"])
    dma("pool", blk[:], C["c_blk"][:], w=["blk"])
    dma("sp", gmix[:], gmix_d[:], w=["gmix"])
    dma("sp", gk2[:], gk2_d[:], w=["gk2"])
    memset("pool", epsb[:], 1e-6, w=["epsb"])
    memset("pool", lnq[:], float(np.log(0.125)), w=["lnq"])

    m_base = A.mark()
    KC = [A.alloc(f"KC{g}", [128, 512], BF16) for g in range(2)]
    VC = [A.alloc(f"VC{g}", [128, 4, 128], BF16) for g in range(2)]
    BKB = A.alloc("BKB", [128, 2048], F32)

    krawb = [A.alloc(f"krawb{i}", [128, 512], BF16) for i in range(2)]
    ksq = [A.alloc(f"ksq{i}", [128, 512], BF16) for i in range(2)]
    klv = [A.alloc(f"klv{i}", [128, 512], F32) for i in range(2)]
    nrm_nbuf = [2]
    nrm_ctr = [0]

    def qknorm(pb, pr, P, N, gcol, out_ap, out_res, qscale=False, psum_idx=5, split=None):
        i2 = nrm_ctr[0] % nrm_nbuf[0]
        nrm_ctr[0] += 1
        kr, kq, kl = krawb[i2], ksq[i2], klv[i2]
        cp("act", kr[:P, :N], pb, r=[pr], w=[f"kraw{i2}"])
        tt("dve", kq[:P, :N], kr[:P, :N], kr[:P, :N], ALU.mult, r=[f"kraw{i2}"], w=[f"ksq{i2}"])
        bi = psum_idx
        pb2 = ps[bi]
        mm(pb2[:P, :N], blk[:P, :P], kq[:P, :N], True, True, r=["blk", f"ksq{i2}"], w=[f"ps{bi}"])
        act(kl[:P, :N], pb2[:P, :N], AF.Ln, r=[f"ps{bi}", "epsb"], w=[f"klv{i2}"], bias=epsb[:P, :], scale=1.0 / 64)
        if qscale:
            act(kl[:P, :N], kl[:P, :N], AF.Exp, r=[f"klv{i2}", "lnq"], w=[f"klv{i2}"], scale=-0.5, bias=lnq[:P, :])
        else:
            act(kl[:P, :N], kl[:P, :N], AF.Exp, r=[f"klv{i2}"], w=[f"klv{i2}"], scale=-0.5)
        if split is not None:
            (oa, ra, ob, rb) = split
            stt(oa, kr[0:64, :N], gcol[0:64, :], kl[0:64, :N], ALU.mult, ALU.mult, r=[f"kraw{i2}", f"klv{i2}", "gk2"], w=[ra])
            stt(ob, kr[64:128, :N], gcol[64:128, :], kl[64:128, :N], ALU.mult, ALU.mult, r=[f"kraw{i2}", f"klv{i2}", "gk2"], w=[rb])
            return
        stt(out_ap, kr[:P, :N], gcol, kl[:P, :N], ALU.mult, ALU.mult, r=[f"kraw{i2}", f"klv{i2}", "gk2"], w=[out_res])

    def qknormA(pb, pr, ssbank):
        i2 = nrm_ctr[0] % nrm_nbuf[0]
        nrm_ctr[0] += 1
        kr, kq = krawb[i2], ksq[i2]
        cp("act", kr[:, :], pb, r=[pr], w=[f"kraw{i2}"])
        tt("dve", kq[:, :], kr[:, :], kr[:, :], ALU.mult, r=[f"kraw{i2}"], w=[f"ksq{i2}"])
        mm(ps[ssbank][:, :], blk[:, :], kq[:, :], True, True, r=["blk", f"ksq{i2}"], w=[f"ps{ssbank}"])
        return i2, ssbank

    def qknormB(st, gcol, out_ap, out_res):
        i2, ssbank = st
        kr, kl = krawb[i2], klv[i2]
        act(kl[:, :], ps[ssbank][:, :], AF.Ln, r=[f"ps{ssbank}", "epsb"], w=[f"klv{i2}"], bias=epsb[:, :], scale=1.0 / 64)
        act(kl[:, :], kl[:, :], AF.Exp, r=[f"klv{i2}"], w=[f"klv{i2}"], scale=-0.5)
        stt(out_ap, kr[:, :], gcol, kl[:, :], ALU.mult, ALU.mult, r=[f"kraw{i2}", f"klv{i2}", "gk2"], w=[out_res])

    def norm_scale(xt, rx, ss, rs, rss):
        for sub in range(4):
            act(junk[:], xt[:, sub, :], AF.Square, r=[rx], w=["junk", rss], accum_out=ss[:, sub:sub + 1])
        act(rs[:], ss[:], AF.Ln, r=[rss, "epsb"], w=[rss + "r"], bias=epsb[:, :], scale=1.0 / D)
        act(rs[:], rs[:], AF.Exp, r=[rss + "r"], w=[rss + "r"], scale=-0.5)
        for sub in range(4):
            ts("dve", xt[:, sub, :], xt[:, sub, :], rs[:, sub:sub + 1], None, ALU.mult, r=[rx, rss + "r"], w=[rx])

    def transpose_gain(xt, rx, hT_dst, rh, gvec, gres):
        for kc in range(8):
            pb = ps[kc % 2]
            pr = f"ps{kc % 2}"
            for sub in range(4):
                tr(pb[:, sub * 128:(sub + 1) * 128], xt[:, sub, kc * 128:(kc + 1) * 128], ident_f[:],
                   r=[rx, "ident_f"], w=[pr])
            if kc % 2 == 0:
                S.op("act", lambda e, o=hT_dst[:, kc, :], i=pb[:], s=gvec[:, kc:kc + 1]:
                     e.activation(out=o, in_=i, func=AF.Copy, scale=s), r=[pr, gres], w=[rh])
            else:
                ts("dve", hT_dst[:, kc, :], pb[:], gvec[:, kc:kc + 1], None, ALU.mult, r=[pr, gres], w=[rh])

    def norm_transpose(src_rows, xt, rx, hT_dst, rh, ss, rs, rss, gvec, gres, evac_ctr, load=True):
        if load:
            dma("sp", xt[:], src_rows.rearrange("(s p) d -> p s d", p=128), w=[rx])
        norm_scale(xt, rx, ss, rs, rss)
        transpose_gain(xt, rx, hT_dst, rh, gvec, gres)

    m1 = A.mark()
    LF = A.alloc("LF", [128, NT * 8], F32)
    krawb.append(A.alloc("krawb2", [128, 512], BF16))
    ksq.append(A.alloc("ksq2", [128, 512], BF16))
    klv.append(A.alloc("klv2", [128, 512], F32))
    nrm_nbuf[0] = 3
    TCk = A.alloc("TCk", [128, S_LEN], BF16)
    TCv = A.alloc("TCv", [128, S_LEN], BF16)
    WK = A.alloc("WK", [128, 8, 1800], BF16)
    w_in_v = w_in.rearrange("(kc p) n -> p kc n", p=128)
    wk_cols = [(512, 1024, 0), (2312, 2440, 512), (2568, 2696, 640), (2056, 2184, 768), (2184, 2312, 896),
               (1024, 1536, 1024), (2440, 2568, 1536), (2696, 2824, 1664), (1536, 1544, 1792)]
    for (c0, c1, d0) in wk_cols:
        dma("pool", WK[:, :, d0:d0 + (c1 - c0)], w_in_v[:, :, c0:c1], w=["WK"])
    bfb = A.alloc("bfb", [128, 8], F32)
    dma("sp", bfb[:], bfb_d[:], w=["bfb"])
    xbuf = [A.alloc(f"xbuf{i}", [128, 4, D], F32) for i in range(3)]
    hbuf = [A.alloc(f"hbuf{i}", [128, 8, 512], BF16) for i in range(2)]
    junk = A.alloc("junk", [128, D], BF16)
    ssb = [A.alloc(f"ss{i}", [128, 4], F32) for i in range(3)]
    rsb = [A.alloc(f"rs{i}", [128, 4], F32) for i in range(3)]
    kn = [A.alloc(f"kn{i}", [128, 512], BF16) for i in range(3)]
    VA = [A.alloc(f"VA{i}", [128, 8, 128], BF16) for i in range(2)]
    VAn = [A.alloc(f"VAn{i}", [128, 2, 128], BF16) for i in range(2)]
    zt = A.alloc("zt", [128, 32], F32)
    for i in range(2):
        memset("pool", VA[i][:, :, 64:128], 1.0, w=[f"VA{i}"])
        memset("pool", VAn[i][:, :, 64:128], 1.0, w=[f"VAn{i}"])
    utri = A.alloc("utri", [128, 128], F32)
    onesm = A.alloc("onesm", [128, 128], F32)
    oh = A.alloc("oh", [128, 4, 64], F32)
    badd = A.alloc("badd", [128, 4, 64], F32)
    dma("sp", utri[:], C["c_utri"][:], w=["utri"])
    dma("sp", onesm[:], C["c_ones"][:], w=["onesm"])
    dma("sp", oh[:], C["c_oh"][:], w=["oh"])
    dma("sp", badd[:], C["c_badd"][:], w=["badd"])
    CKW = A.alloc("CKW", [128, 512], F32)
    TOT = A.alloc("TOT", [128, 512], F32)
    INCL = A.alloc("INCL", [128, 512], F32)
    RS = A.alloc("RS", [128, 32], F32)
    prod = A.alloc("prod", [128, 512], F32)
    W1 = A.alloc("W1", [128, 32, 256], BF16)
    W2 = A.alloc("W2", [128, 2, 64], BF16)
    posT = A.alloc("posT", [128, 32], BF16)
    posb = A.alloc("posb", [128, 2], F32)
    Ucm = A.alloc("Ucm", [128, 512], F32)
    Tcm = A.alloc("Tcm", [128, 512], F32)
    HD = [A.alloc(f"HD{i}", [128, 512], BF16) for i in range(2)]
    for g in range(2):
        memset("pool", KC[g][:], 0.0, w=[f"KC{g}"])
        dma("pool", KC[g][64:65, :], C["c_onerow"][:, 0:512], w=[f"KC{g}"])
        dma("pool", KC[g][96:97, :], C["c_onerow"][:, 0:512], w=[f"KC{g}"])
        memset("pool", VC[g][:], 0.0, w=[f"VC{g}"])
        memset("pool", VC[g][:, :, 64:128], 1.0, w=[f"VC{g}"])
    for i in range(2):
        memset("pool", HD[i][:], 0.0, w=[f"HD{i}"])

    KTf_rows = KT_fox.rearrange("h p t -> (h p) t")
    KTs_rows = KT_s.rearrange("g p t -> (g p) t")
    KTw_rows = KT_w.rearrange("g p t -> (g p) t")
    Vf_v = V_fox.rearrange("h p t c -> p h t c")
    Vs_v = V_s.rearrange("g p t c -> p g t c")
    Vw_v = V_w.rearrange("g p t c -> p g t c")

    npair = [0]
    nss = [0]
    groups = [(xb[G * 512:(G + 1) * 512, :], G, False) for G in range(16)] + \
             [(xw[G * 512:(G + 1) * 512, :], G, True) for G in range(8)]

    def g_load(k):
        src, G, window = groups[k]
        xi = k % 3
        dma("sp", xbuf[xi][:], src.rearrange("(s p) d -> p s d", p=128), w=[f"xbuf{xi}"])

    def g_stageA(k_tr, k_sq):
        if k_tr is not None:
            xi, gi = k_tr % 3, k_tr % 2
            xt, rx, hT, rh = xbuf[xi], f"xbuf{xi}", hbuf[gi], f"hbuf{gi}"
        if k_sq is not None:
            xs = k_sq % 3
            xt2, rx2, ss2, rs2, rss2 = xbuf[xs], f"xbuf{xs}", ssb[xs], rsb[xs], f"ss{xs}"
        for kc in range(8):
            if k_tr is not None:
                pb = ps[kc % 2]
                pr = f"ps{kc % 2}"
                for sub in range(4):
                    tr(pb[:, sub * 128:(sub + 1) * 128], xt[:, sub, kc * 128:(kc + 1) * 128], ident_f[:],
                       r=[rx, "ident_f"], w=[pr])
                if kc % 2 == 0:
                    S.op("act", lambda e, o=hT[:, kc, :], i=pb[:], s_=gmix[:, kc:kc + 1]:
                         e.activation(out=o, in_=i, func=AF.Copy, scale=s_), r=[pr, "gmix"], w=[rh])
                else:
                    ts("dve", hT[:, kc, :], pb[:], gmix[:, kc:kc + 1], None, ALU.mult, r=[pr, "gmix"], w=[rh])
            if k_sq is not None and kc % 2 == 1:
                sub = kc // 2
                act(junk[:], xt2[:, sub, :], AF.Square, r=[rx2], w=["junk", rss2], accum_out=ss2[:, sub:sub + 1])
        if k_sq is not None:
            act(rs2[:], ss2[:], AF.Ln, r=[rss2, "epsb"], w=[rss2 + "r"], bias=epsb[:, :], scale=1.0 / D)
            act(rs2[:], rs2[:], AF.Exp, r=[rss2 + "r"], w=[rss2 + "r"], scale=-0.5)
            for sub in range(4):
                ts("dve", xt2[:, sub, :], xt2[:, sub, :], rs2[:, sub:sub + 1], None, ALU.mult, r=[rx2, rss2 + "r"], w=[rx2])

    def g_stageB(k):
        src, G, window = groups[k]
        gi = k % 2
        hT, rh = hbuf[gi], f"hbuf{gi}"
        pairs = [5] if window else [0, 1, 2, 3, 4, 6, 7]

        def proj(pi):
            c0 = pi * 128
            bi = 2 + npair[0] % 3
            npair[0] += 1
            pb, pr = ps[bi], f"ps{bi}"
            for kc in range(8):
                mm(pb[:], WK[:, kc, c0:c0 + 128], hT[:, kc, :], kc == 0, kc == 7, r=["WK", rh], w=[pr])
            return pb, pr

        def chainA(pi, pb, pr):
            if pi >= 6:
                dst = TCk if pi == 6 else TCv
                cp("act", dst[:, G * 512:(G + 1) * 512], pb[:], r=[pr], w=["TC"])
                return None
            ssbank = 5 if nss[0] % 2 == 0 else 7
            nss[0] += 1
            return qknormA(pb[:], pr, ssbank)

        def chainB(pi, st):
            if st is None:
                return
            kno = kn[npair[0] % 3]
            kres = f"kn{npair[0] % 3}"
            npair[0] += 0
            kno = kn[st[0]]
            kres = f"kn{st[0]}"
            gcol = gk2[:, 0:1] if pi < 4 else gk2[:, 1:2]
            qknormB(st, gcol, kno[:], kres)
            if pi < 4:
                dst = KTf_rows[pi * 128:(pi + 1) * 128, G * 512:(G + 1) * 512]
            elif pi == 4:
                dst = KTs_rows[:, G * 512:(G + 1) * 512]
            else:
                dst = KTw_rows[:, G * 512:(G + 1) * 512]
            dma("pool", dst, kno[:], r=[kres], w=["KT_scr"])

        def vgroup(sub, fox):
            tile = G * 4 + sub
            bi = 6
            pb, pr = ps[bi], f"ps{bi}"
            if fox:
                for kc in range(8):
                    mm(pb[:], hT[:, kc, sub * 128:(sub + 1) * 128], WK[:, kc, 1024:1536], kc == 0, kc == 7,
                       r=["WK", rh], w=[pr])
                va = VA[tile % 2]
                cp("dve" if sub % 2 == 0 else "act", va[:, :, 0:64], pb[:].rearrange("p (h c) -> p h c", c=64),
                   r=[pr], w=[f"VA{tile % 2}"])
                dma("pool", Vf_v[:, :, tile, :], va[:], r=[f"VA{tile % 2}"], w=["V_scr"])
                return
            c0, c1 = (1664, 1792) if window else (1536, 1664)
            for kc in range(8):
                mm(pb[:, 0:128], hT[:, kc, sub * 128:(sub + 1) * 128], WK[:, kc, c0:c1], kc == 0, kc == 7,
                   r=["WK", rh], w=[pr])
            va = VAn[tile % 2]
            cp("act" if sub % 2 == 0 else "dve", va[:, 0:2, 0:64], pb[:, 0:128].rearrange("p (h c) -> p h c", c=64),
               r=[pr], w=[f"VAn{tile % 2}"])
            dma("pool", (Vw_v if window else Vs_v)[:, :, tile, :], va[:, 0:2, :], r=[f"VAn{tile % 2}"], w=["V_scr"])

        vlist = [(sub, False) for sub in range(4)] if window else \
                [(sub, fox) for sub in range(4) for fox in (True, False)]
        npairs = len(pairs)
        pj = {0: proj(pairs[0])}
        st = {0: chainA(pairs[0], *pj[0])}
        if npairs > 1:
            pj[1] = proj(pairs[1])
        for n in range(npairs):
            if n + 2 < npairs:
                pj[n + 2] = proj(pairs[n + 2])
            if n + 1 < npairs:
                st[n + 1] = chainA(pairs[n + 1], *pj[n + 1])
            if vlist:
                vgroup(*vlist.pop(0))
            chainB(pairs[n], st[n])
        while vlist:
            vgroup(*vlist.pop(0))
        if window:
            return
        pb = ps[6]
        for sub in range(4):
            for kc in range(8):
                mm(pb[:, sub * 8:(sub + 1) * 8], hT[:, kc, sub * 128:(sub + 1) * 128], WK[:, kc, 1792:1800],
                   kc == 0, kc == 7, r=["WK", rh], w=["ps6"])
        tt("dve", zt[:].rearrange("p (s h) -> p s h", h=8), pb[:, 0:32].rearrange("p (s h) -> p s h", h=8),
           bfb[:].unsqueeze(1).to_broadcast([128, 4, 8]), ALU.add, r=["ps6", "bfb"], w=["zt"])
        act(zt[:], zt[:], AF.Exp, r=["zt"], w=["zt"], scale=-1.0)
        ts("dve", zt[:], zt[:], 1.0, None, ALU.add, r=["zt"], w=["zt"])
        act(zt[:], zt[:], AF.Ln, r=["zt"], w=["zt"])
        ts("dve", LF[:, G * 32:(G + 1) * 32], zt[:], -1.0, None, ALU.mult, r=["zt"], w=["LF"])

    TOT3 = TOT[:].rearrange("p (t h) -> p t h", h=8)
    INCL3 = INCL[:].rearrange("p (t h) -> p t h", h=8)
    CKW3 = CKW[:].rearrange("p (t h) -> p t h", h=8)

    def piece_cumsum():
        mm(ps[0][:], utri[:], LF[:], True, True, r=["utri", "LF"], w=["ps0"])
        mm(ps[1][:], onesm[:], LF[:], True, True, r=["onesm", "LF"], w=["ps1"])
        cp("act", CKW[:], ps[0][:], r=["ps0"], w=["CKW"])
        cp("dve", TOT[:], ps[1][:], r=["ps1"], w=["TOT"])
        for h in range(8):
            S.op("dve", lambda e, h=h: e.tensor_tensor_scan(out=INCL3[:, :, h], data0=onesm[:, 0:64], data1=TOT3[:, :, h],
                                                            initial=0.0, op0=ALU.mult, op1=ALU.add),
                 r=["TOT", "onesm"], w=["INCL"])
        tt("dve", INCL[:], INCL[:], TOT[:], ALU.subtract, r=["INCL", "TOT"], w=["INCL"])
        tt("dve", CKW[:], CKW[:], INCL[:], ALU.add, r=["CKW", "INCL"], w=["CKW"])
        for s in range(4):
            tt("dve", prod[:].rearrange("p (t h) -> p t h", h=8), INCL3,
               oh[:, s, :].unsqueeze(2).to_broadcast([128, 64, 8]), ALU.mult, r=["INCL", "oh"], w=["prod"])
            S.op("dve", lambda e, s=s: e.tensor_reduce(out=RS[:, s * 8:(s + 1) * 8],
                                                       in_=prod[:].rearrange("p (t h) -> p h t", h=8),
                                                       axis=AX.X, op=ALU.add), r=["prod"], w=["RS"])
            for h in range(8):
                o = BKB[:, (s * 8 + h) * 64:(s * 8 + h + 1) * 64]
                stt(o, CKW3[:, :, h], -1.0, badd[:, s, :], ALU.mult, ALU.add, r=["CKW", "badd"], w=["BKB"])
                ts("dve", o, o, RS[:, s * 8 + h:s * 8 + h + 1], None, ALU.add, r=["BKB", "RS"], w=["BKB"])

    GC = 1.5957691216057308

    def piece_w(kv):
        w1d = w1k_d if kv == 0 else w1v_d
        w2d = w2k_d if kv == 0 else w2v_d
        w1v = w1d.rearrange("(j d) n -> d j n", d=64)
        for half in range(2):
            for jq in range(4):
                dma("pool", W1[64 * half:64 * half + 64, jq * 8:(jq + 1) * 8, :], w1v[:, jq * 8:(jq + 1) * 8, :], w=["W1"])
        dma("pool", W2[:], w2d.rearrange("(hc p) n -> p hc n", p=128), w=["W2"])
        dma("pool", posT[:], (posk_d if kv == 0 else posv_d)[:], w=["posT"])
        for hc in range(2):
            for jj in range(32):
                mm(ps[0][:, hc:hc + 1], W1[0:64, jj, hc * 128:(hc + 1) * 128], posT[0:64, jj:jj + 1], jj == 0, jj == 31,
                   r=["W1", "posT"], w=["ps0"])
        cp("dve", posb[:], ps[0][:, 0:2], r=["ps0"], w=["posb"])

    def piece_mlp(kv, g, hc):
        TC = TCk if kv == 0 else TCv
        TC3 = TC[:].rearrange("p (n s) -> p n s", s=16)
        b0 = 64 * g
        pb, pr = ps[1], "ps1"
        for jj in range(32):
            rhs = TC3[b0:b0 + 64, 0:511, jj] if jj < 16 else TC3[b0:b0 + 64, 1:512, jj - 16]
            mm(pb[:, 0:511], W1[b0:b0 + 64, jj, hc * 128:(hc + 1) * 128], rhs, jj == 0, jj == 31, r=["W1", "TC"], w=[pr])
        act(Ucm[:, 0:511], pb[:, 0:511], AF.Identity, r=[pr, "posb"], w=["Ucm"], bias=posb[:, hc:hc + 1])
        tt("dve", Tcm[:, 0:511], Ucm[:, 0:511], Ucm[:, 0:511], ALU.mult, r=["Ucm"], w=["Tcm"])
        ts("dve", Tcm[:, 0:511], Tcm[:, 0:511], 0.044715, 1.0, ALU.mult, ALU.add, r=["Tcm"], w=["Tcm"])
        tt("dve", Tcm[:, 0:511], Tcm[:, 0:511], Ucm[:, 0:511], ALU.mult, r=["Tcm", "Ucm"], w=["Tcm"])
        act(Tcm[:, 0:511], Tcm[:, 0:511], AF.Exp, r=["Tcm"], w=["Tcm"], scale=-GC)
        ts("dve", Tcm[:, 0:511], Tcm[:, 0:511], 1.0, None, ALU.add, r=["Tcm"], w=["Tcm"])
        recip(Tcm[:, 0:511], Tcm[:, 0:511], r=["Tcm"], w=["Tcm"])
        tt("dve", HD[hc][:, 0:511], Ucm[:, 0:511], Tcm[:, 0:511], ALU.mult, r=["Tcm", "Ucm"], w=[f"HD{hc}"])
        if hc == 0:
            return
        if kv == 0:
            for h2 in range(2):
                mm(ps[0][0:64, 0:511], W2[:, h2, :], HD[h2][:, 0:511], h2 == 0, h2 == 1, r=["W2", f"HD{h2}"], w=["ps0"])
            qknorm(ps[0][0:64, 0:511], "ps0", 64, 511, gk2[0:64, 1:2], KC[g][0:64, 0:511], f"KC{g}")
        else:
            for cc in range(4):
                for h2 in range(2):
                    mm(ps[0][:, cc * 64:(cc + 1) * 64], HD[h2][:, cc * 128:(cc + 1) * 128], W2[:, h2, :], h2 == 0, h2 == 1,
                       r=["W2", f"HD{h2}"], w=["ps0"])
            cp("dve", VC[g][:, :, 0:64], ps[0][:, 0:256].rearrange("p (c d) -> p c d", d=64), r=["ps0"], w=[f"VC{g}"])

    pieces = [piece_cumsum]
    for kv in range(2):
        pieces.append(lambda kv=kv: piece_w(kv))
        for g in range(2):
            for hc in range(2):
                pieces.append(lambda kv=kv, g=g, hc=hc: piece_mlp(kv, g, hc))

    NG = len(groups)
    g_load(0)
    g_load(1)
    g_load(2)
    g_stageA(None, 0)
    g_stageA(0, 1)
    for k in range(NG):
        if k + 1 < NG:
            g_stageA(k + 1, k + 2 if k + 2 < NG else None)
        g_stageB(k)
        if k + 3 < NG:
            g_load(k + 3)
        if k >= 15 and pieces and stop_after >= 2:
            nrun = 2 if k in (15, 18, 21) else 1
            for _ in range(nrun):
                if pieces:
                    pieces.pop(0)()
    if stop_after >= 2:
        while pieces:
            pieces.pop(0)()
    if "dbgA" in debug and stop_after == 2:
        dma("sp", dbgA[:, 0:2048], BKB[:], r=["BKB"], w=["dbgA"])
        dma("sp", dbgA[:, 2048:2560], CKW[:], r=["CKW"], w=["dbgA"])
    if "dbgB" in debug and stop_after == 2:
        for g in range(2):
            cp("dve", Ucm[:, :], KC[g][:, :], r=[f"KC{g}"], w=["Ucm"])
            dma("sp", dbgB[:, g * 512:(g + 1) * 512], Ucm[:], r=["Ucm"], w=["dbgB"])
            cp("dve", Tcm[:, :], VC[g][:].rearrange("p c d -> p (c d)"), r=[f"VC{g}"], w=["Tcm"])
            dma("sp", dbgB[:, 1024 + g * 512:1024 + (g + 1) * 512], Tcm[:], r=["Tcm"], w=["dbgB"])
    S.barrier()
    A.release(m1)
    nrm_nbuf[0] = 2
    if stop_after <= 2:
        return finish()

    HQ = A.alloc("HQ", [128, 8, 2048], BF16)
    WQn = A.alloc("WQn", [128, 8, 536], BF16)
    dma("pool", WQn[:, :, 0:512], w_in_v[:, :, 1544:2056], w=["WQn"])
    dma("pool", WQn[:, :, 512:536], w_in_v[:, :, 2824:2848], w=["WQn"])
    SGHL = A.alloc("SGHL", [64, 2048], BF16)
    memset("pool", SGHL[:], 0.0, w=["SGHL"])
    QT = [A.alloc(f"QT{i}", [128, 2048], BF16) for i in range(4)]
    Pb = [A.alloc(f"P{i}", [128, 512], BF16) for i in range(4)]
    AM = A.alloc("AM", [128, 4, 128], BF16)
    DG = A.alloc("DG", [128, 4, 512], BF16)
    etmp = [A.alloc(f"etmp{i}", [128, 512], F32) for i in range(2)]
    et2b = [A.alloc(f"et2_{i}", [128, 512], F32) for i in range(2)]
    dma("pool", AM[:], C["c_am"][:], w=["AM"])
    dma("pool", DG[:], C["c_dg"][:], w=["DG"])
    for i in range(4):
        memset("pool", QT[i][64:128, :], 0.0, w=[f"QT{i}"])
    mF = A.mark()
    AQH = A.alloc("AQH", [8, 2048], BF16)
    AQL = A.alloc("AQL", [8, 2048], BF16)
    WQf = A.alloc("WQf", [128, 8, 520], BF16)
    dma("pool", WQf[:, :, 0:512], w_in_v[:, :, 0:512], w=["WQf"])
    dma("pool", WQf[:, :, 512:520], w_in_v[:, :, 1536:1544], w=["WQf"])
    mT = A.mark()
    nbfc = A.alloc("nbfc", [128, 1], F32)
    dma("sp", nbfc[:], nbfc_d[:], w=["nbfc"])
    AQ = A.alloc("AQ", [8, 2048], F32)
    onesf = A.alloc("onesf", [128, 512], F32)
    memset("pool", onesf[:], 1.0, w=["onesf"])
    xq_t = A.alloc("xq_t", [128, 4, D], F32)
    junk = A.alloc("junk2", [128, D], BF16)
    ssq = A.alloc("ssq", [128, 4], F32)
    rsq = A.alloc("rsq", [128, 4], F32)
    zq = A.alloc("zq", [32, 512], F32)
    for s in range(4):
        norm_transpose(xq[s * 512:(s + 1) * 512, :], xq_t, "xq_t", HQ[:, :, s * 512:(s + 1) * 512], "HQ", ssq, rsq, "ssq",
                       gmix, "gmix", None)
    for s in range(4):
        sl = slice(s * 512, (s + 1) * 512)
        pb = ps[6]
        for kc in range(8):
            mm(pb[0:8, :], WQf[:, kc, 512:520], HQ[:, kc, sl], kc == 0, kc == 7, r=["WQf", "HQ"], w=["ps6"])
        act(zq[0:8, :], pb[0:8, :], AF.Exp, r=["ps6", "nbfc"], w=["zq"], scale=-1.0, bias=nbfc[0:8, :])
        ts("dve", zq[0:8, :], zq[0:8, :], 1.0, None, ALU.add, r=["zq"], w=["zq"])
        act(zq[0:8, :], zq[0:8, :], AF.Ln, r=["zq"], w=["zq"])
        S.op("dve", lambda e, sl=sl: e.tensor_tensor_scan(out=AQ[0:8, sl], data0=onesf[0:8, :], data1=zq[0:8, :],
                                                          initial=0.0, op0=ALU.mult, op1=ALU.subtract),
             r=["zq", "onesf"], w=["AQ"])
        pb = ps[7]
        for kc in range(8):
            mm(pb[0:24, :], WQn[:, kc, 512:536], HQ[:, kc, sl], kc == 0, kc == 7, r=["WQn", "HQ"], w=["ps7"])
        act(zq[0:24, :], pb[0:24, :], AF.Exp, r=["ps7"], w=["zq"], scale=-1.0)
        ts("dve", zq[0:24, :], zq[0:24, :], 1.0, None, ALU.add, r=["zq"], w=["zq"])
        recip(zq[0:24, :], zq[0:24, :], r=["zq"], w=["zq"])
        cp("dve", SGHL[0:24, sl], zq[0:24, :], r=["zq"], w=["SGHL"])
        tt("dve", SGHL[32:56, sl], zq[0:24, :], SGHL[0:24, sl], ALU.subtract, r=["zq", "SGHL"], w=["SGHL"])
    cp("dve", AQH[:], AQ[:], r=["AQ"], w=["AQH"])
    tt("dve", AQL[:], AQ[:], AQH[:], ALU.subtract, r=["AQ", "AQH"], w=["AQL"])
    S.barrier()
    A.release(mT)

    tile_ctr = [0]
    slot_ctr = [0]

    def attn(qt, qres, tiles, oi, after_exps=None):
        n = len(tiles)
        base = tile_ctr[0]
        tile_ctr[0] += n

        def qk(i):
            t = tiles[i]
            b = (base + i) % 3
            ex = t["extras"]
            mm(ps[b][:], t["k"], qt, True, len(ex) == 0, r=t["kres"] + [qres], w=[f"ps{b}"])
            for ei, (l, rr, res) in enumerate(ex):
                mm(ps[b][:], l, rr, False, ei == len(ex) - 1, r=res, w=[f"ps{b}"])

        LA = 2
        for i in range(min(LA, n)):
            qk(i)
        for i in range(n):
            if i + LA < n:
                qk(i + LA)
            t = tiles[i]
            b = (base + i) % 3
            pi = (base + i) % 4
            act(Pb[pi][:], ps[b][:], AF.Exp, r=[f"ps{b}", t["bres"]], w=[f"P{pi}"], bias=t["bias"])
            mm(ps[oi][:], t["v"], Pb[pi][:], i == 0, i == n - 1, r=[t["vres"], f"P{pi}"], w=[f"ps{oi}"])
        if after_exps is not None:
            after_exps([Pb[(base + i) % 4] for i in range(n)], [f"P{(base + i) % 4}" for i in range(n)])

    def qproj_pair(Wt, wres, c0, gcol, qa, ra, qb_, rb):
        for s in range(4):
            sl = slice(s * 512, (s + 1) * 512)
            for kc in range(8):
                mm(ps[6][:], Wt[:, kc, c0:c0 + 128], HQ[:, kc, sl], kc == 0, kc == 7, r=[wres, "HQ"], w=["ps6"])
            qknorm(ps[6][:], "ps6", 128, 512, gcol, None, None, qscale=True, psum_idx=7,
                   split=(qa[0:64, sl], ra, qb_[0:64, sl], rb))

    KB = [A.alloc(f"KB{i}", [128, S_LEN], BF16) for i in range(2)]
    VB = [A.alloc(f"VB{i}", [128, NT, 128], BF16) for i in range(2)]
    OST = [A.alloc(f"OST{i}", [128, 2048], BF16) for i in range(2)]
    for i in range(2):
        memset("pool", KB[i][64:128, :], 0.0, w=[f"KBa{i}"])
        dma("pool", KB[i][64:65, :], C["c_onerow"][:, :], r=[f"KBa{i}"], w=[f"KBa{i}"])
        dma("pool", KB[i][96:97, :], C["c_onerow"][:, :], r=[f"KBa{i}"], w=[f"KBa{i}"])
    def fox_loads(h):
        kb, vb = KB[h % 2], VB[h % 2]
        for qd in range(4):
            dma("sp", kb[0:64, qd * 2048:(qd + 1) * 2048], KT_fox[h, :, qd * 2048:(qd + 1) * 2048],
                r=["KT_scr"], w=[f"KB{h % 2}_{qd}"])
            dma("sp", vb[:, qd * 16:(qd + 1) * 16, :], V_fox[h, :, qd * 16:(qd + 1) * 16, :],
                r=["V_scr"], w=[f"VB{h % 2}_{qd}"])

    fox_loads(0)
    fox_loads(1)
    for i in range(4):
        q0i = 2 * (i % 2)
        qproj_pair(WQf, "WQf", i * 128, gk2[:, 2:3], QT[q0i], f"QT{q0i}", QT[q0i + 1], f"QT{q0i + 1}")
        for par in range(2):
            qt, qr = QT[q0i + par], f"QT{q0i + par}"
            h = 2 * i + par
            dma("pool", qt[64:65, :], AQH[h:h + 1, :], r=["AQH"], w=[qr])
            dma("pool", qt[96:97, :], AQL[h:h + 1, :], r=["AQL"], w=[qr])
            kb, vb = KB[h % 2], VB[h % 2]
            for s in range(4):
                sl = slice(s * 512, (s + 1) * 512)
                tiles = []
                for t in range(16 * s + 16):
                    ex = []
                    if t >= 16 * s:
                        m_, tl = (t - 16 * s) // 4, (t - 16 * s) % 4
                        ex = [(AM[:, m_, :], DG[:, tl, :], ["AM", "DG"])]
                    tiles.append(dict(k=kb[0:97, t * 128:(t + 1) * 128], kres=[f"KB{h % 2}_{t // 16}", f"KBa{h % 2}"], extras=ex,
                                      bias=BKB[:, (s * 8 + h) * 64 + t:(s * 8 + h) * 64 + t + 1], bres="BKB",
                                      v=vb[:, t, :], vres=f"VB{h % 2}_{t // 16}"))
                oi = 3 + slot_ctr[0] % 2
                et = etmp[slot_ctr[0] % 2]
                er = f"etmp{slot_ctr[0] % 2}"
                slot_ctr[0] += 1
                attn(qt[0:97, sl], qr, tiles, oi)
                recip(et[0:64, :], ps[oi][64:128, :], r=[f"ps{oi}"], w=[er])
                tt("dve", OST[i % 2][par * 64:(par + 1) * 64, sl], ps[oi][0:64, :], et[0:64, :], ALU.mult,
                   r=[f"ps{oi}", er], w=[f"OST{i % 2}"])
            if h + 2 < 8:
                fox_loads(h + 2)
        dma("pool", OA_scr[i], OST[i % 2][:], r=[f"OST{i % 2}"], w=["OA_scr"])
    S.barrier()
    A.release(mF)
    if stop_after <= 3:
        return finish()

    dma("sp", BKB[:], C["c_kbs"][:], w=["BKB"])
    OBT = A.alloc("OBT", [128, 4, 2048], BF16)
    NST = [A.alloc(f"NST{g}", [128, 4, 512], BF16) for g in range(2)]
    SELG = A.alloc("SELG", [64, 24, 128], BF16)
    OV = A.alloc("OV", [128, 4, 128], BF16)
    KBW = A.alloc("KBW", [128, 256], F32)
    KBC = A.alloc("KBC", [128, 128], F32)
    X16 = A.alloc("X16", [128, S_LEN], BF16)
    dma("pool", SELG[:], C["c_selg"][:].rearrange("p (a b) -> p a b", b=128), w=["SELG"])
    dma("pool", OV[:], C["c_ov"][:].rearrange("p (a b) -> p a b", b=128), w=["OV"])
    dma("sp", KBW[:], C["c_kbw"][:], w=["KBW"])
    dma("sp", KBC[:], C["c_kbc"][:], w=["KBC"])
    CMv = X16[:].rearrange("p (s c q) -> p s c q", s=4, c=4)
    cm_flat = C["c_cm"].rearrange("p s c q -> p (s c q)")
    for qd in range(4):
        dma("pool", X16[:, qd * 2048:(qd + 1) * 2048], cm_flat[:, qd * 2048:(qd + 1) * 2048], w=["X16"])

    def nsa_qpair(i):
        q0i = 2 * (i % 2)
        qproj_pair(WQn, "WQn", i * 128, gk2[:, 3:4], QT[q0i], f"QT{q0i}", QT[q0i + 1], f"QT{q0i + 1}")
        for par in range(2):
            h = 2 * i + par
            dma("pool", QT[q0i + par][64:65, :], C["c_qaug"][h, 0:1, :], w=[f"QT{q0i + par}"])
            dma("pool", QT[q0i + par][96:97, :], C["c_qaug"][h, 1:2, :], w=[f"QT{q0i + par}"])

    def nsa_epilogue(h, br, s, oi):
        i, par = h // 2, h % 2
        sl = slice(s * 512, (s + 1) * 512)
        et = etmp[slot_ctr[0] % 2]
        er = f"etmp{slot_ctr[0] % 2}"
        et2 = et2b[slot_ctr[0] % 2]
        e2r = f"et2_{slot_ctr[0] % 2}"
        lo, hi = par * 64, par * 64 + 64
        ts("dve", et[0:64, :], ps[oi][64:128, :], 1e-30, None, ALU.max, r=[f"ps{oi}"], w=[er])
        recip(et[0:64, :], et[0:64, :], r=[er], w=[er])
        tt("dve", et2[lo:hi, :], ps[oi][0:64, :], et[0:64, :], ALU.mult, r=[f"ps{oi}", er], w=[e2r])
        mm(ps[5][:], SELG[:, h * 3 + br, :], SGHL[0:64, sl], True, True, r=["SELG", "SGHL"], w=["ps5"])
        if br == 0:
            tt("dve", OBT[lo:hi, i, sl], et2[lo:hi, :], ps[5][lo:hi, :], ALU.mult, r=[e2r, "ps5"], w=["OBT"])
        else:
            tt("dve", et2[lo:hi, :], et2[lo:hi, :], ps[5][lo:hi, :], ALU.mult, r=[e2r, "ps5"], w=[e2r])
            tt("pool", OBT[lo:hi, i, sl], OBT[lo:hi, i, sl], et2[lo:hi, :], ALU.add, r=[e2r, "OBT"], w=["OBT"])

    mC = A.mark()
    IMP = A.alloc("IMP", [128, 4, 512], F32)
    ASEL = A.alloc("ASEL", [128, 2048], BF16)
    BSEL = A.alloc("BSEL", [128, 2048], BF16)
    SC = A.alloc("SC", [128, 512], F32)
    SC2 = A.alloc("SC2", [128, 128], F32)
    tmpU = A.alloc("tmpU", [128, 512], F32)
    l4 = A.alloc("l4", [128, 4], F32)
    m8 = A.alloc("m8", [128, 16], F32)
    nsel = [A.alloc(f"nsel{i}", [128, 128], BF16) for i in range(2)]
    dma("pool", ASEL[:], C["c_asel"][:], w=["ASEL"])
    dma("pool", BSEL[:], C["c_bsel"][:], w=["BSEL"])
    for i in range(4):
        g = i // 2
        nsa_qpair(i)
        for par in range(2):
            h = 2 * i + par
            first_of_g = (h % 4 == 0)
            qt, qr = QT[2 * (i % 2) + par], f"QT{2 * (i % 2) + par}"
            for s in range(4):
                sl = slice(s * 512, (s + 1) * 512)
                tiles = [dict(k=KC[g][0:97, c * 128:(c + 1) * 128], kres=[f"KC{g}"],
                              extras=[(ident_b[:], CMv[:, s, c, :], ["ident_b", "X16"])],
                              bias=KBC[:, (h * 4 + s) * 4 + c:(h * 4 + s) * 4 + c + 1], bres="KBC",
                              v=VC[g][:, c, :], vres=f"VC{g}") for c in range(4)]
                oi = 3 + slot_ctr[0] % 2

                def imp_fn(Ps, Pres, s=s, first_of_g=first_of_g):
                    for sub in range(4):
                        for c in range(4):
                            mm(ps[6][:, sub * 128:(sub + 1) * 128], Ps[c][:, sub * 128:(sub + 1) * 128], OV[:, c, :],
                               c == 0, c == 3, r=[Pres[c], "OV"], w=["ps6"])
                    U3 = ps[6][:].rearrange("p (a j) -> p a j", j=128)
                    S.op("dve", lambda e: e.tensor_reduce(out=l4[:], in_=U3, axis=AX.X, op=ALU.add), r=["ps6"], w=["l4"])
                    ts("dve", l4[:], l4[:], 1e-30, None, ALU.max, r=["l4"], w=["l4"])
                    recip(l4[:], l4[:], r=["l4"], w=["l4"])
                    rlb = l4[:].unsqueeze(2).to_broadcast([128, 4, 128])
                    I3 = IMP[:, s, :].rearrange("p (a j) -> p a j", j=128)
                    if first_of_g:
                        tt("dve", I3, U3, rlb, ALU.mult, r=["ps6", "l4"], w=["IMP"])
                    else:
                        tt("dve", tmpU[:].rearrange("p (a j) -> p a j", j=128), U3, rlb, ALU.mult, r=["ps6", "l4"], w=["tmpU"])
                        tt("pool", IMP[:, s, :], IMP[:, s, :], tmpU[:], ALU.add, r=["tmpU", "IMP"], w=["IMP"])

                attn(qt[0:97, sl], qr, tiles, oi, after_exps=imp_fn)
                nsa_epilogue(h, 0, s, oi)
                slot_ctr[0] += 1
        if i % 2 == 1:
            for s in range(4):
                tt("dve", SC[:], IMP[:, s, :], ASEL[:, s * 512:(s + 1) * 512], ALU.mult, r=["IMP", "ASEL"], w=["SC"])
                tt("dve", SC[:], SC[:], BSEL[:, s * 512:(s + 1) * 512], ALU.add, r=["SC", "BSEL"], w=["SC"])
                for sub in range(4):
                    scs = SC[:, sub * 128:(sub + 1) * 128]
                    ns = nsel[sub % 2]
                    S.op("dve", lambda e, scs=scs: e.max(out=m8[:, 0:8], in_=scs), r=["SC"], w=["m8"])
                    S.op("dve", lambda e, scs=scs: e.match_replace(out=SC2[:], in_to_replace=m8[:, 0:8], in_values=scs,
                                                                  imm_value=-1e9), r=["SC", "m8"], w=["SC2"])
                    S.op("dve", lambda e: e.max(out=m8[:, 8:16], in_=SC2[:]), r=["SC2"], w=["m8"])
                    ts("dve", ns[:], scs, m8[:, 15:16], NEGM, ALU.is_lt, ALU.mult, r=["SC", "m8"], w=[f"nsel{sub % 2}"])
                    tr(ps7b[:, sub * 128:(sub + 1) * 128], ns[:], ident_b[:], r=[f"nsel{sub % 2}", "ident_b"], w=["ps7"])
                cp("act", NST[g][:, s, :], ps7b[:, 0:512], r=["ps7"], w=[f"NST{g}"])
    if "dbgA" in debug and stop_after == 4:
        for g in range(2):
            for s in range(4):
                cp("dve", SC[:], NST[g][:, s, :], r=[f"NST{g}"], w=["SC"])
                dma("sp", dbgA[:, (g * 4 + s) * 512:(g * 4 + s + 1) * 512], SC[:], r=["SC"], w=["dbgA"])
    S.barrier()
    A.release(mC)
    if stop_after <= 4:
        if "dbgB" in debug:
            for i in range(4):
                cp("dve", etmp[0][:], OBT[:, i, 0:512], r=["OBT"], w=["etmp0"])
                dma("sp", dbgB[:, i * 512:(i + 1) * 512], etmp[0][:], r=["etmp0"], w=["dbgB"])
        return finish()

    for qd in range(4):
        dma("pool", X16[:, qd * 2048:(qd + 1) * 2048], C["c_eall"][:, qd * 2048:(qd + 1) * 2048], w=["X16"])
    WM = A.alloc("WM", [128, 8, 512], BF16)
    dma("pool", WM[:], C["c_wm"][:], w=["WM"])
    KS = A.alloc("KS", [128, S_LEN], BF16)
    VS = A.alloc("VS", [128, NT, 128], BF16)
    KW = A.alloc("KW", [128, 4096], BF16)
    VW = A.alloc("VW", [128, 32, 128], BF16)
    for (kbuf, n, nm) in [(KS, S_LEN, "KSa"), (KW, 4096, "KWa")]:
        memset("pool", kbuf[64:128, :], 0.0, w=[nm])
        dma("pool", kbuf[64:65, :], C["c_onerow"][:, 0:n], r=[nm], w=[nm])
        dma("pool", kbuf[96:97, :], C["c_onerow"][:, 0:n], r=[nm], w=[nm])
    for g in range(2):
        for qd in range(4):
            dma("sp", KS[0:64, qd * 2048:(qd + 1) * 2048], KT_s[g, :, qd * 2048:(qd + 1) * 2048], r=["KT_scr"], w=[f"KS_{qd}"])
            dma("sp", VS[:, qd * 16:(qd + 1) * 16, :], V_s[g, :, qd * 16:(qd + 1) * 16, :], r=["V_scr"], w=[f"VS_{qd}"])
        dma("sp", KW[0:64, :], KT_w[g], r=["KT_scr"], w=["KW"])
        dma("sp", VW[:], V_w[g], r=["V_scr"], w=["VW"])
        for ip in range(2):
            i = 2 * g + ip
            nsa_qpair(i)
            for par in range(2):
                h = 2 * i + par
                qt, qr = QT[2 * (i % 2) + par], f"QT{2 * (i % 2) + par}"
                for s in range(4):
                    sl = slice(s * 512, (s + 1) * 512)
                    tiles = []
                    for t in range(16 * s + 16):
                        ex = [(X16[:, t * 128:(t + 1) * 128], NST[g][:, s, :], ["X16", f"NST{g}"])]
                        if t >= 16 * s:
                            m_, tl = (t - 16 * s) // 4, (t - 16 * s) % 4
                            ex.append((AM[:, m_, :], DG[:, tl, :], ["AM", "DG"]))
                        bi = (h * 4 + s) * 64 + t
                        tiles.append(dict(k=KS[0:97, t * 128:(t + 1) * 128], kres=[f"KS_{t // 16}", "KSa"], extras=ex,
                                          bias=BKB[:, bi:bi + 1], bres="BKB", v=VS[:, t, :], vres=f"VS_{t // 16}"))
                    oi = 3 + slot_ctr[0] % 2
                    attn(qt[0:97, sl], qr, tiles, oi)
                    nsa_epilogue(h, 1, s, oi)
                    slot_ctr[0] += 1
                for s in range(4):
                    sl = slice(s * 512, (s + 1) * 512)
                    tiles = []
                    for t in range(8):
                        bi = (h * 4 + s) * 8 + t
                        tiles.append(dict(k=KW[0:97, (8 * s + t) * 128:(8 * s + t + 1) * 128], kres=["KW", "KWa"],
                                          extras=[(ident_b[:], WM[:, t, :], ["ident_b", "WM"])],
                                          bias=KBW[:, bi:bi + 1], bres="KBW", v=VW[:, 8 * s + t, :], vres="VW"))
                    oi = 3 + slot_ctr[0] % 2
                    attn(qt[0:97, sl], qr, tiles, oi)
                    nsa_epilogue(h, 2, s, oi)
                    slot_ctr[0] += 1
    if "dbgB" in debug and stop_after == 5:
        for i in range(4):
            for s in range(4):
                cp("dve", etmp[0][:], OBT[:, i, s * 512:(s + 1) * 512], r=["OBT"], w=["etmp0"])
                dma("sp", dbgB_big[:, (i * 4 + s) * 512:(i * 4 + s + 1) * 512], etmp[0][:], r=["etmp0"], w=["dbgB_big"])
    for i in range(4):
        dma("sp", OB_scr[i], OBT[:, i, :], r=["OBT"], w=["OB_scr"])
    S.barrier()
    A.release(m_base)
    if stop_after <= 5:
        return finish()

    ACC = A.alloc("ACC", [128, 16, D], F32)
    gffn = A.alloc("gffn", [128, 8], F32)
    dma("sp", gffn[:], gffn_d[:], w=["gffn"])
    m5 = A.mark()
    WA = A.alloc("WA", [128, 4, D], BF16)
    WB = A.alloc("WB", [128, 4, D], BF16)
    WO = A.alloc("WO", [128, 8, D], BF16)
    WG = A.alloc("WG", [128, 8, 2048], BF16)
    dma("pool", WA[:], wfu_d.rearrange("(c p) n -> p c n", p=128), w=["WA"])
    dma("pool", WB[:], wnu_d.rearrange("(c p) n -> p c n", p=128), w=["WB"])
    for qd in range(4):
        dma("pool", WG[:, :, qd * 512:(qd + 1) * 512], w_in_v[:, :, 2848 + qd * 512:2848 + (qd + 1) * 512], w=[f"WG{qd}"])
    dma("pool", WO[:], wout_d.rearrange("(c p) n -> p c n", p=128), w=["WO"])
    xt3 = A.alloc("xt3", [128, 4, D], F32)
    xraw = A.alloc("xraw", [128, 4, D], F32)
    junk = A.alloc("junk3", [128, D], BF16)
    ss3 = A.alloc("ss3", [128, 4], F32)
    rs3 = A.alloc("rs3", [128, 4], F32)
    HQs = A.alloc("HQs", [128, 8, 512], BF16)
    OAs = A.alloc("OAs", [128, 4, 512], BF16)
    OBs = A.alloc("OBs", [128, 4, 512], BF16)
    MIX = A.alloc("MIX", [128, 8, 512], BF16)
    sgt = [A.alloc(f"sgt{i}", [128, 512], F32) for i in range(2)]
    mxa = A.alloc("mxa", [128, 512], F32)
    for s in range(4):
        sl = slice(s * 512, (s + 1) * 512)
        dma("sp", xraw[:], xq[sl, :].rearrange("(u p) d -> p u d", p=128), w=["xraw"])
        dma("sp", OAs[:], OA_scr.rearrange("c p t -> p c t")[:, :, sl], r=["OA_scr"], w=["OAs"])
        dma("sp", OBs[:], OB_scr.rearrange("c p t -> p c t")[:, :, sl], r=["OB_scr"], w=["OBs"])
        norm_transpose(xq[sl, :], xt3, "xt3", HQs, "HQs", ss3, rs3, "ss3", gmix, "gmix", None)
        for m in range(8):
            for ab in range(2):
                c0 = ab * 1024 + m * 128
                wgr = f"WG{c0 // 512}"
                pg, pgr = ps[2 + 2 * ab], f"ps{2 + 2 * ab}"
                po, por = ps[3 + 2 * ab], f"ps{3 + 2 * ab}"
                for kc in range(8):
                    mm(pg[:], WG[:, kc, c0:c0 + 128], HQs[:, kc, :], kc == 0, kc == 7, r=[wgr, "HQs"], w=[pgr])
                Wx, wxr, Ox, oxr = (WA, "WA", OAs, "OAs") if ab == 0 else (WB, "WB", OBs, "OBs")
                for c in range(4):
                    mm(po[:], Wx[:, c, m * 128:(m + 1) * 128], Ox[:, c, :], c == 0, c == 3, r=[wxr, oxr], w=[por])
                act(sgt[ab][:], pg[:], AF.Sigmoid, r=[pgr], w=[f"sgt{ab}"])
                if ab == 0:
                    tt("dve", mxa[:], po[:], sgt[0][:], ALU.mult, r=[por, "sgt0"], w=["mxa"])
                else:
                    tt("dve", sgt[1][:], po[:], sgt[1][:], ALU.mult, r=[por, "sgt1"], w=["sgt1"])
                    tt("pool", MIX[:, m, :], mxa[:], sgt[1][:], ALU.add, r=["mxa", "sgt1"], w=["MIX"])
        for u in range(4):
            for hf in range(2):
                bi = 6 + (u * 2 + hf) % 2
                for kc in range(8):
                    mm(ps[bi][:], MIX[:, kc, u * 128:(u + 1) * 128], WO[:, kc, hf * 512:(hf + 1) * 512], kc == 0, kc == 7,
                       r=["MIX", "WO"], w=[f"ps{bi}"])
                tt("dve", ACC[:, s * 4 + u, hf * 512:(hf + 1) * 512], ps[bi][:], xraw[:, u, hf * 512:(hf + 1) * 512], ALU.add,
                   r=[f"ps{bi}", "xraw"], w=[f"ACC{s}"])
    S.barrier()
    A.release(m5)

    TT = A.alloc("TT", [128, 8, 2048], BF16)
    COMB = A.alloc("COMB", [128, 16, 16], F32)
    m6 = A.mark()
    WRf = A.alloc("WRf", [128, 8, 20], F32)
    WRh = A.alloc("WRh", [128, 8, 20], BF16)
    WRl = A.alloc("WRl", [128, 8, 20], BF16)
    brt = A.alloc("brt", [128, 20], F32)
    dma("sp", WRf[:], wr_d.rearrange("(c p) n -> p c n", p=128), w=["WRf"])
    dma("sp", brt[:], br_d[:], w=["brt"])
    cp("dve", WRh[:], WRf[:], r=["WRf"], w=["WRh"])
    tt("dve", WRl[:], WRf[:], WRh[:], ALU.subtract, r=["WRf", "WRh"], w=["WRl"])
    xn2 = [A.alloc(f"xn2_{i}", [128, 4, D], F32) for i in range(2)]
    junk = A.alloc("junk4", [128, D], BF16)
    ss4 = [A.alloc(f"ss4_{i}", [128, 4], F32) for i in range(2)]
    rs4 = [A.alloc(f"rs4_{i}", [128, 4], F32) for i in range(2)]
    tlo = [A.alloc(f"tlo{i}", [128, 8, 512], BF16) for i in range(2)]
    RTs = [A.alloc(f"RT{i}", [128, 64], F32) for i in range(2)]
    for s in range(4):
        sl = slice(s * 512, (s + 1) * 512)
        i2 = s % 2
        xn, ssx, rsx, tl = xn2[i2], ss4[i2], rs4[i2], tlo[i2]
        xr_, sr_, tr_ = f"xn2_{i2}", f"ss4_{i2}", f"tlo{i2}"
        for u in range(4):
            act(junk[:], ACC[:, s * 4 + u, :], AF.Square, r=[f"ACC{s}"], w=["junk", sr_], accum_out=ssx[:, u:u + 1])
        ts("dve", rsx[:], ssx[:], 1.0 / D, 1e-6, ALU.mult, ALU.add, r=[sr_], w=[sr_ + "r"])
        act(rsx[:], rsx[:], AF.Sqrt, r=[sr_ + "r"], w=[sr_ + "r"])
        recip(rsx[:], rsx[:], r=[sr_ + "r"], w=[sr_ + "r"])
        for u in range(4):
            ts("dve", xn[:, u, :], ACC[:, s * 4 + u, :], rsx[:, u:u + 1], None, ALU.mult,
               r=[f"ACC{s}", sr_ + "r"], w=[xr_])
        for kc in range(8):
            pb, pr = ps[kc % 2], f"ps{kc % 2}"
            for u in range(4):
                tr(pb[:, u * 128:(u + 1) * 128], xn[:, u, kc * 128:(kc + 1) * 128], ident_f[:], r=[xr_, "ident_f"], w=[pr])
            S.op("act", lambda e, o=TT[:, kc, sl], i=pb[:], sc=gffn[:, kc:kc + 1]:
                 e.activation(out=o, in_=i, func=AF.Copy, scale=sc), r=[pr, "gffn"], w=[f"TT{s}"])
            stt(tl[:, kc, :], pb[:], gffn[:, kc:kc + 1], TT[:, kc, sl], ALU.mult, ALU.subtract, r=[pr, "gffn", f"TT{s}"], w=[tr_])
        for u in range(4):
            tok = slice(s * 512 + u * 128, s * 512 + (u + 1) * 128)
            pl = ps[2 + u % 2]
            plr = f"ps{2 + u % 2}"
            k = 0
            for (lh, wr_, wrr) in [("TT", WRh, "WRh"), ("tlo", WRh, "WRh"), ("TT", WRl, "WRl")]:
                for kc in range(8):
                    lhsT = TT[:, kc, tok] if lh == "TT" else tl[:, kc, u * 128:(u + 1) * 128]
                    mm(pl[:, 0:20], lhsT, wr_[:, kc, :], k == 0, k == 23, r=[f"TT{s}" if lh == "TT" else tr_, wrr], w=[plr])
                    k += 1
            RT = RTs[u % 2]
            rr = [f"RT{u % 2}"]
            L = RT[:, 0:20]
            tt("dve", L, pl[:, 0:20], brt[:], ALU.add, r=[plr, "brt"], w=rr)
            gl, el = RT[:, 0:4], RT[:, 4:20]
            gmax, ngmax, sumg, pgc = RT[:, 20:21], RT[:, 21:22], RT[:, 22:23], RT[:, 23:24]
            ohg, ein, msk, e2 = RT[:, 24:28], RT[:, 28:32], RT[:, 32:36], RT[:, 36:40]
            m1_, nm1, m2_, den = RT[:, 40:41], RT[:, 41:42], RT[:, 42:43], RT[:, 43:44]
            ex, exg = RT[:, 44:48], RT[:, 48:52]
            S.op("dve", lambda e, gl=gl, gmax=gmax: e.tensor_reduce(out=gmax, in_=gl, axis=AX.X, op=ALU.max), r=rr, w=rr)
            ts("dve", ngmax, gmax, -1.0, None, ALU.mult, r=rr, w=rr)
            ts("dve", ohg, gl, gmax, None, ALU.is_ge, r=rr, w=rr)
            act(exg, gl, AF.Exp, r=rr, w=rr, bias=ngmax, accum_out=sumg)
            recip(pgc, sumg, r=rr, w=rr)
            ts("dve", ein, el[:, 0:4], ohg[:, 0:1], None, ALU.mult, r=rr, w=rr)
            for g in range(1, 4):
                stt(ein, el[:, 4 * g:4 * g + 4], ohg[:, g:g + 1], ein, ALU.mult, ALU.add, r=rr, w=rr)
            S.op("dve", lambda e, ein=ein, m1_=m1_: e.tensor_reduce(out=m1_, in_=ein, axis=AX.X, op=ALU.max), r=rr, w=rr)
            ts("dve", msk, ein, m1_, -1e30, ALU.is_ge, ALU.mult, r=rr, w=rr)
            tt("dve", e2, msk, ein, ALU.add, r=rr, w=rr)
            S.op("dve", lambda e, e2=e2, m2_=m2_: e.tensor_reduce(out=m2_, in_=e2, axis=AX.X, op=ALU.max), r=rr, w=rr)
            ts("dve", msk, ein, m2_, None, ALU.is_ge, r=rr, w=rr)
            ts("dve", nm1, m1_, -1.0, None, ALU.mult, r=rr, w=rr)
            act(ex, ein, AF.Exp, r=rr, w=rr, bias=nm1)
            tt("dve", ex, ex, msk, ALU.mult, r=rr, w=rr)
            S.op("dve", lambda e, ex=ex, den=den: e.tensor_reduce(out=den, in_=ex, axis=AX.X, op=ALU.add), r=rr, w=rr)
            recip(den, den, r=rr, w=rr)
            ts("dve", ex, ex, den, pgc, ALU.mult, ALU.mult, r=rr, w=rr)
            for g in range(4):
                ts("dve", COMB[:, s * 4 + u, 4 * g:4 * g + 4], ex, ohg[:, g:g + 1], None, ALU.mult, r=rr, w=["COMB"])
    if "dbgB_big" in debug and stop_after == 6:
        for u in range(8):
            dma("sp", dbgB_big[:, u * 1024:(u + 1) * 1024], ACC[:, u, :], r=[f"ACC{u // 4}"], w=["dbgB_big"])
        dma("sp", dbgA[:, 0:256], COMB[:].rearrange("p a b -> p (a b)"), r=["COMB"], w=["dbgA"])
    S.barrier()
    A.release(m6)
    if stop_after <= 6:
        return finish()

    WGb = [A.alloc(f"WGb{i}", [128, 8, 512], BF16) for i in range(2)]
    WUb = [A.alloc(f"WUb{i}", [128, 8, 512], BF16) for i in range(2)]
    WDb = [A.alloc(f"WDb{i}", [128, 4, D], BF16) for i in range(2)]
    hid = [A.alloc(f"hid{i}", [128, 512], BF16) for i in range(8)]
    sgm = [A.alloc(f"sgm{i}", [128, 512], F32) for i in range(2)]
    hctr = [0]
    for e_ in range(16):
        bi = e_ % 2
        wgv = wg_d[e_].rearrange("(c p) n -> p c n", p=128)
        wuv = wu_d[e_].rearrange("(c p) n -> p c n", p=128)
        wdv = wd_d[e_].rearrange("(c p) n -> p c n", p=128)
        for hlf in range(2):
            dma("pool", WGb[bi][:, hlf * 4:(hlf + 1) * 4, :], wgv[:, hlf * 4:(hlf + 1) * 4, :], w=[f"WGb{bi}"])
            dma("pool", WUb[bi][:, hlf * 4:(hlf + 1) * 4, :], wuv[:, hlf * 4:(hlf + 1) * 4, :], w=[f"WUb{bi}"])
            dma("pool", WDb[bi][:, hlf * 2:(hlf + 1) * 2, :], wdv[:, hlf * 2:(hlf + 1) * 2, :], w=[f"WDb{bi}"])
        for tg in range(4):
            tsl = slice(tg * 512, (tg + 1) * 512)
            hs = []
            for fc in range(4):
                pgi, pui = (fc % 2) * 2, (fc % 2) * 2 + 1
                for kc in range(8):
                    mm(ps[pgi][:], WGb[bi][:, kc, fc * 128:(fc + 1) * 128], TT[:, kc, tsl], kc == 0, kc == 7,
                       r=[f"WGb{bi}", f"TT{tg}"], w=[f"ps{pgi}"])
                for kc in range(8):
                    mm(ps[pui][:], WUb[bi][:, kc, fc * 128:(fc + 1) * 128], TT[:, kc, tsl], kc == 0, kc == 7,
                       r=[f"WUb{bi}", f"TT{tg}"], w=[f"ps{pui}"])
                sg_ = sgm[fc % 2]
                hh_ = hid[hctr[0] % 8]
                hr = f"hid{hctr[0] % 8}"
                hctr[0] += 1
                act(sg_[:], ps[pgi][:], AF.Silu, r=[f"ps{pgi}"], w=[f"sgm{fc % 2}"])
                tt("dve", hh_[:], ps[pui][:], sg_[:], ALU.mult, r=[f"ps{pui}", f"sgm{fc % 2}"], w=[hr])
                hs.append((hh_, hr))
            for u in range(4):
                for hf in range(2):
                    yi = 4 + (u * 2 + hf) % 4
                    for fc in range(4):
                        mm(ps[yi][:], hs[fc][0][:, u * 128:(u + 1) * 128], WDb[bi][:, fc, hf * 512:(hf + 1) * 512],
                           fc == 0, fc == 3, r=[hs[fc][1], f"WDb{bi}"], w=[f"ps{yi}"])
                    a_ = ACC[:, tg * 4 + u, hf * 512:(hf + 1) * 512]
                    stt(a_, ps[yi][:], COMB[:, tg * 4 + u, e_:e_ + 1], a_, ALU.mult, ALU.add,
                        r=[f"ps{yi}", "COMB", f"ACC{tg}"], w=[f"ACC{tg}"])
    for u in range(16):
        dma("sp", out_d[u * 128:(u + 1) * 128, :], ACC[:, u, :], r=[f"ACC{u // 4}"], w=["out"])
    return finish()


def make_in_maps(inputs):
    f = np.float32
    g = lambda k: np.asarray(inputs[k], f)
    x = g("x")
    maps = []
    shared = {}
    shared["w_in"] = np.ascontiguousarray(g("w_in")[0])
    shared["gmix"] = np.ascontiguousarray(g("norm_mix_g")[0].reshape(8, 128).T)
    shared["gffn"] = np.ascontiguousarray(g("norm_ffn_g")[0].reshape(8, 128).T)
    shared["gk2"] = np.ascontiguousarray(np.stack([np.tile(g("fox_k_g")[0], 2), np.tile(g("nsa_k_g")[0], 2),
                                                    np.tile(g("fox_q_g")[0], 2), np.tile(g("nsa_q_g")[0], 2)], 1))
    shared["bfb"] = np.ascontiguousarray(np.tile(g("b_forget")[0][None, :], (128, 1)))
    nb = np.zeros((128, 1), f)
    nb[:8, 0] = -g("b_forget")[0]
    shared["nbfc"] = nb
    shared["cmp_k_w1"] = np.ascontiguousarray(g("cmp_k_w1")[0])
    shared["cmp_v_w1"] = np.ascontiguousarray(g("cmp_v_w1")[0])
    shared["cmp_k_w2"] = np.ascontiguousarray(g("cmp_k_w2")[0])
    shared["cmp_v_w2"] = np.ascontiguousarray(g("cmp_v_w2")[0])
    shared["poskT"] = np.ascontiguousarray(np.tile(g("cmp_k_pos")[0].T, (2, 1)))
    shared["posvT"] = np.ascontiguousarray(np.tile(g("cmp_v_pos")[0].T, (2, 1)))
    shared["w_fox_up"] = np.ascontiguousarray(g("w_fox_up")[0])
    shared["w_nsa_up"] = np.ascontiguousarray(g("w_nsa_up")[0])
    shared["w_out"] = np.ascontiguousarray(g("w_out")[0])
    shared["w_rt"] = np.ascontiguousarray(np.concatenate([g("w_group")[0], g("w_router")[0]], 1))
    shared["b_rt"] = np.ascontiguousarray(np.tile(np.concatenate([g("b_group")[0], g("b_router")[0]])[None, :], (128, 1)))
    shared["w_gate"] = np.ascontiguousarray(g("w_gate")[0])
    shared["w_up"] = np.ascontiguousarray(g("w_up")[0])
    shared["w_down"] = np.ascontiguousarray(g("w_down")[0])
    consts = [host_consts(j) for j in range(4)]
    for c in range(8):
        b, j = c // 4, c % 4
        m = dict(shared)
        m["xb"] = np.ascontiguousarray(x[b])
        m["xq"] = np.ascontiguousarray(np.concatenate([x[b, 512 * (4 * s + j):512 * (4 * s + j + 1)] for s in range(4)], 0))
        xw = np.zeros((4096, D), f)
        for s in range(4):
            q0 = 512 * (4 * s + j)
            lo = q0 - 512
            if lo >= 0:
                xw[1024 * s:1024 * (s + 1)] = x[b, lo:lo + 1024]
            else:
                xw[1024 * s + 512:1024 * (s + 1)] = x[b, 0:512]
        m["xw"] = xw
        for k, v in consts[j].items():
            m[k] = np.ascontiguousarray(v.reshape(CONST_SHAPES[k]))
        maps.append(m)
    return maps


def kernel(**inputs):
    nc = build()
    maps = make_in_maps(inputs)
    res = run_bass_kernel_spmd(nc, maps, core_ids=list(range(8)))
    out = np.zeros((2, S_LEN, D), np.float32)
    for c in range(8):
        b, j = c // 4, c % 4
        o = res.results[c]["out"]
        for s in range(4):
            out[b, 512 * (4 * s + j):512 * (4 * s + j + 1)] = o[512 * s:512 * (s + 1)]
    return out
```

```python
import contextlib
import numpy as np
import concourse.bass as bass
import concourse.mybir as mybir
from concourse.bass_utils import run_bass_kernel_spmd

F32 = mybir.dt.float32
BF16 = mybir.dt.bfloat16
AF = mybir.ActivationFunctionType
ALU = mybir.AluOpType
AX = mybir.AxisListType

S_LEN = 8192
D = 1024
NT = 64
NEGM = -30000.0
STRICT_SAME_ENGINE = True
DIN = 4896


class _Op:
    __slots__ = ("id", "eng", "fn", "deps", "dma", "needs_inc", "sem", "val")

    def __init__(self, id, eng, fn, dma):
        self.id = id
        self.eng = eng
        self.fn = fn
        self.deps = []
        self.dma = dma
        self.needs_inc = False
        self.sem = None
        self.val = 0


class Sched:
    ENGS = ("pe", "act", "dve", "pool", "sp")
    NDMA = {"sp": 12, "pool": 8, "act": 2, "pe": 1, "dve": 1}

    def __init__(self, nc):
        self.nc = nc
        self.ops = {e: [] for e in self.ENGS}
        self.last_w = {}
        self.readers = {}
        self.n = 0
        self.dma_hist = {e: [] for e in self.ENGS}

    def op(self, eng, fn, r=(), w=(), dma=False):
        o = _Op(self.n, eng, fn, dma)
        self.n += 1
        deps = {}
        for res in r:
            lw = self.last_w.get(res)
            if lw is not None:
                deps[lw.id] = lw
        for res in w:
            lw = self.last_w.get(res)
            if lw is not None:
                deps[lw.id] = lw
            for rd in self.readers.get(res, ()):
                deps[rd.id] = rd
        if dma:
            hist = self.dma_hist[eng]
            n = self.NDMA[eng]
            if len(hist) >= n:
                p = hist[len(hist) - n]
                deps[p.id] = p
            hist.append(o)
        for d in deps.values():
            if d is o:
                continue
            if d.eng == eng and (eng == "pe" or not STRICT_SAME_ENGINE) and not d.dma and not dma:
                continue
            if not d.dma:
                d.needs_inc = True
            o.deps.append(d)
        for res in r:
            self.readers.setdefault(res, []).append(o)
        for res in w:
            self.last_w[res] = o
            self.readers[res] = []
        self.ops[eng].append(o)
        return o

    def barrier(self):
        lasts = []
        for e in self.ENGS:
            comp = [o for o in self.ops[e] if not o.dma and o.fn is not None]
            if comp:
                lasts.append(comp[-1])
            lasts.extend(self.dma_hist[e][-self.NDMA[e]:])
        for e in self.ENGS:
            o = _Op(self.n, e, None, False)
            self.n += 1
            for d in lasts:
                if d.eng == e and e == "pe" and not d.dma:
                    continue
                if not d.dma:
                    d.needs_inc = True
                o.deps.append(d)
            self.ops[e].append(o)
        self.last_w = {}
        self.readers = {}

    def emit(self):
        nc = self.nc
        with contextlib.ExitStack() as st:
            esem = {e: st.enter_context(nc.semaphore(f"s_{e}")) for e in self.ENGS}
            dsem = {e: [st.enter_context(nc.semaphore(f"d_{e}{i}")) for i in range(self.NDMA[e])]
                    for e in self.ENGS}
            for e in self.ENGS:
                cnt = 0
                dcnt = [0] * self.NDMA[e]
                k = 0
                for o in self.ops[e]:
                    if o.dma:
                        i = k % self.NDMA[e]
                        k += 1
                        dcnt[i] += 16
                        o.sem = dsem[e][i]
                        o.val = dcnt[i]
                    elif o.needs_inc:
                        cnt += 1
                        o.sem = esem[e]
                        o.val = cnt
            block = st.enter_context(nc.Block())

            def run(engobj, ops):
                waited = {}
                for o in ops:
                    need = {}
                    for d in o.deps:
                        key = id(d.sem)
                        if d.val > need.get(key, (0, None))[0]:
                            need[key] = (d.val, d.sem)
                    for key, (val, sem) in need.items():
                        if waited.get(key, 0) < val:
                            engobj.wait_ge(sem, val)
                            waited[key] = val
                    if o.fn is None:
                        continue
                    ins = o.fn(engobj)
                    if o.dma:
                        ins.then_inc(o.sem, 16)
                    elif o.needs_inc:
                        ins.then_inc(o.sem, 1)

            if self.ops["pe"]:
                @block.tensor
                def _(e):
                    run(e, self.ops["pe"])
            if self.ops["act"]:
                @block.scalar
                def _(e):
                    run(e, self.ops["act"])
            if self.ops["dve"]:
                @block.vector
                def _(e):
                    run(e, self.ops["dve"])
            if self.ops["pool"]:
                @block.gpsimd
                def _(e):
                    run(e, self.ops["pool"])
            if self.ops["sp"]:
                @block.sync
                def _(e):
                    run(e, self.ops["sp"])


class Arena:
    def __init__(self, nc, base=18560, limit=229376):
        self.nc = nc
        self.off = base
        self.limit = limit
        self.k = 0

    def alloc(self, name, shape, dt):
        nb = int(np.prod(shape[1:])) * (4 if dt == F32 else 2)
        nb = (nb + 63) // 64 * 64
        assert self.off + nb <= self.limit, f"SBUF overflow at {name}: {self.off}+{nb}"
        self.k += 1
        t = self.nc.alloc_sbuf_tensor_at(f"{name}_{self.k}", list(shape), dt, offset=self.off)
        self.off += nb
        self.hw = max(getattr(self, "hw", 0), self.off)
        return t.ap()

    def mark(self):
        return self.off

    def release(self, m):
        self.off = m


def host_consts(j):
    c = {}
    f = np.float32
    c["c_ident"] = np.eye(128, dtype=f)
    blk = np.zeros((128, 128), f)
    blk[:64, :64] = 1.0
    blk[64:, 64:] = 1.0
    c["c_blk"] = blk
    p = np.arange(128)
    c["c_utri"] = (p[:, None] <= p[None, :]).astype(f)
    c["c_ones"] = np.ones((128, 128), f)
    q0 = np.array([512 * (4 * s + j) for s in range(4)])
    t64 = np.arange(64)
    oh = np.zeros((128, 4, 64), f)
    badd = np.zeros((128, 4, 64), f)
    for s in range(4):
        oh[:, s, 4 * (4 * s + j)] = 1.0
        badd[:, s, 16 * s + 4 * (j + 1):16 * s + 16] = NEGM
    c["c_oh"] = oh
    c["c_badd"] = badd
    am = np.zeros((128, 4, 128), f)
    am[:, j, :] = np.eye(128, dtype=f)
    c["c_am"] = am
    ql = np.arange(512)
    dg = np.zeros((128, 4, 512), f)
    for tl in range(4):
        dg[:, tl, :] = np.where(128 * tl + p[:, None] <= ql[None, :], 0.0, NEGM)
    c["c_dg"] = dg
    wm = np.zeros((128, 8, 512), f)
    for t in range(8):
        dist = ql[None, :] - (128 * t + p[:, None]) + 512
        wm[:, t, :] = np.where((dist >= 0) & (dist < 512), 0.0, NEGM)
    c["c_wm"] = wm
    cm = np.zeros((128, 4, 4, 512), f)
    for s in range(4):
        for cc in range(4):
            n = 128 * cc + p[:, None]
            q = q0[s] + ql[None, :]
            cm[:, s, cc, :] = np.where((16 * n + 31 <= q) & (n < 511), 0.0, NEGM)
    c["c_cm"] = cm
    slopes = np.array([2.0 ** (-(h + 1)) for h in range(8)], np.float64)
    kbs = np.zeros((128, 8, 4, 64), f)
    kbw = np.zeros((128, 8, 4, 8), f)
    kbc = np.zeros((128, 8, 4, 4), f)
    for h in range(8):
        for s in range(4):
            kpos = 128 * t64[None, :] + p[:, None]
            kbs[:, h, s, :] = slopes[h] * (kpos - q0[s]) + badd[:, s, :]
            wpos = q0[s] - 512 + 128 * np.arange(8)[None, :] + p[:, None]
            kbw[:, h, s, :] = np.where(wpos >= 0, slopes[h] * (wpos - q0[s]), NEGM)
            cend = 16 * (128 * np.arange(4)[None, :] + p[:, None]) + 31
            kbc[:, h, s, :] = slopes[h] * (cend - q0[s])
    c["c_kbs"] = kbs.reshape(128, -1)
    c["c_kbw"] = kbw.reshape(128, -1)
    c["c_kbc"] = kbc.reshape(128, -1)
    asel = np.zeros((128, 4, 4, 128), f)
    bsel = np.zeros((128, 4, 4, 128), f)
    jb = np.arange(128)
    for s in range(4):
        for sub in range(4):
            q = q0[s] + 128 * sub + p[:, None]
            qb = q // 64
            future = 64 * jb[None, :] > q
            forced = (jb[None, :] == 0) | (jb[None, :] == qb) | (jb[None, :] == qb - 1)
            asel[:, s, sub, :] = np.where(future | forced, 0.0, 1.0)
            bsel[:, s, sub, :] = np.where(future, -1.0, np.where(forced, 1e4, 0.0))
    c["c_asel"] = asel.reshape(128, -1)
    c["c_bsel"] = bsel.reshape(128, -1)
    ov = np.zeros((128, 4, 128), f)
    for cc in range(4):
        for pp in range(128):
            n = 128 * cc + pp
            if n >= 511:
                continue
            a0, a1 = 16 * n, 16 * n + 32
            for b_ in range(a0 // 64, (a1 - 1) // 64 + 1):
                ovl = min(a1, 64 * b_ + 64) - max(a0, 64 * b_)
                ov[pp, cc, b_] = ovl / 32.0
    c["c_ov"] = ov.reshape(128, -1)
    e_all = np.zeros((128, S_LEN), f)
    for b_ in range(128):
        e_all[b_, 64 * b_:64 * b_ + 64] = 1.0
    c["c_eall"] = e_all
    c["c_onerow"] = np.ones((1, S_LEN), f)
    qaug = np.zeros((8, 2, 2048), f)
    qq = np.arange(2048) % 512
    for h in range(8):
        qaug[h, 0] = -slopes[h] * (qq % 256)
        qaug[h, 1] = -slopes[h] * 256 * (qq // 256)
    c["c_qaug"] = qaug
    selg = np.zeros((64, 24, 128), f)
    for hb in range(24):
        selg[hb, hb, :] = 1.0
        selg[32 + hb, hb, :] = 1.0
    c["c_selg"] = selg.reshape(64, -1)
    return c


CONST_SHAPES = {
    "c_ident": [128, 128], "c_blk": [128, 128], "c_utri": [128, 128], "c_ones": [128, 128],
    "c_oh": [128, 4, 64], "c_badd": [128, 4, 64], "c_am": [128, 4, 128], "c_dg": [128, 4, 512],
    "c_wm": [128, 8, 512], "c_cm": [128, 4, 4, 512], "c_kbs": [128, 2048], "c_kbw": [128, 256],
    "c_kbc": [128, 128], "c_asel": [128, 2048], "c_bsel": [128, 2048], "c_ov": [128, 512],
    "c_eall": [128, S_LEN], "c_onerow": [1, S_LEN], "c_qaug": [8, 2, 2048], "c_selg": [64, 24 * 128],
}


def build(debug=None, stop_after=99):
    nc = bass.Bass("TRN2", target_bir_lowering=False)
    S = Sched(nc)
    A = Arena(nc)
    debug = debug or set()

    def din(name, shape):
        return nc.dram_tensor(name, list(shape), F32, kind="ExternalInput").ap()

    def scratch(name, shape, dt):
        kind = "ExternalOutput" if name in debug else "Internal"
        return nc.dram_tensor(name, list(shape), dt, kind=kind).ap()

    xb = din("xb", [S_LEN, D])
    xq = din("xq", [2048, D])
    xw = din("xw", [4096, D])
    w_in = din("w_in", [D, DIN])
    C = {k: din(k, v) for k, v in CONST_SHAPES.items()}
    gmix_d = din("gmix", [128, 8])
    gffn_d = din("gffn", [128, 8])
    gk2_d = din("gk2", [128, 4])
    bfb_d = din("bfb", [128, 8])
    nbfc_d = din("nbfc", [128, 1])
    w1k_d = din("cmp_k_w1", [2048, 256])
    w1v_d = din("cmp_v_w1", [2048, 256])
    w2k_d = din("cmp_k_w2", [256, 64])
    w2v_d = din("cmp_v_w2", [256, 64])
    posk_d = din("poskT", [128, 32])
    posv_d = din("posvT", [128, 32])
    wfu_d = din("w_fox_up", [512, D])
    wnu_d = din("w_nsa_up", [512, D])
    wout_d = din("w_out", [D, D])
    wr_d = din("w_rt", [D, 20])
    br_d = din("b_rt", [128, 20])
    wg_d = din("w_gate", [16, D, 512])
    wu_d = din("w_up", [16, D, 512])
    wd_d = din("w_down", [16, 512, D])
    out_d = nc.dram_tensor("out", [2048, D], F32, kind="ExternalOutput").ap()

    KT_fox = scratch("KT_fox", [8, 64, S_LEN], BF16)
    V_fox = scratch("V_fox", [8, 128, NT, 128], BF16)
    KT_s = scratch("KT_s", [2, 64, S_LEN], BF16)
    KT_w = scratch("KT_w", [2, 64, 4096], BF16)
    V_s = scratch("V_s", [2, 128, NT, 128], BF16)
    V_w = scratch("V_w", [2, 128, 32, 128], BF16)
    OA_scr = scratch("OA_scr", [4, 128, 2048], BF16)
    OB_scr = scratch("OB_scr", [4, 128, 2048], BF16)
    dbgA = scratch("dbgA", [128, 4096], F32)
    dbgB = scratch("dbgB", [128, 4096], F32)
    dbgB_big = scratch("dbgB_big", [128, 8192], F32)

    ps = [nc.alloc_psum_tensor(f"ps{i}", [128, 512], F32).ap() for i in range(8)]
    ps7b = ps[7].bitcast(BF16)

    def dma(eng, out, in_, r=(), w=()):
        return S.op(eng, lambda e: e.dma_start(out=out, in_=in_), r=r, w=w, dma=True)

    def mm(out, lhsT, rhs, start, stop, r=(), w=()):
        return S.op("pe", lambda e: e.matmul(out, lhsT=lhsT, rhs=rhs, start=start, stop=stop,
                                             skip_group_check=True), r=r, w=w)

    def tr(out, in_, ident, r=(), w=()):
        return S.op("pe", lambda e: e.transpose(out=out, in_=in_, identity=ident), r=r, w=w)

    def act(out, in_, func, r=(), w=(), bias=None, scale=1.0, accum_out=None):
        def f(e):
            kw = {}
            if bias is not None:
                kw["bias"] = bias
            if accum_out is not None:
                kw["accum_out"] = accum_out
            return e.activation(out=out, in_=in_, func=func, scale=scale, **kw)
        return S.op("act", f, r=r, w=w)

    def ts(eng, out, in0, s1, s2, op0, op1=None, r=(), w=()):
        def f(e):
            if op1 is None:
                return e.tensor_scalar(out=out, in0=in0, scalar1=s1, scalar2=None, op0=op0)
            return e.tensor_scalar(out=out, in0=in0, scalar1=s1, scalar2=s2, op0=op0, op1=op1)
        return S.op(eng, f, r=r, w=w)

    def tt(eng, out, in0, in1, op, r=(), w=()):
        return S.op(eng, lambda e: e.tensor_tensor(out=out, in0=in0, in1=in1, op=op), r=r, w=w)

    def stt(out, in0, scalar, in1, op0, op1, r=(), w=()):
        return S.op("dve", lambda e: e.scalar_tensor_tensor(out=out, in0=in0, scalar=scalar, in1=in1,
                                                            op0=op0, op1=op1), r=r, w=w)

    def cp(eng, out, in_, r=(), w=()):
        if eng == "act":
            return act(out, in_, AF.Copy, r=r, w=w)
        return S.op(eng, lambda e: e.tensor_copy(out=out, in_=in_), r=r, w=w)

    def recip(out, in_, r=(), w=()):
        return S.op("dve", lambda e: e.reciprocal(out=out, in_=in_), r=r, w=w)

    def memset(eng, ap, val, w=()):
        return S.op(eng, lambda e: e.memset(ap, val), w=w)

    def finish():
        if debug:
            print("arena high water", A.hw - 18560, "bytes of", A.limit - 18560)
        S.barrier()
        S.emit()
        return nc

    ident_f = A.alloc("ident_f", [128, 128], F32)
    ident_b = A.alloc("ident_b", [128, 128], BF16)
    blk = A.alloc("blk", [128, 128], BF16)
    epsb = A.alloc("epsb", [128, 1], F32)
    lnq = A.alloc("lnq", [128, 1], F32)
    gmix = A.alloc("gmix", [128, 8], F32)
    gk2 = A.alloc("gk2", [128, 4], F32)
    dma("sp", ident_f[:], C["c_ident"][:], w=["ident_f"])
    dma("pool", ident_b[:], C["c_ident"][:], w=["ident_b"])
    dma("pool", blk[:], C["c_blk"][:], w=["blk"])
    dma("sp", gmix[:], gmix_d[:], w=["gmix"])
    dma("sp", gk2[:], gk2_d[:], w=["gk2"])
    memset("pool", epsb[:], 1e-6, w=["epsb"])
    memset("pool", lnq[:], float(np.log(0.125)), w=["lnq"])
    tinyb = A.alloc("tinyb", [128, 1], F32)
    memset("pool", tinyb[:], 1e-30, w=["tinyb"])

    m_base = A.mark()
    KC = [A.alloc(f"KC{g}", [128, 512], BF16) for g in range(2)]
    VC = [A.alloc(f"VC{g}", [128, 4, 128], BF16) for g in range(2)]
    BKB = A.alloc("BKB", [128, 2048], F32)

    krawb = [A.alloc(f"krawb{i}", [128, 512], BF16) for i in range(2)]
    ksq = [A.alloc(f"ksq{i}", [128, 512], BF16) for i in range(2)]
    klv = [A.alloc(f"klv{i}", [128, 512], F32) for i in range(2)]
    nrm_nbuf = [2]
    nrm_ctr = [0]

    def qknorm(pb, pr, P, N, gcol, out_ap, out_res, qscale=False, psum_idx=5, split=None):
        i2 = nrm_ctr[0] % nrm_nbuf[0]
        nrm_ctr[0] += 1
        kr, kq, kl = krawb[i2], ksq[i2], klv[i2]
        cp("act", kr[:P, :N], pb, r=[pr], w=[f"kraw{i2}"])
        tt("dve", kq[:P, :N], kr[:P, :N], kr[:P, :N], ALU.mult, r=[f"kraw{i2}"], w=[f"ksq{i2}"])
        bi = psum_idx
        pb2 = ps[bi]
        mm(pb2[:P, :N], blk[:P, :P], kq[:P, :N], True, True, r=["blk", f"ksq{i2}"], w=[f"ps{bi}"])
        act(kl[:P, :N], pb2[:P, :N], AF.Ln, r=[f"ps{bi}", "epsb"], w=[f"klv{i2}"], bias=epsb[:P, :], scale=1.0 / 64)
        if qscale:
            act(kl[:P, :N], kl[:P, :N], AF.Exp, r=[f"klv{i2}", "lnq"], w=[f"klv{i2}"], scale=-0.5, bias=lnq[:P, :])
        else:
            act(kl[:P, :N], kl[:P, :N], AF.Exp, r=[f"klv{i2}"], w=[f"klv{i2}"], scale=-0.5)
        if split is not None:
            (oa, ra, ob, rb) = split
            stt(oa, kr[0:64, :N], gcol[0:64, :], kl[0:64, :N], ALU.mult, ALU.mult, r=[f"kraw{i2}", f"klv{i2}", "gk2"], w=[ra])
            stt(ob, kr[64:128, :N], gcol[64:128, :], kl[64:128, :N], ALU.mult, ALU.mult, r=[f"kraw{i2}", f"klv{i2}", "gk2"], w=[rb])
            return
        stt(out_ap, kr[:P, :N], gcol, kl[:P, :N], ALU.mult, ALU.mult, r=[f"kraw{i2}", f"klv{i2}", "gk2"], w=[out_res])

    def qknormA(pb, pr, ssbank):
        i2 = nrm_ctr[0] % nrm_nbuf[0]
        nrm_ctr[0] += 1
        kr, kq = krawb[i2], ksq[i2]
        cp("act", kr[:, :], pb, r=[pr], w=[f"kraw{i2}"])
        tt("dve", kq[:, :], kr[:, :], kr[:, :], ALU.mult, r=[f"kraw{i2}"], w=[f"ksq{i2}"])
        mm(ps[ssbank][:, :], blk[:, :], kq[:, :], True, True, r=["blk", f"ksq{i2}"], w=[f"ps{ssbank}"])
        return i2, ssbank

    def qknormB(st, gcol, out_ap, out_res):
        i2, ssbank = st
        kr, kl = krawb[i2], klv[i2]
        act(kl[:, :], ps[ssbank][:, :], AF.Ln, r=[f"ps{ssbank}", "epsb"], w=[f"klv{i2}"], bias=epsb[:, :], scale=1.0 / 64)
        act(kl[:, :], kl[:, :], AF.Exp, r=[f"klv{i2}"], w=[f"klv{i2}"], scale=-0.5)
        stt(out_ap, kr[:, :], gcol, kl[:, :], ALU.mult, ALU.mult, r=[f"kraw{i2}", f"klv{i2}", "gk2"], w=[out_res])

    def norm_scale(xt, rx, ss, rs, rss):
        for sub in range(4):
            act(junk[:], xt[:, sub, :], AF.Square, r=[rx], w=["junk", rss], accum_out=ss[:, sub:sub + 1])
        act(rs[:], ss[:], AF.Ln, r=[rss, "epsb"], w=[rss + "r"], bias=epsb[:, :], scale=1.0 / D)
        act(rs[:], rs[:], AF.Exp, r=[rss + "r"], w=[rss + "r"], scale=-0.5)
        for sub in range(4):
            ts("dve", xt[:, sub, :], xt[:, sub, :], rs[:, sub:sub + 1], None, ALU.mult, r=[rx, rss + "r"], w=[rx])

    def transpose_gain(xt, rx, hT_dst, rh, gvec, gres):
        for kc in range(8):
            pb = ps[kc % 2]
            pr = f"ps{kc % 2}"
            for sub in range(4):
                tr(pb[:, sub * 128:(sub + 1) * 128], xt[:, sub, kc * 128:(kc + 1) * 128], ident_f[:],
                   r=[rx, "ident_f"], w=[pr])
            if kc % 2 == 0:
                S.op("act", lambda e, o=hT_dst[:, kc, :], i=pb[:], s=gvec[:, kc:kc + 1]:
                     e.activation(out=o, in_=i, func=AF.Copy, scale=s), r=[pr, gres], w=[rh])
            else:
                ts("dve", hT_dst[:, kc, :], pb[:], gvec[:, kc:kc + 1], None, ALU.mult, r=[pr, gres], w=[rh])

    def norm_transpose(src_rows, xt, rx, hT_dst, rh, ss, rs, rss, gvec, gres, evac_ctr, load=True):
        if load:
            dma("sp", xt[:], src_rows.rearrange("(s p) d -> p s d", p=128), w=[rx])
        norm_scale(xt, rx, ss, rs, rss)
        transpose_gain(xt, rx, hT_dst, rh, gvec, gres)

    m1 = A.mark()
    LF = A.alloc("LF", [128, NT * 8], F32)
    krawb.append(A.alloc("krawb2", [128, 512], BF16))
    ksq.append(A.alloc("ksq2", [128, 512], BF16))
    klv.append(A.alloc("klv2", [128, 512], F32))
    nrm_nbuf[0] = 3
    TCk = A.alloc("TCk", [128, S_LEN], BF16)
    TCv = A.alloc("TCv", [128, S_LEN], BF16)
    WK = A.alloc("WK", [128, 8, 1800], BF16)
    w_in_v = w_in.rearrange("(kc p) n -> p kc n", p=128)
    wk_cols = [(512, 1024, 0), (2312, 2440, 512), (2568, 2696, 640), (2056, 2184, 768), (2184, 2312, 896),
               (1024, 1536, 1024), (2440, 2568, 1536), (2696, 2824, 1664), (1536, 1544, 1792)]
    for (c0, c1, d0) in wk_cols:
        dma("pool", WK[:, :, d0:d0 + (c1 - c0)], w_in_v[:, :, c0:c1], w=["WK"])
    bfb = A.alloc("bfb", [128, 8], F32)
    dma("sp", bfb[:], bfb_d[:], w=["bfb"])
    xbuf = [A.alloc(f"xbuf{i}", [128, 4, D], F32) for i in range(3)]
    hbuf = [A.alloc(f"hbuf{i}", [128, 8, 512], BF16) for i in range(2)]
    junk = A.alloc("junk", [128, D], BF16)
    ssb = [A.alloc(f"ss{i}", [128, 4], F32) for i in range(3)]
    rsb = [A.alloc(f"rs{i}", [128, 4], F32) for i in range(3)]
    kn = [A.alloc(f"kn{i}", [128, 512], BF16) for i in range(3)]
    VA = [A.alloc(f"VA{i}", [128, 8, 128], BF16) for i in range(2)]
    VAn = [A.alloc(f"VAn{i}", [128, 2, 128], BF16) for i in range(2)]
    zt = A.alloc("zt", [128, 32], F32)
    for i in range(2):
        memset("pool", VA[i][:, :, 64:128], 1.0, w=[f"VA{i}"])
        memset("pool", VAn[i][:, :, 64:128], 1.0, w=[f"VAn{i}"])
    utri = A.alloc("utri", [128, 128], F32)
    onesm = A.alloc("onesm", [128, 128], F32)
    oh = A.alloc("oh", [128, 4, 64], F32)
    badd = A.alloc("badd", [128, 4, 64], F32)
    dma("sp", utri[:], C["c_utri"][:], w=["utri"])
    dma("sp", onesm[:], C["c_ones"][:], w=["onesm"])
    dma("sp", oh[:], C["c_oh"][:], w=["oh"])
    dma("sp", badd[:], C["c_badd"][:], w=["badd"])
    CKW = A.alloc("CKW", [128, 512], F32)
    TOT = A.alloc("TOT", [128, 512], F32)
    INCL = A.alloc("INCL", [128, 512], F32)
    RS = A.alloc("RS", [128, 32], F32)
    prod = A.alloc("prod", [128, 512], F32)
    W1 = A.alloc("W1", [128, 32, 256], BF16)
    W2 = A.alloc("W2", [128, 2, 64], BF16)
    posT = A.alloc("posT", [128, 32], BF16)
    posb = A.alloc("posb", [128, 2], F32)
    Ucm = A.alloc("Ucm", [128, 512], F32)
    Tcm = A.alloc("Tcm", [128, 512], F32)
    HD = [A.alloc(f"HD{i}", [128, 512], BF16) for i in range(2)]
    for g in range(2):
        memset("pool", KC[g][:], 0.0, w=[f"KC{g}"])
        dma("pool", KC[g][64:65, :], C["c_onerow"][:, 0:512], w=[f"KC{g}"])
        dma("pool", KC[g][96:97, :], C["c_onerow"][:, 0:512], w=[f"KC{g}"])
        memset("pool", VC[g][:], 0.0, w=[f"VC{g}"])
        memset("pool", VC[g][:, :, 64:128], 1.0, w=[f"VC{g}"])
    for i in range(2):
        memset("pool", HD[i][:], 0.0, w=[f"HD{i}"])

    KTf_rows = KT_fox.rearrange("h p t -> (h p) t")
    KTs_rows = KT_s.rearrange("g p t -> (g p) t")
    KTw_rows = KT_w.rearrange("g p t -> (g p) t")
    Vf_v = V_fox.rearrange("h p t c -> p h t c")
    Vs_v = V_s.rearrange("g p t c -> p g t c")
    Vw_v = V_w.rearrange("g p t c -> p g t c")

    npair = [0]
    nss = [0]
    groups = [(xb[G * 512:(G + 1) * 512, :], G, False) for G in range(16)] + \
             [(xw[G * 512:(G + 1) * 512, :], G, True) for G in range(8)]

    def g_load(k):
        src, G, window = groups[k]
        xi = k % 3
        dma("sp", xbuf[xi][:], src.rearrange("(s p) d -> p s d", p=128), w=[f"xbuf{xi}"])

    def g_stageA(k_tr, k_sq):
        if k_tr is not None:
            xi, gi = k_tr % 3, k_tr % 2
            xt, rx, hT, rh = xbuf[xi], f"xbuf{xi}", hbuf[gi], f"hbuf{gi}"
        if k_sq is not None:
            xs = k_sq % 3
            xt2, rx2, ss2, rs2, rss2 = xbuf[xs], f"xbuf{xs}", ssb[xs], rsb[xs], f"ss{xs}"
        for kc in range(8):
            if k_tr is not None:
                pb = ps[kc % 2]
                pr = f"ps{kc % 2}"
                for sub in range(4):
                    tr(pb[:, sub * 128:(sub + 1) * 128], xt[:, sub, kc * 128:(kc + 1) * 128], ident_f[:],
                       r=[rx, "ident_f"], w=[pr])
                if kc % 2 == 0:
                    S.op("act", lambda e, o=hT[:, kc, :], i=pb[:], s_=gmix[:, kc:kc + 1]:
                         e.activation(out=o, in_=i, func=AF.Copy, scale=s_), r=[pr, "gmix"], w=[rh])
                else:
                    ts("dve", hT[:, kc, :], pb[:], gmix[:, kc:kc + 1], None, ALU.mult, r=[pr, "gmix"], w=[rh])
            if k_sq is not None and kc % 2 == 1:
                sub = kc // 2
                act(junk[:], xt2[:, sub, :], AF.Square, r=[rx2], w=["junk", rss2], accum_out=ss2[:, sub:sub + 1])
        if k_sq is not None:
            act(rs2[:], ss2[:], AF.Ln, r=[rss2, "epsb"], w=[rss2 + "r"], bias=epsb[:, :], scale=1.0 / D)
            act(rs2[:], rs2[:], AF.Exp, r=[rss2 + "r"], w=[rss2 + "r"], scale=-0.5)
            for sub in range(4):
                ts("dve", xt2[:, sub, :], xt2[:, sub, :], rs2[:, sub:sub + 1], None, ALU.mult, r=[rx2, rss2 + "r"], w=[rx2])

    def g_stageB(k):
        src, G, window = groups[k]
        gi = k % 2
        hT, rh = hbuf[gi], f"hbuf{gi}"
        pairs = [5] if window else [0, 1, 2, 3, 4, 6, 7]

        def proj(pi):
            c0 = pi * 128
            bi = 2 + npair[0] % 3
            npair[0] += 1
            pb, pr = ps[bi], f"ps{bi}"
            for kc in range(8):
                mm(pb[:], WK[:, kc, c0:c0 + 128], hT[:, kc, :], kc == 0, kc == 7, r=["WK", rh], w=[pr])
            return pb, pr

        def chainA(pi, pb, pr):
            if pi >= 6:
                dst = TCk if pi == 6 else TCv
                cp("act", dst[:, G * 512:(G + 1) * 512], pb[:], r=[pr], w=["TC"])
                return None
            ssbank = 5 if nss[0] % 2 == 0 else 7
            nss[0] += 1
            return qknormA(pb[:], pr, ssbank)

        def chainB(pi, st):
            if st is None:
                return
            kno = kn[npair[0] % 3]
            kres = f"kn{npair[0] % 3}"
            npair[0] += 0
            kno = kn[st[0]]
            kres = f"kn{st[0]}"
            gcol = gk2[:, 0:1] if pi < 4 else gk2[:, 1:2]
            qknormB(st, gcol, kno[:], kres)
            if pi < 4:
                dst = KTf_rows[pi * 128:(pi + 1) * 128, G * 512:(G + 1) * 512]
            elif pi == 4:
                dst = KTs_rows[:, G * 512:(G + 1) * 512]
            else:
                dst = KTw_rows[:, G * 512:(G + 1) * 512]
            dma("pool", dst, kno[:], r=[kres], w=["KT_scr"])

        def vgroup(sub, fox):
            tile = G * 4 + sub
            bi = 6
            pb, pr = ps[bi], f"ps{bi}"
            if fox:
                for kc in range(8):
                    mm(pb[:], hT[:, kc, sub * 128:(sub + 1) * 128], WK[:, kc, 1024:1536], kc == 0, kc == 7,
                       r=["WK", rh], w=[pr])
                va = VA[tile % 2]
                cp("dve" if sub % 2 == 0 else "act", va[:, :, 0:64], pb[:].rearrange("p (h c) -> p h c", c=64),
                   r=[pr], w=[f"VA{tile % 2}"])
                dma("pool", Vf_v[:, :, tile, :], va[:], r=[f"VA{tile % 2}"], w=["V_scr"])
                return
            c0, c1 = (1664, 1792) if window else (1536, 1664)
            for kc in range(8):
                mm(pb[:, 0:128], hT[:, kc, sub * 128:(sub + 1) * 128], WK[:, kc, c0:c1], kc == 0, kc == 7,
                   r=["WK", rh], w=[pr])
            va = VAn[tile % 2]
            cp("act" if sub % 2 == 0 else "dve", va[:, 0:2, 0:64], pb[:, 0:128].rearrange("p (h c) -> p h c", c=64),
               r=[pr], w=[f"VAn{tile % 2}"])
            dma("pool", (Vw_v if window else Vs_v)[:, :, tile, :], va[:, 0:2, :], r=[f"VAn{tile % 2}"], w=["V_scr"])

        vlist = [(sub, False) for sub in range(4)] if window else \
                [(sub, fox) for sub in range(4) for fox in (True, False)]
        npairs = len(pairs)
        pj = {0: proj(pairs[0])}
        st = {0: chainA(pairs[0], *pj[0])}
        if npairs > 1:
            pj[1] = proj(pairs[1])
        for n in range(npairs):
            if n + 2 < npairs:
                pj[n + 2] = proj(pairs[n + 2])
            if n + 1 < npairs:
                st[n + 1] = chainA(pairs[n + 1], *pj[n + 1])
            if vlist:
                vgroup(*vlist.pop(0))
            chainB(pairs[n], st[n])
        while vlist:
            vgroup(*vlist.pop(0))
        if window:
            return
        pb = ps[6]
        for sub in range(4):
            for kc in range(8):
                mm(pb[:, sub * 8:(sub + 1) * 8], hT[:, kc, sub * 128:(sub + 1) * 128], WK[:, kc, 1792:1800],
                   kc == 0, kc == 7, r=["WK", rh], w=["ps6"])
        tt("dve", zt[:].rearrange("p (s h) -> p s h", h=8), pb[:, 0:32].rearrange("p (s h) -> p s h", h=8),
           bfb[:].unsqueeze(1).to_broadcast([128, 4, 8]), ALU.add, r=["ps6", "bfb"], w=["zt"])
        act(zt[:], zt[:], AF.Exp, r=["zt"], w=["zt"], scale=-1.0)
        ts("dve", zt[:], zt[:], 1.0, None, ALU.add, r=["zt"], w=["zt"])
        act(zt[:], zt[:], AF.Ln, r=["zt"], w=["zt"])
        ts("dve", LF[:, G * 32:(G + 1) * 32], zt[:], -1.0, None, ALU.mult, r=["zt"], w=["LF"])

    TOT3 = TOT[:].rearrange("p (t h) -> p t h", h=8)
    INCL3 = INCL[:].rearrange("p (t h) -> p t h", h=8)
    CKW3 = CKW[:].rearrange("p (t h) -> p t h", h=8)

    def piece_cumsum():
        mm(ps[0][:], utri[:], LF[:], True, True, r=["utri", "LF"], w=["ps0"])
        mm(ps[1][:], onesm[:], LF[:], True, True, r=["onesm", "LF"], w=["ps1"])
        cp("act", CKW[:], ps[0][:], r=["ps0"], w=["CKW"])
        cp("dve", TOT[:], ps[1][:], r=["ps1"], w=["TOT"])
        for h in range(8):
            S.op("dve", lambda e, h=h: e.tensor_tensor_scan(out=INCL3[:, :, h], data0=onesm[:, 0:64], data1=TOT3[:, :, h],
                                                            initial=0.0, op0=ALU.mult, op1=ALU.add),
                 r=["TOT", "onesm"], w=["INCL"])
        tt("dve", INCL[:], INCL[:], TOT[:], ALU.subtract, r=["INCL", "TOT"], w=["INCL"])
        tt("dve", CKW[:], CKW[:], INCL[:], ALU.add, r=["CKW", "INCL"], w=["CKW"])
        for s in range(4):
            tt("dve", prod[:].rearrange("p (t h) -> p t h", h=8), INCL3,
               oh[:, s, :].unsqueeze(2).to_broadcast([128, 64, 8]), ALU.mult, r=["INCL", "oh"], w=["prod"])
            S.op("dve", lambda e, s=s: e.tensor_reduce(out=RS[:, s * 8:(s + 1) * 8],
                                                       in_=prod[:].rearrange("p (t h) -> p h t", h=8),
                                                       axis=AX.X, op=ALU.add), r=["prod"], w=["RS"])
            for h in range(8):
                o = BKB[:, (s * 8 + h) * 64:(s * 8 + h + 1) * 64]
                stt(o, CKW3[:, :, h], -1.0, badd[:, s, :], ALU.mult, ALU.add, r=["CKW", "badd"], w=["BKB"])
                ts("dve", o, o, RS[:, s * 8 + h:s * 8 + h + 1], None, ALU.add, r=["BKB", "RS"], w=["BKB"])

    GC = 1.5957691216057308

    def piece_w(kv):
        w1d = w1k_d if kv == 0 else w1v_d
        w2d = w2k_d if kv == 0 else w2v_d
        w1v = w1d.rearrange("(j d) n -> d j n", d=64)
        for half in range(2):
            for jq in range(4):
                dma("pool", W1[64 * half:64 * half + 64, jq * 8:(jq + 1) * 8, :], w1v[:, jq * 8:(jq + 1) * 8, :], w=["W1"])
        dma("pool", W2[:], w2d.rearrange("(hc p) n -> p hc n", p=128), w=["W2"])
        dma("pool", posT[:], (posk_d if kv == 0 else posv_d)[:], w=["posT"])
        for hc in range(2):
            for jj in range(32):
                mm(ps[0][:, hc:hc + 1], W1[0:64, jj, hc * 128:(hc + 1) * 128], posT[0:64, jj:jj + 1], jj == 0, jj == 31,
                   r=["W1", "posT"], w=["ps0"])
        cp("dve", posb[:], ps[0][:, 0:2], r=["ps0"], w=["posb"])

    def piece_mlp(kv, g, hc):
        TC = TCk if kv == 0 else TCv
        TC3 = TC[:].rearrange("p (n s) -> p n s", s=16)
        b0 = 64 * g
        pb, pr = ps[1], "ps1"
        for jj in range(32):
            rhs = TC3[b0:b0 + 64, 0:511, jj] if jj < 16 else TC3[b0:b0 + 64, 1:512, jj - 16]
            mm(pb[:, 0:511], W1[b0:b0 + 64, jj, hc * 128:(hc + 1) * 128], rhs, jj == 0, jj == 31, r=["W1", "TC"], w=[pr])
        act(Ucm[:, 0:511], pb[:, 0:511], AF.Identity, r=[pr, "posb"], w=["Ucm"], bias=posb[:, hc:hc + 1])
        tt("dve", Tcm[:, 0:511], Ucm[:, 0:511], Ucm[:, 0:511], ALU.mult, r=["Ucm"], w=["Tcm"])
        ts("dve", Tcm[:, 0:511], Tcm[:, 0:511], 0.044715, 1.0, ALU.mult, ALU.add, r=["Tcm"], w=["Tcm"])
        tt("dve", Tcm[:, 0:511], Tcm[:, 0:511], Ucm[:, 0:511], ALU.mult, r=["Tcm", "Ucm"], w=["Tcm"])
        act(Tcm[:, 0:511], Tcm[:, 0:511], AF.Exp, r=["Tcm"], w=["Tcm"], scale=-GC)
        ts("dve", Tcm[:, 0:511], Tcm[:, 0:511], 1.0, None, ALU.add, r=["Tcm"], w=["Tcm"])
        recip(Tcm[:, 0:511], Tcm[:, 0:511], r=["Tcm"], w=["Tcm"])
        tt("dve", HD[hc][:, 0:511], Ucm[:, 0:511], Tcm[:, 0:511], ALU.mult, r=["Tcm", "Ucm"], w=[f"HD{hc}"])
        if hc == 0:
            return
        if kv == 0:
            for h2 in range(2):
                mm(ps[0][0:64, 0:511], W2[:, h2, :], HD[h2][:, 0:511], h2 == 0, h2 == 1, r=["W2", f"HD{h2}"], w=["ps0"])
            qknorm(ps[0][0:64, 0:511], "ps0", 64, 511, gk2[0:64, 1:2], KC[g][0:64, 0:511], f"KC{g}")
        else:
            for cc in range(4):
                for h2 in range(2):
                    mm(ps[0][:, cc * 64:(cc + 1) * 64], HD[h2][:, cc * 128:(cc + 1) * 128], W2[:, h2, :], h2 == 0, h2 == 1,
                       r=["W2", f"HD{h2}"], w=["ps0"])
            cp("dve", VC[g][:, :, 0:64], ps[0][:, 0:256].rearrange("p (c d) -> p c d", d=64), r=["ps0"], w=[f"VC{g}"])

    pieces = [piece_cumsum]
    for kv in range(2):
        pieces.append(lambda kv=kv: piece_w(kv))
        for g in range(2):
            for hc in range(2):
                pieces.append(lambda kv=kv, g=g, hc=hc: piece_mlp(kv, g, hc))

    NG = len(groups)
    g_load(0)
    g_load(1)
    g_load(2)
    g_stageA(None, 0)
    g_stageA(0, 1)
    for k in range(NG):
        if k + 1 < NG:
            g_stageA(k + 1, k + 2 if k + 2 < NG else None)
        g_stageB(k)
        if k + 3 < NG:
            g_load(k + 3)
        if k >= 15 and pieces and stop_after >= 2:
            nrun = 2 if k in (15, 18, 21) else 1
            for _ in range(nrun):
                if pieces:
                    pieces.pop(0)()
    if stop_after >= 2:
        while pieces:
            pieces.pop(0)()
    if "dbgA" in debug and stop_after == 2:
        dma("sp", dbgA[:, 0:2048], BKB[:], r=["BKB"], w=["dbgA"])
        dma("sp", dbgA[:, 2048:2560], CKW[:], r=["CKW"], w=["dbgA"])
    if "dbgB" in debug and stop_after == 2:
        for g in range(2):
            cp("dve", Ucm[:, :], KC[g][:, :], r=[f"KC{g}"], w=["Ucm"])
            dma("sp", dbgB[:, g * 512:(g + 1) * 512], Ucm[:], r=["Ucm"], w=["dbgB"])
            cp("dve", Tcm[:, :], VC[g][:].rearrange("p c d -> p (c d)"), r=[f"VC{g}"], w=["Tcm"])
            dma("sp", dbgB[:, 1024 + g * 512:1024 + (g + 1) * 512], Tcm[:], r=["Tcm"], w=["dbgB"])
    S.barrier()
    A.release(m1)
    nrm_nbuf[0] = 2
    if stop_after <= 2:
        return finish()

    HQ = A.alloc("HQ", [128, 8, 2048], BF16)
    WQn = A.alloc("WQn", [128, 8, 536], BF16)
    dma("pool", WQn[:, :, 0:512], w_in_v[:, :, 1544:2056], w=["WQn"])
    dma("pool", WQn[:, :, 512:536], w_in_v[:, :, 2824:2848], w=["WQn"])
    SGHL = A.alloc("SGHL", [64, 2048], BF16)
    memset("pool", SGHL[:], 0.0, w=["SGHL"])
    QT = [A.alloc(f"QT{i}", [128, 2048], BF16) for i in range(4)]
    Pb = [A.alloc(f"P{i}", [128, 512], BF16) for i in range(4)]
    AM = A.alloc("AM", [128, 4, 128], BF16)
    DG = A.alloc("DG", [128, 4, 512], BF16)
    etmp = [A.alloc(f"etmp{i}", [128, 512], F32) for i in range(2)]
    et2b = [A.alloc(f"et2_{i}", [128, 512], F32) for i in range(2)]
    dma("pool", AM[:], C["c_am"][:], w=["AM"])
    dma("pool", DG[:], C["c_dg"][:], w=["DG"])
    for i in range(4):
        memset("pool", QT[i][64:128, :], 0.0, w=[f"QT{i}"])
    mF = A.mark()
    AQH = A.alloc("AQH", [8, 2048], BF16)
    AQL = A.alloc("AQL", [8, 2048], BF16)
    WQf = A.alloc("WQf", [128, 8, 520], BF16)
    dma("pool", WQf[:, :, 0:512], w_in_v[:, :, 0:512], w=["WQf"])
    dma("pool", WQf[:, :, 512:520], w_in_v[:, :, 1536:1544], w=["WQf"])
    mT = A.mark()
    nbfc = A.alloc("nbfc", [128, 1], F32)
    dma("sp", nbfc[:], nbfc_d[:], w=["nbfc"])
    AQ = A.alloc("AQ", [8, 2048], F32)
    onesf = A.alloc("onesf", [128, 512], F32)
    memset("pool", onesf[:], 1.0, w=["onesf"])
    xq_t = [A.alloc(f"xq_t{i}", [128, 4, D], F32) for i in range(3)]
    junk = A.alloc("junk2", [128, D], BF16)
    ssq = [A.alloc(f"ssq{i}", [128, 4], F32) for i in range(3)]
    rsq = [A.alloc(f"rsq{i}", [128, 4], F32) for i in range(3)]
    zq = A.alloc("zq", [32, 512], F32)
    for s in range(3):
        dma("sp", xq_t[s][:], xq[s * 512:(s + 1) * 512, :].rearrange("(u p) d -> p u d", p=128), w=[f"xq_t{s}"])
    for s in range(4):
        i3 = s % 3
        norm_transpose(xq[s * 512:(s + 1) * 512, :], xq_t[i3], f"xq_t{i3}", HQ[:, :, s * 512:(s + 1) * 512], "HQ",
                       ssq[i3], rsq[i3], f"ssq{i3}", gmix, "gmix", None, load=False)
        if s == 0:
            dma("sp", xq_t[0][:], xq[3 * 512:4 * 512, :].rearrange("(u p) d -> p u d", p=128), w=["xq_t0"])
    for s in range(4):
        sl = slice(s * 512, (s + 1) * 512)
        pb = ps[6]
        for kc in range(8):
            mm(pb[0:8, :], WQf[:, kc, 512:520], HQ[:, kc, sl], kc == 0, kc == 7, r=["WQf", "HQ"], w=["ps6"])
        act(zq[0:8, :], pb[0:8, :], AF.Exp, r=["ps6", "nbfc"], w=["zq"], scale=-1.0, bias=nbfc[0:8, :])
        ts("dve", zq[0:8, :], zq[0:8, :], 1.0, None, ALU.add, r=["zq"], w=["zq"])
        act(zq[0:8, :], zq[0:8, :], AF.Ln, r=["zq"], w=["zq"])
        S.op("dve", lambda e, sl=sl: e.tensor_tensor_scan(out=AQ[0:8, sl], data0=onesf[0:8, :], data1=zq[0:8, :],
                                                          initial=0.0, op0=ALU.mult, op1=ALU.subtract),
             r=["zq", "onesf"], w=["AQ"])
        pb = ps[7]
        for kc in range(8):
            mm(pb[0:24, :], WQn[:, kc, 512:536], HQ[:, kc, sl], kc == 0, kc == 7, r=["WQn", "HQ"], w=["ps7"])
        act(zq[0:24, :], pb[0:24, :], AF.Exp, r=["ps7"], w=["zq"], scale=-1.0)
        ts("dve", zq[0:24, :], zq[0:24, :], 1.0, None, ALU.add, r=["zq"], w=["zq"])
        recip(zq[0:24, :], zq[0:24, :], r=["zq"], w=["zq"])
        cp("dve", SGHL[0:24, sl], zq[0:24, :], r=["zq"], w=["SGHL"])
        tt("dve", SGHL[32:56, sl], zq[0:24, :], SGHL[0:24, sl], ALU.subtract, r=["zq", "SGHL"], w=["SGHL"])
    cp("dve", AQH[:], AQ[:], r=["AQ"], w=["AQH"])
    tt("dve", AQL[:], AQ[:], AQH[:], ALU.subtract, r=["AQ", "AQH"], w=["AQL"])
    S.barrier()
    A.release(mT)

    tile_ctr = [0]
    slot_ctr = [0]

    def attn(qt, qres, tiles, oi, after_exps=None):
        n = len(tiles)
        base = tile_ctr[0]
        tile_ctr[0] += n

        def qk(i):
            t = tiles[i]
            b = (base + i) % 3
            ex = t["extras"]
            mm(ps[b][:], t["k"], qt, True, len(ex) == 0, r=t["kres"] + [qres], w=[f"ps{b}"])
            for ei, (l, rr, res) in enumerate(ex):
                mm(ps[b][:], l, rr, False, ei == len(ex) - 1, r=res, w=[f"ps{b}"])

        LA = 2
        for i in range(min(LA, n)):
            qk(i)
        for i in range(n):
            if i + LA < n:
                qk(i + LA)
            t = tiles[i]
            b = (base + i) % 3
            pi = (base + i) % 4
            act(Pb[pi][:], ps[b][:], AF.Exp, r=[f"ps{b}", t["bres"]], w=[f"P{pi}"], bias=t["bias"])
            mm(ps[oi][:], t["v"], Pb[pi][:], i == 0, i == n - 1, r=[t["vres"], f"P{pi}"], w=[f"ps{oi}"])
        if after_exps is not None:
            after_exps([Pb[(base + i) % 4] for i in range(n)], [f"P{(base + i) % 4}" for i in range(n)])

    def qproj_pair(Wt, wres, c0, gcol, qa, ra, qb_, rb):
        for s in range(4):
            sl = slice(s * 512, (s + 1) * 512)
            for kc in range(8):
                mm(ps[6][:], Wt[:, kc, c0:c0 + 128], HQ[:, kc, sl], kc == 0, kc == 7, r=[wres, "HQ"], w=["ps6"])
            qknorm(ps[6][:], "ps6", 128, 512, gcol, None, None, qscale=True, psum_idx=7,
                   split=(qa[0:64, sl], ra, qb_[0:64, sl], rb))

    KB = [A.alloc(f"KB{i}", [128, S_LEN], BF16) for i in range(2)]
    VB = [A.alloc(f"VB{i}", [128, NT, 128], BF16) for i in range(2)]
    OST = [A.alloc(f"OST{i}", [128, 2048], BF16) for i in range(2)]
    for i in range(2):
        memset("pool", KB[i][64:128, :], 0.0, w=[f"KBa{i}"])
        dma("pool", KB[i][64:65, :], C["c_onerow"][:, :], r=[f"KBa{i}"], w=[f"KBa{i}"])
        dma("pool", KB[i][96:97, :], C["c_onerow"][:, :], r=[f"KBa{i}"], w=[f"KBa{i}"])
    def fox_loads(h):
        kb, vb = KB[h % 2], VB[h % 2]
        for qd in range(4):
            dma("sp", kb[0:64, qd * 2048:(qd + 1) * 2048], KT_fox[h, :, qd * 2048:(qd + 1) * 2048],
                r=["KT_scr"], w=[f"KB{h % 2}_{qd}"])
            dma("sp", vb[:, qd * 16:(qd + 1) * 16, :], V_fox[h, :, qd * 16:(qd + 1) * 16, :],
                r=["V_scr"], w=[f"VB{h % 2}_{qd}"])

    fox_loads(0)
    fox_loads(1)
    for i in range(4):
        q0i = 2 * (i % 2)
        qproj_pair(WQf, "WQf", i * 128, gk2[:, 2:3], QT[q0i], f"QT{q0i}", QT[q0i + 1], f"QT{q0i + 1}")
        for par in range(2):
            qt, qr = QT[q0i + par], f"QT{q0i + par}"
            h = 2 * i + par
            dma("pool", qt[64:65, :], AQH[h:h + 1, :], r=["AQH"], w=[qr])
            dma("pool", qt[96:97, :], AQL[h:h + 1, :], r=["AQL"], w=[qr])
            kb, vb = KB[h % 2], VB[h % 2]
            for s in range(4):
                sl = slice(s * 512, (s + 1) * 512)
                tiles = []
                for t in range(16 * s + 16):
                    ex = []
                    if t >= 16 * s:
                        m_, tl = (t - 16 * s) // 4, (t - 16 * s) % 4
                        ex = [(AM[:, m_, :], DG[:, tl, :], ["AM", "DG"])]
                    tiles.append(dict(k=kb[0:97, t * 128:(t + 1) * 128], kres=[f"KB{h % 2}_{t // 16}", f"KBa{h % 2}"], extras=ex,
                                      bias=BKB[:, (s * 8 + h) * 64 + t:(s * 8 + h) * 64 + t + 1], bres="BKB",
                                      v=vb[:, t, :], vres=f"VB{h % 2}_{t // 16}"))
                oi = 3 + slot_ctr[0] % 2
                et = etmp[slot_ctr[0] % 2]
                er = f"etmp{slot_ctr[0] % 2}"
                slot_ctr[0] += 1
                attn(qt[0:97, sl], qr, tiles, oi)
                recip(et[0:64, :], ps[oi][64:128, :], r=[f"ps{oi}"], w=[er])
                tt("dve", OST[i % 2][par * 64:(par + 1) * 64, sl], ps[oi][0:64, :], et[0:64, :], ALU.mult,
                   r=[f"ps{oi}", er], w=[f"OST{i % 2}"])
            if h + 2 < 8:
                fox_loads(h + 2)
        dma("pool", OA_scr[i], OST[i % 2][:], r=[f"OST{i % 2}"], w=["OA_scr"])
    S.barrier()
    A.release(mF)
    if stop_after <= 3:
        return finish()

    dma("sp", BKB[:], C["c_kbs"][:], w=["BKB"])
    OBT = A.alloc("OBT", [128, 4, 2048], BF16)
    NST = [A.alloc(f"NST{g}", [128, 4, 512], BF16) for g in range(2)]
    SELG = A.alloc("SELG", [64, 24, 128], BF16)
    OV = A.alloc("OV", [128, 4, 128], BF16)
    KBW = A.alloc("KBW", [128, 256], F32)
    KBC = A.alloc("KBC", [128, 128], F32)
    X16 = A.alloc("X16", [128, S_LEN], BF16)
    dma("pool", SELG[:], C["c_selg"][:].rearrange("p (a b) -> p a b", b=128), w=["SELG"])
    dma("pool", OV[:], C["c_ov"][:].rearrange("p (a b) -> p a b", b=128), w=["OV"])
    dma("sp", KBW[:], C["c_kbw"][:], w=["KBW"])
    dma("sp", KBC[:], C["c_kbc"][:], w=["KBC"])
    CMv = X16[:].rearrange("p (s c q) -> p s c q", s=4, c=4)
    cm_flat = C["c_cm"].rearrange("p s c q -> p (s c q)")
    for qd in range(4):
        dma("pool", X16[:, qd * 2048:(qd + 1) * 2048], cm_flat[:, qd * 2048:(qd + 1) * 2048], w=["X16"])

    def nsa_qpair(i):
        q0i = 2 * (i % 2)
        qproj_pair(WQn, "WQn", i * 128, gk2[:, 3:4], QT[q0i], f"QT{q0i}", QT[q0i + 1], f"QT{q0i + 1}")
        for par in range(2):
            h = 2 * i + par
            dma("pool", QT[q0i + par][64:65, :], C["c_qaug"][h, 0:1, :], w=[f"QT{q0i + par}"])
            dma("pool", QT[q0i + par][96:97, :], C["c_qaug"][h, 1:2, :], w=[f"QT{q0i + par}"])

    def nsa_epilogue(h, br, s, oi):
        i, par = h // 2, h % 2
        sl = slice(s * 512, (s + 1) * 512)
        et = etmp[slot_ctr[0] % 2]
        er = f"etmp{slot_ctr[0] % 2}"
        et2 = et2b[slot_ctr[0] % 2]
        e2r = f"et2_{slot_ctr[0] % 2}"
        lo, hi = par * 64, par * 64 + 64
        if br == 0:
            act(et[0:64, :], ps[oi][64:128, :], AF.Ln, r=[f"ps{oi}", "tinyb"], w=[er], bias=tinyb[0:64, :])
            act(et[0:64, :], et[0:64, :], AF.Exp, r=[er], w=[er], scale=-1.0)
        else:
            ts("dve", et[0:64, :], ps[oi][64:128, :], 1e-30, None, ALU.max, r=[f"ps{oi}"], w=[er])
            recip(et[0:64, :], et[0:64, :], r=[er], w=[er])
        tt("dve", et2[lo:hi, :], ps[oi][0:64, :], et[0:64, :], ALU.mult, r=[f"ps{oi}", er], w=[e2r])
        mm(ps[5][:], SELG[:, h * 3 + br, :], SGHL[0:64, sl], True, True, r=["SELG", "SGHL"], w=["ps5"])
        if br == 0:
            tt("dve", OBT[lo:hi, i, sl], et2[lo:hi, :], ps[5][lo:hi, :], ALU.mult, r=[e2r, "ps5"], w=["OBT"])
        else:
            tt("dve", et2[lo:hi, :], et2[lo:hi, :], ps[5][lo:hi, :], ALU.mult, r=[e2r, "ps5"], w=[e2r])
            tt("pool", OBT[lo:hi, i, sl], OBT[lo:hi, i, sl], et2[lo:hi, :], ALU.add, r=[e2r, "OBT"], w=["OBT"])

    mC = A.mark()
    IMP = A.alloc("IMP", [128, 4, 512], F32)
    ASEL = A.alloc("ASEL", [128, 2048], BF16)
    BSEL = A.alloc("BSEL", [128, 2048], BF16)
    SC = A.alloc("SC", [128, 512], F32)
    SC2 = A.alloc("SC2", [128, 128], F32)
    tmpU = A.alloc("tmpU", [128, 512], F32)
    l4 = A.alloc("l4", [128, 4], F32)
    m8 = A.alloc("m8", [128, 16], F32)
    nsel = [A.alloc(f"nsel{i}", [128, 128], BF16) for i in range(2)]
    dma("pool", ASEL[:], C["c_asel"][:], w=["ASEL"])
    dma("pool", BSEL[:], C["c_bsel"][:], w=["BSEL"])
    for i in range(4):
        g = i // 2
        nsa_qpair(i)
        for par in range(2):
            h = 2 * i + par
            first_of_g = (h % 4 == 0)
            qt, qr = QT[2 * (i % 2) + par], f"QT{2 * (i % 2) + par}"
            for s in range(4):
                sl = slice(s * 512, (s + 1) * 512)
                tiles = [dict(k=KC[g][0:97, c * 128:(c + 1) * 128], kres=[f"KC{g}"],
                              extras=[(ident_b[:], CMv[:, s, c, :], ["ident_b", "X16"])],
                              bias=KBC[:, (h * 4 + s) * 4 + c:(h * 4 + s) * 4 + c + 1], bres="KBC",
                              v=VC[g][:, c, :], vres=f"VC{g}") for c in range(4)]
                oi = 3 + slot_ctr[0] % 2

                def imp_fn(Ps, Pres, s=s, first_of_g=first_of_g):
                    for sub in range(4):
                        for c in range(4):
                            mm(ps[6][:, sub * 128:(sub + 1) * 128], Ps[c][:, sub * 128:(sub + 1) * 128], OV[:, c, :],
                               c == 0, c == 3, r=[Pres[c], "OV"], w=["ps6"])
                    U3 = ps[6][:].rearrange("p (a j) -> p a j", j=128)
                    S.op("dve", lambda e: e.tensor_reduce(out=l4[:], in_=U3, axis=AX.X, op=ALU.add), r=["ps6"], w=["l4"])
                    ts("dve", l4[:], l4[:], 1e-30, None, ALU.max, r=["l4"], w=["l4"])
                    recip(l4[:], l4[:], r=["l4"], w=["l4"])
                    rlb = l4[:].unsqueeze(2).to_broadcast([128, 4, 128])
                    I3 = IMP[:, s, :].rearrange("p (a j) -> p a j", j=128)
                    if first_of_g:
                        tt("dve", I3, U3, rlb, ALU.mult, r=["ps6", "l4"], w=["IMP"])
                    else:
                        tt("dve", tmpU[:].rearrange("p (a j) -> p a j", j=128), U3, rlb, ALU.mult, r=["ps6", "l4"], w=["tmpU"])
                        tt("pool", IMP[:, s, :], IMP[:, s, :], tmpU[:], ALU.add, r=["tmpU", "IMP"], w=["IMP"])

                attn(qt[0:97, sl], qr, tiles, oi, after_exps=imp_fn)
                nsa_epilogue(h, 0, s, oi)
                slot_ctr[0] += 1
        if i % 2 == 1:
            for s in range(4):
                tt("dve", SC[:], IMP[:, s, :], ASEL[:, s * 512:(s + 1) * 512], ALU.mult, r=["IMP", "ASEL"], w=["SC"])
                tt("dve", SC[:], SC[:], BSEL[:, s * 512:(s + 1) * 512], ALU.add, r=["SC", "BSEL"], w=["SC"])
                for sub in range(4):
                    scs = SC[:, sub * 128:(sub + 1) * 128]
                    ns = nsel[sub % 2]
                    S.op("dve", lambda e, scs=scs: e.max(out=m8[:, 0:8], in_=scs), r=["SC"], w=["m8"])
                    S.op("dve", lambda e, scs=scs: e.match_replace(out=SC2[:], in_to_replace=m8[:, 0:8], in_values=scs,
                                                                  imm_value=-1e9), r=["SC", "m8"], w=["SC2"])
                    S.op("dve", lambda e: e.max(out=m8[:, 8:16], in_=SC2[:]), r=["SC2"], w=["m8"])
                    ts("dve", ns[:], scs, m8[:, 15:16], NEGM, ALU.is_lt, ALU.mult, r=["SC", "m8"], w=[f"nsel{sub % 2}"])
                    tr(ps7b[:, sub * 128:(sub + 1) * 128], ns[:], ident_b[:], r=[f"nsel{sub % 2}", "ident_b"], w=["ps7"])
                cp("act", NST[g][:, s, :], ps7b[:, 0:512], r=["ps7"], w=[f"NST{g}"])
    if "dbgA" in debug and stop_after == 4:
        for g in range(2):
            for s in range(4):
                cp("dve", SC[:], NST[g][:, s, :], r=[f"NST{g}"], w=["SC"])
                dma("sp", dbgA[:, (g * 4 + s) * 512:(g * 4 + s + 1) * 512], SC[:], r=["SC"], w=["dbgA"])
    S.barrier()
    A.release(mC)
    if stop_after <= 4:
        if "dbgB" in debug:
            for i in range(4):
                cp("dve", etmp[0][:], OBT[:, i, 0:512], r=["OBT"], w=["etmp0"])
                dma("sp", dbgB[:, i * 512:(i + 1) * 512], etmp[0][:], r=["etmp0"], w=["dbgB"])
        return finish()

    for qd in range(4):
        dma("pool", X16[:, qd * 2048:(qd + 1) * 2048], C["c_eall"][:, qd * 2048:(qd + 1) * 2048], w=["X16"])
    WM = A.alloc("WM", [128, 8, 512], BF16)
    dma("pool", WM[:], C["c_wm"][:], w=["WM"])
    KS = A.alloc("KS", [128, S_LEN], BF16)
    VS = A.alloc("VS", [128, NT, 128], BF16)
    KW = A.alloc("KW", [128, 4096], BF16)
    VW = A.alloc("VW", [128, 32, 128], BF16)
    for (kbuf, n, nm) in [(KS, S_LEN, "KSa"), (KW, 4096, "KWa")]:
        memset("pool", kbuf[64:128, :], 0.0, w=[nm])
        dma("pool", kbuf[64:65, :], C["c_onerow"][:, 0:n], r=[nm], w=[nm])
        dma("pool", kbuf[96:97, :], C["c_onerow"][:, 0:n], r=[nm], w=[nm])
    for g in range(2):
        for qd in range(4):
            dma("sp", KS[0:64, qd * 2048:(qd + 1) * 2048], KT_s[g, :, qd * 2048:(qd + 1) * 2048], r=["KT_scr"], w=[f"KS_{qd}"])
            dma("sp", VS[:, qd * 16:(qd + 1) * 16, :], V_s[g, :, qd * 16:(qd + 1) * 16, :], r=["V_scr"], w=[f"VS_{qd}"])
        dma("sp", KW[0:64, :], KT_w[g], r=["KT_scr"], w=["KW"])
        dma("sp", VW[:], V_w[g], r=["V_scr"], w=["VW"])
        for ip in range(2):
            i = 2 * g + ip
            nsa_qpair(i)
            for par in range(2):
                h = 2 * i + par
                qt, qr = QT[2 * (i % 2) + par], f"QT{2 * (i % 2) + par}"
                for s in range(4):
                    sl = slice(s * 512, (s + 1) * 512)
                    tiles = []
                    for t in range(16 * s + 16):
                        ex = [(X16[:, t * 128:(t + 1) * 128], NST[g][:, s, :], ["X16", f"NST{g}"])]
                        if t >= 16 * s:
                            m_, tl = (t - 16 * s) // 4, (t - 16 * s) % 4
                            ex.append((AM[:, m_, :], DG[:, tl, :], ["AM", "DG"]))
                        bi = (h * 4 + s) * 64 + t
                        tiles.append(dict(k=KS[0:97, t * 128:(t + 1) * 128], kres=[f"KS_{t // 16}", "KSa"], extras=ex,
                                          bias=BKB[:, bi:bi + 1], bres="BKB", v=VS[:, t, :], vres=f"VS_{t // 16}"))
                    oi = 3 + slot_ctr[0] % 2
                    attn(qt[0:97, sl], qr, tiles, oi)
                    nsa_epilogue(h, 1, s, oi)
                    slot_ctr[0] += 1
                for s in range(4):
                    sl = slice(s * 512, (s + 1) * 512)
                    tiles = []
                    for t in range(8):
                        bi = (h * 4 + s) * 8 + t
                        tiles.append(dict(k=KW[0:97, (8 * s + t) * 128:(8 * s + t + 1) * 128], kres=["KW", "KWa"],
                                          extras=[(ident_b[:], WM[:, t, :], ["ident_b", "WM"])],
                                          bias=KBW[:, bi:bi + 1], bres="KBW", v=VW[:, 8 * s + t, :], vres="VW"))
                    oi = 3 + slot_ctr[0] % 2
                    attn(qt[0:97, sl], qr, tiles, oi)
                    nsa_epilogue(h, 2, s, oi)
                    slot_ctr[0] += 1
    if "dbgB" in debug and stop_after == 5:
        for i in range(4):
            for s in range(4):
                cp("dve", etmp[0][:], OBT[:, i, s * 512:(s + 1) * 512], r=["OBT"], w=["etmp0"])
                dma("sp", dbgB_big[:, (i * 4 + s) * 512:(i * 4 + s + 1) * 512], etmp[0][:], r=["etmp0"], w=["dbgB_big"])
    for i in range(4):
        dma("sp", OB_scr[i], OBT[:, i, :], r=["OBT"], w=["OB_scr"])
    S.barrier()
    A.release(m_base)
    if stop_after <= 5:
        return finish()

    ACC = A.alloc("ACC", [128, 16, D], F32)
    gffn = A.alloc("gffn", [128, 8], F32)
    dma("sp", gffn[:], gffn_d[:], w=["gffn"])
    m5 = A.mark()
    WA = A.alloc("WA", [128, 4, D], BF16)
    WB = A.alloc("WB", [128, 4, D], BF16)
    WO = A.alloc("WO", [128, 8, D], BF16)
    WG = A.alloc("WG", [128, 8, 2048], BF16)
    dma("pool", WA[:], wfu_d.rearrange("(c p) n -> p c n", p=128), w=["WA"])
    dma("pool", WB[:], wnu_d.rearrange("(c p) n -> p c n", p=128), w=["WB"])
    for qd in range(4):
        dma("pool", WG[:, :, qd * 512:(qd + 1) * 512], w_in_v[:, :, 2848 + qd * 512:2848 + (qd + 1) * 512], w=[f"WG{qd}"])
    dma("pool", WO[:], wout_d.rearrange("(c p) n -> p c n", p=128), w=["WO"])
    xt3 = A.alloc("xt3", [128, 4, D], F32)
    xraw = A.alloc("xraw", [128, 4, D], F32)
    junk = A.alloc("junk3", [128, D], BF16)
    ss3 = A.alloc("ss3", [128, 4], F32)
    rs3 = A.alloc("rs3", [128, 4], F32)
    HQs = A.alloc("HQs", [128, 8, 512], BF16)
    OAs = A.alloc("OAs", [128, 4, 512], BF16)
    OBs = A.alloc("OBs", [128, 4, 512], BF16)
    MIX = A.alloc("MIX", [128, 8, 512], BF16)
    sgt = [A.alloc(f"sgt{i}", [128, 512], F32) for i in range(2)]
    mxa = A.alloc("mxa", [128, 512], F32)
    for s in range(4):
        sl = slice(s * 512, (s + 1) * 512)
        dma("sp", xraw[:], xq[sl, :].rearrange("(u p) d -> p u d", p=128), w=["xraw"])
        dma("sp", OAs[:], OA_scr.rearrange("c p t -> p c t")[:, :, sl], r=["OA_scr"], w=["OAs"])
        dma("sp", OBs[:], OB_scr.rearrange("c p t -> p c t")[:, :, sl], r=["OB_scr"], w=["OBs"])
        norm_transpose(xq[sl, :], xt3, "xt3", HQs, "HQs", ss3, rs3, "ss3", gmix, "gmix", None)
        for m in range(8):
            for ab in range(2):
                c0 = ab * 1024 + m * 128
                wgr = f"WG{c0 // 512}"
                pg, pgr = ps[2 + 2 * ab], f"ps{2 + 2 * ab}"
                po, por = ps[3 + 2 * ab], f"ps{3 + 2 * ab}"
                for kc in range(8):
                    mm(pg[:], WG[:, kc, c0:c0 + 128], HQs[:, kc, :], kc == 0, kc == 7, r=[wgr, "HQs"], w=[pgr])
                Wx, wxr, Ox, oxr = (WA, "WA", OAs, "OAs") if ab == 0 else (WB, "WB", OBs, "OBs")
                for c in range(4):
                    mm(po[:], Wx[:, c, m * 128:(m + 1) * 128], Ox[:, c, :], c == 0, c == 3, r=[wxr, oxr], w=[por])
                act(sgt[ab][:], pg[:], AF.Sigmoid, r=[pgr], w=[f"sgt{ab}"])
                if ab == 0:
                    tt("dve", mxa[:], po[:], sgt[0][:], ALU.mult, r=[por, "sgt0"], w=["mxa"])
                else:
                    tt("dve", sgt[1][:], po[:], sgt[1][:], ALU.mult, r=[por, "sgt1"], w=["sgt1"])
                    tt("pool", MIX[:, m, :], mxa[:], sgt[1][:], ALU.add, r=["mxa", "sgt1"], w=["MIX"])
        for u in range(4):
            for hf in range(2):
                bi = 6 + (u * 2 + hf) % 2
                for kc in range(8):
                    mm(ps[bi][:], MIX[:, kc, u * 128:(u + 1) * 128], WO[:, kc, hf * 512:(hf + 1) * 512], kc == 0, kc == 7,
                       r=["MIX", "WO"], w=[f"ps{bi}"])
                tt("dve", ACC[:, s * 4 + u, hf * 512:(hf + 1) * 512], ps[bi][:], xraw[:, u, hf * 512:(hf + 1) * 512], ALU.add,
                   r=[f"ps{bi}", "xraw"], w=[f"ACC{s}"])
    S.barrier()
    A.release(m5)

    TT = A.alloc("TT", [128, 8, 2048], BF16)
    COMB = A.alloc("COMB", [128, 16, 16], F32)
    m6 = A.mark()
    WRf = A.alloc("WRf", [128, 8, 20], F32)
    WRh = A.alloc("WRh", [128, 8, 20], BF16)
    WRl = A.alloc("WRl", [128, 8, 20], BF16)
    brt = A.alloc("brt", [128, 20], F32)
    dma("sp", WRf[:], wr_d.rearrange("(c p) n -> p c n", p=128), w=["WRf"])
    dma("sp", brt[:], br_d[:], w=["brt"])
    cp("dve", WRh[:], WRf[:], r=["WRf"], w=["WRh"])
    tt("dve", WRl[:], WRf[:], WRh[:], ALU.subtract, r=["WRf", "WRh"], w=["WRl"])
    xn2 = [A.alloc(f"xn2_{i}", [128, 4, D], F32) for i in range(2)]
    junk = A.alloc("junk4", [128, D], BF16)
    ss4 = [A.alloc(f"ss4_{i}", [128, 4], F32) for i in range(2)]
    rs4 = [A.alloc(f"rs4_{i}", [128, 4], F32) for i in range(2)]
    tlo = [A.alloc(f"tlo{i}", [128, 8, 512], BF16) for i in range(2)]
    LALL = A.alloc("LALL", [128, 16, 20], F32)
    for s in range(4):
        sl = slice(s * 512, (s + 1) * 512)
        i2 = s % 2
        xn, ssx, rsx, tl = xn2[i2], ss4[i2], rs4[i2], tlo[i2]
        xr_, sr_, tr_ = f"xn2_{i2}", f"ss4_{i2}", f"tlo{i2}"
        for u in range(4):
            act(junk[:], ACC[:, s * 4 + u, :], AF.Square, r=[f"ACC{s}"], w=["junk", sr_], accum_out=ssx[:, u:u + 1])
        ts("dve", rsx[:], ssx[:], 1.0 / D, 1e-6, ALU.mult, ALU.add, r=[sr_], w=[sr_ + "r"])
        act(rsx[:], rsx[:], AF.Sqrt, r=[sr_ + "r"], w=[sr_ + "r"])
        recip(rsx[:], rsx[:], r=[sr_ + "r"], w=[sr_ + "r"])
        for u in range(4):
            ts("dve", xn[:, u, :], ACC[:, s * 4 + u, :], rsx[:, u:u + 1], None, ALU.mult,
               r=[f"ACC{s}", sr_ + "r"], w=[xr_])
        for kc in range(8):
            pb, pr = ps[kc % 2], f"ps{kc % 2}"
            for u in range(4):
                tr(pb[:, u * 128:(u + 1) * 128], xn[:, u, kc * 128:(kc + 1) * 128], ident_f[:], r=[xr_, "ident_f"], w=[pr])
            S.op("act", lambda e, o=TT[:, kc, sl], i=pb[:], sc=gffn[:, kc:kc + 1]:
                 e.activation(out=o, in_=i, func=AF.Copy, scale=sc), r=[pr, "gffn"], w=[f"TT{s}"])
            stt(tl[:, kc, :], pb[:], gffn[:, kc:kc + 1], TT[:, kc, sl], ALU.mult, ALU.subtract, r=[pr, "gffn", f"TT{s}"], w=[tr_])
        for u in range(4):
            tok = slice(s * 512 + u * 128, s * 512 + (u + 1) * 128)
            pl = ps[2 + u % 2]
            plr = f"ps{2 + u % 2}"
            k = 0
            for (lh, wr_, wrr) in [("TT", WRh, "WRh"), ("tlo", WRh, "WRh"), ("TT", WRl, "WRl")]:
                for kc in range(8):
                    lhsT = TT[:, kc, tok] if lh == "TT" else tl[:, kc, u * 128:(u + 1) * 128]
                    mm(pl[:, 0:20], lhsT, wr_[:, kc, :], k == 0, k == 23, r=[f"TT{s}" if lh == "TT" else tr_, wrr], w=[plr])
                    k += 1
            tt("dve", LALL[:, s * 4 + u, :], pl[:, 0:20], brt[:], ALU.add, r=[plr, "brt"], w=["LALL"])
    rr = ["RTB"]
    gl = LALL[:, :, 0:4]
    el4 = LALL[:, :, 4:20].rearrange("p a (g i) -> p a g i", i=4)
    def R(name, n):
        return A.alloc(name, [128, 16, n], F32)
    gmax, ohg, gsh, sumg, pgc = R("r_gmax", 1), R("r_ohg", 4), R("r_gsh", 4), R("r_sumg", 1), R("r_pg", 1)
    tmp16, ein, m1_, msk, e2, m2_ = R("r_tmp16", 16), R("r_ein", 4), R("r_m1", 1), R("r_msk", 4), R("r_e2", 4), R("r_m2", 1)
    esh, den, w4 = R("r_esh", 4), R("r_den", 1), R("r_w4", 4)
    def bc(ap1, n):
        return ap1.to_broadcast([128, 16, n])
    def red(out, in_, op):
        S.op("dve", lambda e: e.tensor_reduce(out=out, in_=in_, axis=AX.X, op=op), r=rr + ["LALL"], w=rr)
    red(gmax[:, :, 0], gl, ALU.max)
    tt("dve", ohg[:], gl, bc(gmax[:], 4), ALU.is_ge, r=rr + ["LALL"], w=rr)
    tt("dve", gsh[:], gl, bc(gmax[:], 4), ALU.subtract, r=rr + ["LALL"], w=rr)
    act(gsh[:], gsh[:], AF.Exp, r=rr, w=rr)
    red(sumg[:, :, 0], gsh[:], ALU.add)
    recip(pgc[:], sumg[:], r=rr, w=rr)
    t4 = tmp16[:].rearrange("p a (g i) -> p a g i", i=4)
    tt("dve", t4, el4, ohg[:].unsqueeze(3).to_broadcast([128, 16, 4, 4]), ALU.mult, r=rr + ["LALL"], w=rr)
    red(ein[:], tmp16[:].rearrange("p a (g i) -> p a i g", i=4), ALU.add)
    red(m1_[:, :, 0], ein[:], ALU.max)
    tt("dve", msk[:], ein[:], bc(m1_[:], 4), ALU.is_ge, r=rr, w=rr)
    stt(e2[:], msk[:], -1e30, ein[:], ALU.mult, ALU.add, r=rr, w=rr)
    red(m2_[:, :, 0], e2[:], ALU.max)
    tt("dve", msk[:], ein[:], bc(m2_[:], 4), ALU.is_ge, r=rr, w=rr)
    tt("dve", esh[:], ein[:], bc(m1_[:], 4), ALU.subtract, r=rr, w=rr)
    act(esh[:], esh[:], AF.Exp, r=rr, w=rr)
    tt("dve", esh[:], esh[:], msk[:], ALU.mult, r=rr, w=rr)
    red(den[:, :, 0], esh[:], ALU.add)
    recip(den[:], den[:], r=rr, w=rr)
    tt("dve", den[:], den[:], pgc[:], ALU.mult, r=rr, w=rr)
    tt("dve", w4[:], esh[:], bc(den[:], 4), ALU.mult, r=rr, w=rr)
    for g in range(4):
        tt("dve", COMB[:, :, 4 * g:4 * g + 4], w4[:], bc(ohg[:, :, g:g + 1], 4), ALU.mult, r=rr, w=["COMB"])
    if "dbgB_big" in debug and stop_after == 6:
        for u in range(8):
            dma("sp", dbgB_big[:, u * 1024:(u + 1) * 1024], ACC[:, u, :], r=[f"ACC{u // 4}"], w=["dbgB_big"])
        dma("sp", dbgA[:, 0:256], COMB[:].rearrange("p a b -> p (a b)"), r=["COMB"], w=["dbgA"])
    S.barrier()
    A.release(m6)
    if stop_after <= 6:
        return finish()

    WGb = [A.alloc(f"WGb{i}", [128, 8, 512], BF16) for i in range(2)]
    WUb = [A.alloc(f"WUb{i}", [128, 8, 512], BF16) for i in range(2)]
    WDb = [A.alloc(f"WDb{i}", [128, 4, D], BF16) for i in range(2)]
    hid = [A.alloc(f"hid{i}", [128, 512], BF16) for i in range(8)]
    sgm = [A.alloc(f"sgm{i}", [128, 512], F32) for i in range(2)]
    hctr = [0]
    for e_ in range(16):
        bi = e_ % 2
        wgv = wg_d[e_].rearrange("(c p) n -> p c n", p=128)
        wuv = wu_d[e_].rearrange("(c p) n -> p c n", p=128)
        wdv = wd_d[e_].rearrange("(c p) n -> p c n", p=128)
        for hlf in range(2):
            dma("pool", WGb[bi][:, hlf * 4:(hlf + 1) * 4, :], wgv[:, hlf * 4:(hlf + 1) * 4, :], w=[f"WGb{bi}"])
            dma("pool", WUb[bi][:, hlf * 4:(hlf + 1) * 4, :], wuv[:, hlf * 4:(hlf + 1) * 4, :], w=[f"WUb{bi}"])
            dma("pool", WDb[bi][:, hlf * 2:(hlf + 1) * 2, :], wdv[:, hlf * 2:(hlf + 1) * 2, :], w=[f"WDb{bi}"])
        for tg in range(4):
            tsl = slice(tg * 512, (tg + 1) * 512)
            hs = []
            for fc in range(4):
                pgi, pui = (fc % 2) * 2, (fc % 2) * 2 + 1
                for kc in range(8):
                    mm(ps[pgi][:], WGb[bi][:, kc, fc * 128:(fc + 1) * 128], TT[:, kc, tsl], kc == 0, kc == 7,
                       r=[f"WGb{bi}", f"TT{tg}"], w=[f"ps{pgi}"])
                for kc in range(8):
                    mm(ps[pui][:], WUb[bi][:, kc, fc * 128:(fc + 1) * 128], TT[:, kc, tsl], kc == 0, kc == 7,
                       r=[f"WUb{bi}", f"TT{tg}"], w=[f"ps{pui}"])
                sg_ = sgm[fc % 2]
                hh_ = hid[hctr[0] % 8]
                hr = f"hid{hctr[0] % 8}"
                hctr[0] += 1
                act(sg_[:], ps[pgi][:], AF.Silu, r=[f"ps{pgi}"], w=[f"sgm{fc % 2}"])
                tt("dve", hh_[:], ps[pui][:], sg_[:], ALU.mult, r=[f"ps{pui}", f"sgm{fc % 2}"], w=[hr])
                hs.append((hh_, hr))
            for u in range(4):
                for hf in range(2):
                    yi = 4 + (u * 2 + hf) % 4
                    for fc in range(4):
                        mm(ps[yi][:], hs[fc][0][:, u * 128:(u + 1) * 128], WDb[bi][:, fc, hf * 512:(hf + 1) * 512],
                           fc == 0, fc == 3, r=[hs[fc][1], f"WDb{bi}"], w=[f"ps{yi}"])
                    a_ = ACC[:, tg * 4 + u, hf * 512:(hf + 1) * 512]
                    stt(a_, ps[yi][:], COMB[:, tg * 4 + u, e_:e_ + 1], a_, ALU.mult, ALU.add,
                        r=[f"ps{yi}", "COMB", f"ACC{tg}"], w=[f"ACC{tg}"])
    for u in range(16):
        dma("sp", out_d[u * 128:(u + 1) * 128, :], ACC[:, u, :], r=[f"ACC{u // 4}"], w=["out"])
    return finish()


def make_in_maps(inputs):
    f = np.float32
    g = lambda k: np.asarray(inputs[k], f)
    x = g("x")
    maps = []
    shared = {}
    shared["w_in"] = np.ascontiguousarray(g("w_in")[0])
    shared["gmix"] = np.ascontiguousarray(g("norm_mix_g")[0].reshape(8, 128).T)
    shared["gffn"] = np.ascontiguousarray(g("norm_ffn_g")[0].reshape(8, 128).T)
    shared["gk2"] = np.ascontiguousarray(np.stack([np.tile(g("fox_k_g")[0], 2), np.tile(g("nsa_k_g")[0], 2),
                                                    np.tile(g("fox_q_g")[0], 2), np.tile(g("nsa_q_g")[0], 2)], 1))
    shared["bfb"] = np.ascontiguousarray(np.tile(g("b_forget")[0][None, :], (128, 1)))
    nb = np.zeros((128, 1), f)
    nb[:8, 0] = -g("b_forget")[0]
    shared["nbfc"] = nb
    shared["cmp_k_w1"] = np.ascontiguousarray(g("cmp_k_w1")[0])
    shared["cmp_v_w1"] = np.ascontiguousarray(g("cmp_v_w1")[0])
    shared["cmp_k_w2"] = np.ascontiguousarray(g("cmp_k_w2")[0])
    shared["cmp_v_w2"] = np.ascontiguousarray(g("cmp_v_w2")[0])
    shared["poskT"] = np.ascontiguousarray(np.tile(g("cmp_k_pos")[0].T, (2, 1)))
    shared["posvT"] = np.ascontiguousarray(np.tile(g("cmp_v_pos")[0].T, (2, 1)))
    shared["w_fox_up"] = np.ascontiguousarray(g("w_fox_up")[0])
    shared["w_nsa_up"] = np.ascontiguousarray(g("w_nsa_up")[0])
    shared["w_out"] = np.ascontiguousarray(g("w_out")[0])
    shared["w_rt"] = np.ascontiguousarray(np.concatenate([g("w_group")[0], g("w_router")[0]], 1))
    shared["b_rt"] = np.ascontiguousarray(np.tile(np.concatenate([g("b_group")[0], g("b_router")[0]])[None, :], (128, 1)))
    shared["w_gate"] = np.ascontiguousarray(g("w_gate")[0])
    shared["w_up"] = np.ascontiguousarray(g("w_up")[0])
    shared["w_down"] = np.ascontiguousarray(g("w_down")[0])
    consts = [host_consts(j) for j in range(4)]
    for c in range(8):
        b, j = c // 4, c % 4
        m = dict(shared)
        m["xb"] = np.ascontiguousarray(x[b])
        m["xq"] = np.ascontiguousarray(np.concatenate([x[b, 512 * (4 * s + j):512 * (4 * s + j + 1)] for s in range(4)], 0))
        xw = np.zeros((4096, D), f)
        for s in range(4):
            q0 = 512 * (4 * s + j)
            lo = q0 - 512
            if lo >= 0:
                xw[1024 * s:1024 * (s + 1)] = x[b, lo:lo + 1024]
            else:
                xw[1024 * s + 512:1024 * (s + 1)] = x[b, 0:512]
        m["xw"] = xw
        for k, v in consts[j].items():
            m[k] = np.ascontiguousarray(v.reshape(CONST_SHAPES[k]))
        maps.append(m)
    return maps


def kernel(**inputs):
    nc = build()
    maps = make_in_maps(inputs)
    res = run_bass_kernel_spmd(nc, maps, core_ids=list(range(8)))
    out = np.zeros((2, S_LEN, D), np.float32)
    for c in range(8):
        b, j = c // 4, c % 4
        o = res.results[c]["out"]
        for s in range(4):
            out[b, 512 * (4 * s + j):512 * (4 * s + j + 1)] = o[512 * s:512 * (s + 1)]
    return out
```

```python
import contextlib
import numpy as np
import concourse.bass as bass
import concourse.mybir as mybir
from concourse.bass_utils import run_bass_kernel_spmd

F32 = mybir.dt.float32
BF16 = mybir.dt.bfloat16
AF = mybir.ActivationFunctionType
ALU = mybir.AluOpType
AX = mybir.AxisListType

S_LEN = 8192
D = 1024
NT = 64
NEGM = -30000.0
STRICT_SAME_ENGINE = True
DIN = 4896


class _Op:
    __slots__ = ("id", "eng", "fn", "deps", "dma", "needs_inc", "sem", "val")

    def __init__(self, id, eng, fn, dma):
        self.id = id
        self.eng = eng
        self.fn = fn
        self.deps = []
        self.dma = dma
        self.needs_inc = False
        self.sem = None
        self.val = 0


class Sched:
    ENGS = ("pe", "act", "dve", "pool", "sp")
    NDMA = {"sp": 12, "pool": 8, "act": 2, "pe": 1, "dve": 1}

    def __init__(self, nc):
        self.nc = nc
        self.ops = {e: [] for e in self.ENGS}
        self.last_w = {}
        self.readers = {}
        self.n = 0
        self.dma_hist = {e: [] for e in self.ENGS}

    def op(self, eng, fn, r=(), w=(), dma=False):
        o = _Op(self.n, eng, fn, dma)
        self.n += 1
        deps = {}
        for res in r:
            lw = self.last_w.get(res)
            if lw is not None:
                deps[lw.id] = lw
        for res in w:
            lw = self.last_w.get(res)
            if lw is not None:
                deps[lw.id] = lw
            for rd in self.readers.get(res, ()):
                deps[rd.id] = rd
        if dma:
            hist = self.dma_hist[eng]
            n = self.NDMA[eng]
            if len(hist) >= n:
                p = hist[len(hist) - n]
                deps[p.id] = p
            hist.append(o)
        for d in deps.values():
            if d is o:
                continue
            if d.eng == eng and (eng == "pe" or not STRICT_SAME_ENGINE) and not d.dma and not dma:
                continue
            if not d.dma:
                d.needs_inc = True
            o.deps.append(d)
        for res in r:
            self.readers.setdefault(res, []).append(o)
        for res in w:
            self.last_w[res] = o
            self.readers[res] = []
        self.ops[eng].append(o)
        return o

    def barrier(self):
        lasts = []
        for e in self.ENGS:
            comp = [o for o in self.ops[e] if not o.dma and o.fn is not None]
            if comp:
                lasts.append(comp[-1])
            lasts.extend(self.dma_hist[e][-self.NDMA[e]:])
        for e in self.ENGS:
            o = _Op(self.n, e, None, False)
            self.n += 1
            for d in lasts:
                if d.eng == e and e == "pe" and not d.dma:
                    continue
                if not d.dma:
                    d.needs_inc = True
                o.deps.append(d)
            self.ops[e].append(o)
        self.last_w = {}
        self.readers = {}

    def emit(self):
        nc = self.nc
        with contextlib.ExitStack() as st:
            esem = {e: st.enter_context(nc.semaphore(f"s_{e}")) for e in self.ENGS}
            dsem = {e: [st.enter_context(nc.semaphore(f"d_{e}{i}")) for i in range(self.NDMA[e])]
                    for e in self.ENGS}
            for e in self.ENGS:
                cnt = 0
                dcnt = [0] * self.NDMA[e]
                k = 0
                for o in self.ops[e]:
                    if o.dma:
                        i = k % self.NDMA[e]
                        k += 1
                        dcnt[i] += 16
                        o.sem = dsem[e][i]
                        o.val = dcnt[i]
                    elif o.needs_inc:
                        cnt += 1
                        o.sem = esem[e]
                        o.val = cnt
            block = st.enter_context(nc.Block())

            def run(engobj, ops):
                waited = {}
                for o in ops:
                    need = {}
                    for d in o.deps:
                        key = id(d.sem)
                        if d.val > need.get(key, (0, None))[0]:
                            need[key] = (d.val, d.sem)
                    for key, (val, sem) in need.items():
                        if waited.get(key, 0) < val:
                            engobj.wait_ge(sem, val)
                            waited[key] = val
                    if o.fn is None:
                        continue
                    ins = o.fn(engobj)
                    if o.dma:
                        ins.then_inc(o.sem, 16)
                    elif o.needs_inc:
                        ins.then_inc(o.sem, 1)

            if self.ops["pe"]:
                @block.tensor
                def _(e):
                    run(e, self.ops["pe"])
            if self.ops["act"]:
                @block.scalar
                def _(e):
                    run(e, self.ops["act"])
            if self.ops["dve"]:
                @block.vector
                def _(e):
                    run(e, self.ops["dve"])
            if self.ops["pool"]:
                @block.gpsimd
                def _(e):
                    run(e, self.ops["pool"])
            if self.ops["sp"]:
                @block.sync
                def _(e):
                    run(e, self.ops["sp"])


class Arena:
    def __init__(self, nc, base=18560, limit=229376):
        self.nc = nc
        self.off = base
        self.limit = limit
        self.k = 0

    def alloc(self, name, shape, dt):
        nb = int(np.prod(shape[1:])) * (4 if dt == F32 else 2)
        nb = (nb + 63) // 64 * 64
        assert self.off + nb <= self.limit, f"SBUF overflow at {name}: {self.off}+{nb}"
        self.k += 1
        t = self.nc.alloc_sbuf_tensor_at(f"{name}_{self.k}", list(shape), dt, offset=self.off)
        self.off += nb
        self.hw = max(getattr(self, "hw", 0), self.off)
        return t.ap()

    def mark(self):
        return self.off

    def release(self, m):
        self.off = m


def host_consts(j):
    c = {}
    f = np.float32
    c["c_ident"] = np.eye(128, dtype=f)
    blk = np.zeros((128, 128), f)
    blk[:64, :64] = 1.0
    blk[64:, 64:] = 1.0
    c["c_blk"] = blk
    p = np.arange(128)
    c["c_utri"] = (p[:, None] <= p[None, :]).astype(f)
    c["c_ones"] = np.ones((128, 128), f)
    q0 = np.array([512 * (4 * s + j) for s in range(4)])
    t64 = np.arange(64)
    oh = np.zeros((128, 4, 64), f)
    badd = np.zeros((128, 4, 64), f)
    for s in range(4):
        oh[:, s, 4 * (4 * s + j)] = 1.0
        badd[:, s, 16 * s + 4 * (j + 1):16 * s + 16] = NEGM
    c["c_oh"] = oh
    c["c_badd"] = badd
    am = np.zeros((128, 4, 128), f)
    am[:, j, :] = np.eye(128, dtype=f)
    c["c_am"] = am
    ql = np.arange(512)
    dg = np.zeros((128, 4, 512), f)
    for tl in range(4):
        dg[:, tl, :] = np.where(128 * tl + p[:, None] <= ql[None, :], 0.0, NEGM)
    c["c_dg"] = dg
    wm = np.zeros((128, 8, 512), f)
    for t in range(8):
        dist = ql[None, :] - (128 * t + p[:, None]) + 512
        wm[:, t, :] = np.where((dist >= 0) & (dist < 512), 0.0, NEGM)
    c["c_wm"] = wm
    cm = np.zeros((128, 4, 4, 512), f)
    for s in range(4):
        for cc in range(4):
            n = 128 * cc + p[:, None]
            q = q0[s] + ql[None, :]
            cm[:, s, cc, :] = np.where((16 * n + 31 <= q) & (n < 511), 0.0, NEGM)
    c["c_cm"] = cm
    slopes = np.array([2.0 ** (-(h + 1)) for h in range(8)], np.float64)
    kbs = np.zeros((128, 8, 4, 64), f)
    kbw = np.zeros((128, 8, 4, 8), f)
    kbc = np.zeros((128, 8, 4, 4), f)
    for h in range(8):
        for s in range(4):
            kpos = 128 * t64[None, :] + p[:, None]
            kbs[:, h, s, :] = slopes[h] * (kpos - q0[s]) + badd[:, s, :]
            wpos = q0[s] - 512 + 128 * np.arange(8)[None, :] + p[:, None]
            kbw[:, h, s, :] = np.where(wpos >= 0, slopes[h] * (wpos - q0[s]), NEGM)
            cend = 16 * (128 * np.arange(4)[None, :] + p[:, None]) + 31
            kbc[:, h, s, :] = slopes[h] * (cend - q0[s])
    c["c_kbs"] = kbs.reshape(128, -1)
    c["c_kbw"] = kbw.reshape(128, -1)
    c["c_kbc"] = kbc.reshape(128, -1)
    asel = np.zeros((128, 4, 4, 128), f)
    bsel = np.zeros((128, 4, 4, 128), f)
    jb = np.arange(128)
    for s in range(4):
        for sub in range(4):
            q = q0[s] + 128 * sub + p[:, None]
            qb = q // 64
            future = 64 * jb[None, :] > q
            forced = (jb[None, :] == 0) | (jb[None, :] == qb) | (jb[None, :] == qb - 1)
            asel[:, s, sub, :] = np.where(future | forced, 0.0, 1.0)
            bsel[:, s, sub, :] = np.where(future, -1.0, np.where(forced, 1e4, 0.0))
    c["c_asel"] = asel.reshape(128, -1)
    c["c_bsel"] = bsel.reshape(128, -1)
    ov = np.zeros((128, 4, 128), f)
    for cc in range(4):
        for pp in range(128):
            n = 128 * cc + pp
            if n >= 511:
                continue
            a0, a1 = 16 * n, 16 * n + 32
            for b_ in range(a0 // 64, (a1 - 1) // 64 + 1):
                ovl = min(a1, 64 * b_ + 64) - max(a0, 64 * b_)
                ov[pp, cc, b_] = ovl / 32.0
    c["c_ov"] = ov.reshape(128, -1)
    e_all = np.zeros((128, S_LEN), f)
    for b_ in range(128):
        e_all[b_, 64 * b_:64 * b_ + 64] = 1.0
    c["c_eall"] = e_all
    c["c_onerow"] = np.ones((1, S_LEN), f)
    qaug = np.zeros((8, 2, 2048), f)
    qq = np.arange(2048) % 512
    for h in range(8):
        qaug[h, 0] = -slopes[h] * (qq % 256)
        qaug[h, 1] = -slopes[h] * 256 * (qq // 256)
    c["c_qaug"] = qaug
    selg = np.zeros((64, 24, 128), f)
    for hb in range(24):
        selg[hb, hb, :] = 1.0
        selg[32 + hb, hb, :] = 1.0
    c["c_selg"] = selg.reshape(64, -1)
    return c


CONST_SHAPES = {
    "c_ident": [128, 128], "c_blk": [128, 128], "c_utri": [128, 128], "c_ones": [128, 128],
    "c_oh": [128, 4, 64], "c_badd": [128, 4, 64], "c_am": [128, 4, 128], "c_dg": [128, 4, 512],
    "c_wm": [128, 8, 512], "c_cm": [128, 4, 4, 512], "c_kbs": [128, 2048], "c_kbw": [128, 256],
    "c_kbc": [128, 128], "c_asel": [128, 2048], "c_bsel": [128, 2048], "c_ov": [128, 512],
    "c_eall": [128, S_LEN], "c_onerow": [1, S_LEN], "c_qaug": [8, 2, 2048], "c_selg": [64, 24 * 128],
}


def build(debug=None, stop_after=99):
    nc = bass.Bass("TRN2", target_bir_lowering=False)
    S = Sched(nc)
    A = Arena(nc)
    debug = debug or set()

    def din(name, shape):
        return nc.dram_tensor(name, list(shape), F32, kind="ExternalInput").ap()

    def scratch(name, shape, dt):
        kind = "ExternalOutput" if name in debug else "Internal"
        return nc.dram_tensor(name, list(shape), dt, kind=kind).ap()

    xb = din("xb", [S_LEN, D])
    xq = din("xq", [2048, D])
    xw = din("xw", [4096, D])
    w_in = din("w_in", [D, DIN])
    C = {k: din(k, v) for k, v in CONST_SHAPES.items()}
    gmix_d = din("gmix", [128, 8])
    gffn_d = din("gffn", [128, 8])
    gk2_d = din("gk2", [128, 4])
    bfb_d = din("bfb", [128, 8])
    nbfc_d = din("nbfc", [128, 1])
    w1k_d = din("cmp_k_w1", [2048, 256])
    w1v_d = din("cmp_v_w1", [2048, 256])
    w2k_d = din("cmp_k_w2", [256, 64])
    w2v_d = din("cmp_v_w2", [256, 64])
    posk_d = din("poskT", [128, 32])
    posv_d = din("posvT", [128, 32])
    wfu_d = din("w_fox_up", [512, D])
    wnu_d = din("w_nsa_up", [512, D])
    wout_d = din("w_out", [D, D])
    wr_d = din("w_rt", [D, 20])
    br_d = din("b_rt", [128, 20])
    wg_d = din("w_gate", [16, D, 512])
    wu_d = din("w_up", [16, D, 512])
    wd_d = din("w_down", [16, 512, D])
    out_d = nc.dram_tensor("out", [2048, D], F32, kind="ExternalOutput").ap()

    KT_fox = scratch("KT_fox", [8, 64, S_LEN], BF16)
    V_fox = scratch("V_fox", [8, 128, NT, 128], BF16)
    KT_s = scratch("KT_s", [2, 64, S_LEN], BF16)
    KT_w = scratch("KT_w", [2, 64, 4096], BF16)
    V_s = scratch("V_s", [2, 128, NT, 128], BF16)
    V_w = scratch("V_w", [2, 128, 32, 128], BF16)
    OA_scr = scratch("OA_scr", [4, 128, 2048], BF16)
    OB_scr = scratch("OB_scr", [4, 128, 2048], BF16)
    dbgA = scratch("dbgA", [128, 4096], F32)
    dbgB = scratch("dbgB", [128, 4096], F32)
    dbgB_big = scratch("dbgB_big", [128, 8192], F32)

    ps = [nc.alloc_psum_tensor(f"ps{i}", [128, 512], F32).ap() for i in range(8)]
    ps7b = ps[7].bitcast(BF16)

    def dma(eng, out, in_, r=(), w=()):
        return S.op(eng, lambda e: e.dma_start(out=out, in_=in_), r=r, w=w, dma=True)

    def mm(out, lhsT, rhs, start, stop, r=(), w=()):
        return S.op("pe", lambda e: e.matmul(out, lhsT=lhsT, rhs=rhs, start=start, stop=stop,
                                             skip_group_check=True), r=r, w=w)

    def tr(out, in_, ident, r=(), w=()):
        return S.op("pe", lambda e: e.transpose(out=out, in_=in_, identity=ident), r=r, w=w)

    def act(out, in_, func, r=(), w=(), bias=None, scale=1.0, accum_out=None):
        def f(e):
            kw = {}
            if bias is not None:
                kw["bias"] = bias
            if accum_out is not None:
                kw["accum_out"] = accum_out
            return e.activation(out=out, in_=in_, func=func, scale=scale, **kw)
        return S.op("act", f, r=r, w=w)

    def ts(eng, out, in0, s1, s2, op0, op1=None, r=(), w=()):
        def f(e):
            if op1 is None:
                return e.tensor_scalar(out=out, in0=in0, scalar1=s1, scalar2=None, op0=op0)
            return e.tensor_scalar(out=out, in0=in0, scalar1=s1, scalar2=s2, op0=op0, op1=op1)
        return S.op(eng, f, r=r, w=w)

    def tt(eng, out, in0, in1, op, r=(), w=()):
        return S.op(eng, lambda e: e.tensor_tensor(out=out, in0=in0, in1=in1, op=op), r=r, w=w)

    def stt(out, in0, scalar, in1, op0, op1, r=(), w=()):
        return S.op("dve", lambda e: e.scalar_tensor_tensor(out=out, in0=in0, scalar=scalar, in1=in1,
                                                            op0=op0, op1=op1), r=r, w=w)

    def cp(eng, out, in_, r=(), w=()):
        if eng == "act":
            return act(out, in_, AF.Copy, r=r, w=w)
        return S.op(eng, lambda e: e.tensor_copy(out=out, in_=in_), r=r, w=w)

    def recip(out, in_, r=(), w=()):
        return S.op("dve", lambda e: e.reciprocal(out=out, in_=in_), r=r, w=w)

    def memset(eng, ap, val, w=()):
        return S.op(eng, lambda e: e.memset(ap, val), w=w)

    def finish():
        if debug:
            print("arena high water", A.hw - 18560, "bytes of", A.limit - 18560)
        S.barrier()
        S.emit()
        return nc

    ident_f = A.alloc("ident_f", [128, 128], F32)
    ident_b = A.alloc("ident_b", [128, 128], BF16)
    blk = A.alloc("blk", [128, 128], BF16)
    epsb = A.alloc("epsb", [128, 1], F32)
    lnq = A.alloc("lnq", [128, 1], F32)
    gmix = A.alloc("gmix", [128, 8], F32)
    gk2 = A.alloc("gk2", [128, 4], F32)
    dma("sp", ident_f[:], C["c_ident"][:], w=["ident_f"])
    dma("pool", ident_b[:], C["c_ident"][:], w=["ident_b"])
    dma("pool", blk[:], C["c_blk"][:], w=["blk"])
    dma("sp", gmix[:], gmix_d[:], w=["gmix"])
    dma("sp", gk2[:], gk2_d[:], w=["gk2"])
    memset("pool", epsb[:], 1e-6, w=["epsb"])
    memset("pool", lnq[:], float(np.log(0.125)), w=["lnq"])
    tinyb = A.alloc("tinyb", [128, 1], F32)
    memset("pool", tinyb[:], 1e-30, w=["tinyb"])

    m_base = A.mark()
    KC = [A.alloc(f"KC{g}", [128, 512], BF16) for g in range(2)]
    VC = [A.alloc(f"VC{g}", [128, 4, 128], BF16) for g in range(2)]
    BKB = A.alloc("BKB", [128, 2048], F32)

    krawb = [A.alloc(f"krawb{i}", [128, 512], BF16) for i in range(2)]
    ksq = [A.alloc(f"ksq{i}", [128, 512], BF16) for i in range(2)]
    klv = [A.alloc(f"klv{i}", [128, 512], F32) for i in range(2)]
    nrm_nbuf = [2]
    nrm_ctr = [0]

    def qknorm(pb, pr, P, N, gcol, out_ap, out_res, qscale=False, psum_idx=5, split=None):
        i2 = nrm_ctr[0] % nrm_nbuf[0]
        nrm_ctr[0] += 1
        kr, kq, kl = krawb[i2], ksq[i2], klv[i2]
        cp("act", kr[:P, :N], pb, r=[pr], w=[f"kraw{i2}"])
        tt("dve", kq[:P, :N], kr[:P, :N], kr[:P, :N], ALU.mult, r=[f"kraw{i2}"], w=[f"ksq{i2}"])
        bi = psum_idx
        pb2 = ps[bi]
        mm(pb2[:P, :N], blk[:P, :P], kq[:P, :N], True, True, r=["blk", f"ksq{i2}"], w=[f"ps{bi}"])
        act(kl[:P, :N], pb2[:P, :N], AF.Ln, r=[f"ps{bi}", "epsb"], w=[f"klv{i2}"], bias=epsb[:P, :], scale=1.0 / 64)
        if qscale:
            act(kl[:P, :N], kl[:P, :N], AF.Exp, r=[f"klv{i2}", "lnq"], w=[f"klv{i2}"], scale=-0.5, bias=lnq[:P, :])
        else:
            act(kl[:P, :N], kl[:P, :N], AF.Exp, r=[f"klv{i2}"], w=[f"klv{i2}"], scale=-0.5)
        if split is not None:
            (oa, ra, ob, rb) = split
            stt(oa, kr[0:64, :N], gcol[0:64, :], kl[0:64, :N], ALU.mult, ALU.mult, r=[f"kraw{i2}", f"klv{i2}", "gk2"], w=[ra])
            stt(ob, kr[64:128, :N], gcol[64:128, :], kl[64:128, :N], ALU.mult, ALU.mult, r=[f"kraw{i2}", f"klv{i2}", "gk2"], w=[rb])
            return
        stt(out_ap, kr[:P, :N], gcol, kl[:P, :N], ALU.mult, ALU.mult, r=[f"kraw{i2}", f"klv{i2}", "gk2"], w=[out_res])

    def qknormA(pb, pr, ssbank):
        i2 = nrm_ctr[0] % nrm_nbuf[0]
        nrm_ctr[0] += 1
        kr, kq = krawb[i2], ksq[i2]
        cp("act", kr[:, :], pb, r=[pr], w=[f"kraw{i2}"])
        tt("dve", kq[:, :], kr[:, :], kr[:, :], ALU.mult, r=[f"kraw{i2}"], w=[f"ksq{i2}"])
        mm(ps[ssbank][:, :], blk[:, :], kq[:, :], True, True, r=["blk", f"ksq{i2}"], w=[f"ps{ssbank}"])
        return i2, ssbank

    def qknormB(st, gcol, out_ap, out_res):
        i2, ssbank = st
        kr, kl = krawb[i2], klv[i2]
        act(kl[:, :], ps[ssbank][:, :], AF.Ln, r=[f"ps{ssbank}", "epsb"], w=[f"klv{i2}"], bias=epsb[:, :], scale=1.0 / 64)
        act(kl[:, :], kl[:, :], AF.Exp, r=[f"klv{i2}"], w=[f"klv{i2}"], scale=-0.5)
        stt(out_ap, kr[:, :], gcol, kl[:, :], ALU.mult, ALU.mult, r=[f"kraw{i2}", f"klv{i2}", "gk2"], w=[out_res])

    def norm_scale(xt, rx, ss, rs, rss):
        for sub in range(4):
            act(junk[:], xt[:, sub, :], AF.Square, r=[rx], w=["junk", rss], accum_out=ss[:, sub:sub + 1])
        act(rs[:], ss[:], AF.Ln, r=[rss, "epsb"], w=[rss + "r"], bias=epsb[:, :], scale=1.0 / D)
        act(rs[:], rs[:], AF.Exp, r=[rss + "r"], w=[rss + "r"], scale=-0.5)
        for sub in range(4):
            ts("dve", xt[:, sub, :], xt[:, sub, :], rs[:, sub:sub + 1], None, ALU.mult, r=[rx, rss + "r"], w=[rx])

    def transpose_gain(xt, rx, hT_dst, rh, gvec, gres):
        for kc in range(8):
            pb = ps[kc % 2]
            pr = f"ps{kc % 2}"
            for sub in range(4):
                tr(pb[:, sub * 128:(sub + 1) * 128], xt[:, sub, kc * 128:(kc + 1) * 128], ident_f[:],
                   r=[rx, "ident_f"], w=[pr])
            if kc % 2 == 0:
                S.op("act", lambda e, o=hT_dst[:, kc, :], i=pb[:], s=gvec[:, kc:kc + 1]:
                     e.activation(out=o, in_=i, func=AF.Copy, scale=s), r=[pr, gres], w=[rh])
            else:
                ts("dve", hT_dst[:, kc, :], pb[:], gvec[:, kc:kc + 1], None, ALU.mult, r=[pr, gres], w=[rh])

    def norm_transpose(src_rows, xt, rx, hT_dst, rh, ss, rs, rss, gvec, gres, evac_ctr, load=True):
        if load:
            dma("sp", xt[:], src_rows.rearrange("(s p) d -> p s d", p=128), w=[rx])
        norm_scale(xt, rx, ss, rs, rss)
        transpose_gain(xt, rx, hT_dst, rh, gvec, gres)

    m1 = A.mark()
    LF = A.alloc("LF", [128, NT * 8], F32)
    krawb.append(A.alloc("krawb2", [128, 512], BF16))
    ksq.append(A.alloc("ksq2", [128, 512], BF16))
    klv.append(A.alloc("klv2", [128, 512], F32))
    nrm_nbuf[0] = 3
    TCk = A.alloc("TCk", [128, S_LEN], BF16)
    TCv = A.alloc("TCv", [128, S_LEN], BF16)
    WK = A.alloc("WK", [128, 8, 1800], BF16)
    w_in_v = w_in.rearrange("(kc p) n -> p kc n", p=128)
    wk_cols = [(512, 1024, 0), (2312, 2440, 512), (2568, 2696, 640), (2056, 2184, 768), (2184, 2312, 896),
               (1024, 1536, 1024), (2440, 2568, 1536), (2696, 2824, 1664), (1536, 1544, 1792)]
    for (c0, c1, d0) in wk_cols:
        dma("pool", WK[:, :, d0:d0 + (c1 - c0)], w_in_v[:, :, c0:c1], w=["WK"])
    bfb = A.alloc("bfb", [128, 8], F32)
    dma("sp", bfb[:], bfb_d[:], w=["bfb"])
    xbuf = [A.alloc(f"xbuf{i}", [128, 4, D], F32) for i in range(3)]
    hbuf = [A.alloc(f"hbuf{i}", [128, 8, 512], BF16) for i in range(2)]
    junk = A.alloc("junk", [128, D], BF16)
    ssb = [A.alloc(f"ss{i}", [128, 4], F32) for i in range(3)]
    rsb = [A.alloc(f"rs{i}", [128, 4], F32) for i in range(3)]
    kn = [A.alloc(f"kn{i}", [128, 512], BF16) for i in range(3)]
    VA = [A.alloc(f"VA{i}", [128, 8, 128], BF16) for i in range(2)]
    VAn = [A.alloc(f"VAn{i}", [128, 2, 128], BF16) for i in range(2)]
    zt = A.alloc("zt", [128, 32], F32)
    for i in range(2):
        memset("pool", VA[i][:, :, 64:128], 1.0, w=[f"VA{i}"])
        memset("pool", VAn[i][:, :, 64:128], 1.0, w=[f"VAn{i}"])
    utri = A.alloc("utri", [128, 128], F32)
    onesm = A.alloc("onesm", [128, 128], F32)
    oh = A.alloc("oh", [128, 4, 64], F32)
    badd = A.alloc("badd", [128, 4, 64], F32)
    dma("sp", utri[:], C["c_utri"][:], w=["utri"])
    dma("sp", onesm[:], C["c_ones"][:], w=["onesm"])
    dma("sp", oh[:], C["c_oh"][:], w=["oh"])
    dma("sp", badd[:], C["c_badd"][:], w=["badd"])
    CKW = A.alloc("CKW", [128, 512], F32)
    TOT = A.alloc("TOT", [128, 512], F32)
    INCL = A.alloc("INCL", [128, 512], F32)
    RS = A.alloc("RS", [128, 32], F32)
    prod = A.alloc("prod", [128, 512], F32)
    W1 = A.alloc("W1", [128, 32, 256], BF16)
    W2 = A.alloc("W2", [128, 2, 64], BF16)
    posT = A.alloc("posT", [128, 32], BF16)
    posb = A.alloc("posb", [128, 2], F32)
    Ucm = A.alloc("Ucm", [128, 512], F32)
    Tcm = A.alloc("Tcm", [128, 512], F32)
    HD = [A.alloc(f"HD{i}", [128, 512], BF16) for i in range(2)]
    for g in range(2):
        memset("pool", KC[g][:], 0.0, w=[f"KC{g}"])
        dma("pool", KC[g][64:65, :], C["c_onerow"][:, 0:512], w=[f"KC{g}"])
        dma("pool", KC[g][96:97, :], C["c_onerow"][:, 0:512], w=[f"KC{g}"])
        memset("pool", VC[g][:], 0.0, w=[f"VC{g}"])
        memset("pool", VC[g][:, :, 64:128], 1.0, w=[f"VC{g}"])
    for i in range(2):
        memset("pool", HD[i][:], 0.0, w=[f"HD{i}"])

    KTf_rows = KT_fox.rearrange("h p t -> (h p) t")
    KTs_rows = KT_s.rearrange("g p t -> (g p) t")
    KTw_rows = KT_w.rearrange("g p t -> (g p) t")
    Vf_v = V_fox.rearrange("h p t c -> p h t c")
    Vs_v = V_s.rearrange("g p t c -> p g t c")
    Vw_v = V_w.rearrange("g p t c -> p g t c")

    npair = [0]
    nss = [0]
    nvg = [0]
    groups = [(xb[G * 512:(G + 1) * 512, :], G, False) for G in range(16)] + \
             [(xw[G * 512:(G + 1) * 512, :], G, True) for G in range(8)]

    def g_load(k):
        src, G, window = groups[k]
        xi = k % 3
        dma("sp", xbuf[xi][:], src.rearrange("(s p) d -> p s d", p=128), w=[f"xbuf{xi}"])

    def g_stageA(k_tr, k_sq):
        if k_tr is not None:
            xi, gi = k_tr % 3, k_tr % 2
            xt, rx, hT, rh = xbuf[xi], f"xbuf{xi}", hbuf[gi], f"hbuf{gi}"
        if k_sq is not None:
            xs = k_sq % 3
            xt2, rx2, ss2, rs2, rss2 = xbuf[xs], f"xbuf{xs}", ssb[xs], rsb[xs], f"ss{xs}"
        for kc in range(8):
            if k_tr is not None:
                pb = ps[kc % 2]
                pr = f"ps{kc % 2}"
                for sub in range(4):
                    tr(pb[:, sub * 128:(sub + 1) * 128], xt[:, sub, kc * 128:(kc + 1) * 128], ident_f[:],
                       r=[rx, "ident_f"], w=[pr])
                if kc % 2 == 0:
                    S.op("act", lambda e, o=hT[:, kc, :], i=pb[:], s_=gmix[:, kc:kc + 1]:
                         e.activation(out=o, in_=i, func=AF.Copy, scale=s_), r=[pr, "gmix"], w=[rh])
                else:
                    ts("dve", hT[:, kc, :], pb[:], gmix[:, kc:kc + 1], None, ALU.mult, r=[pr, "gmix"], w=[rh])
            if k_sq is not None and kc % 2 == 1:
                sub = kc // 2
                act(junk[:], xt2[:, sub, :], AF.Square, r=[rx2], w=["junk", rss2], accum_out=ss2[:, sub:sub + 1])
        deferred = []
        if k_sq is not None:
            act(rs2[:], ss2[:], AF.Ln, r=[rss2, "epsb"], w=[rss2 + "r"], bias=epsb[:, :], scale=1.0 / D)
            act(rs2[:], rs2[:], AF.Exp, r=[rss2 + "r"], w=[rss2 + "r"], scale=-0.5)
            for sub in range(4):
                deferred.append(lambda sub=sub: ts("dve", xt2[:, sub, :], xt2[:, sub, :], rs2[:, sub:sub + 1], None, ALU.mult,
                                                   r=[rx2, rss2 + "r"], w=[rx2]))
        return deferred

    def g_stageB(k, deferred=()):
        deferred = list(deferred)
        src, G, window = groups[k]
        gi = k % 2
        hT, rh = hbuf[gi], f"hbuf{gi}"
        pairs = [5] if window else [0, 1, 2, 3, 4, 6, 7]

        def proj(pi):
            c0 = pi * 128
            bi = 2 + npair[0] % 2
            npair[0] += 1
            pb, pr = ps[bi], f"ps{bi}"
            for kc in range(8):
                mm(pb[:], WK[:, kc, c0:c0 + 128], hT[:, kc, :], kc == 0, kc == 7, r=["WK", rh], w=[pr])
            return pb, pr

        def chainA(pi, pb, pr):
            if pi >= 6:
                dst = TCk if pi == 6 else TCv
                cp("act", dst[:, G * 512:(G + 1) * 512], pb[:], r=[pr], w=["TC"])
                return None
            ssbank = 5 if nss[0] % 2 == 0 else 7
            nss[0] += 1
            return qknormA(pb[:], pr, ssbank)

        def chainB(pi, st):
            if st is None:
                return
            kno = kn[npair[0] % 3]
            kres = f"kn{npair[0] % 3}"
            npair[0] += 0
            kno = kn[st[0]]
            kres = f"kn{st[0]}"
            gcol = gk2[:, 0:1] if pi < 4 else gk2[:, 1:2]
            qknormB(st, gcol, kno[:], kres)
            if pi < 4:
                dst = KTf_rows[pi * 128:(pi + 1) * 128, G * 512:(G + 1) * 512]
            elif pi == 4:
                dst = KTs_rows[:, G * 512:(G + 1) * 512]
            else:
                dst = KTw_rows[:, G * 512:(G + 1) * 512]
            dma("pool", dst, kno[:], r=[kres], w=["KT_scr"])

        def vgroup(sub, fox):
            tile = G * 4 + sub
            bi = 4 if nvg[0] % 2 == 0 else 6
            nvg[0] += 1
            pb, pr = ps[bi], f"ps{bi}"
            if fox:
                for kc in range(8):
                    mm(pb[:], hT[:, kc, sub * 128:(sub + 1) * 128], WK[:, kc, 1024:1536], kc == 0, kc == 7,
                       r=["WK", rh], w=[pr])
                va = VA[tile % 2]
                cp("dve" if sub % 2 == 0 else "act", va[:, :, 0:64], pb[:].rearrange("p (h c) -> p h c", c=64),
                   r=[pr], w=[f"VA{tile % 2}"])
                dma("pool", Vf_v[:, :, tile, :], va[:], r=[f"VA{tile % 2}"], w=["V_scr"])
                return
            c0, c1 = (1664, 1792) if window else (1536, 1664)
            for kc in range(8):
                mm(pb[:, 0:128], hT[:, kc, sub * 128:(sub + 1) * 128], WK[:, kc, c0:c1], kc == 0, kc == 7,
                   r=["WK", rh], w=[pr])
            va = VAn[tile % 2]
            cp("act" if sub % 2 == 0 else "dve", va[:, 0:2, 0:64], pb[:, 0:128].rearrange("p (h c) -> p h c", c=64),
               r=[pr], w=[f"VAn{tile % 2}"])
            dma("pool", (Vw_v if window else Vs_v)[:, :, tile, :], va[:, 0:2, :], r=[f"VAn{tile % 2}"], w=["V_scr"])

        vlist = [(sub, False) for sub in range(4)] if window else \
                [(sub, fox) for sub in range(4) for fox in (True, False)]
        npairs = len(pairs)
        pj = {0: proj(pairs[0])}
        st = {0: chainA(pairs[0], *pj[0])}
        if npairs > 1:
            pj[1] = proj(pairs[1])
        for n in range(npairs):
            if n + 2 < npairs:
                pj[n + 2] = proj(pairs[n + 2])
            if n + 1 < npairs:
                st[n + 1] = chainA(pairs[n + 1], *pj[n + 1])
            if vlist:
                vgroup(*vlist.pop(0))
            chainB(pairs[n], st[n])
            if deferred and n >= 1:
                deferred.pop(0)()
        while vlist:
            vgroup(*vlist.pop(0))
        while deferred:
            deferred.pop(0)()
        if window:
            return
        pb = ps[6]
        for sub in range(4):
            for kc in range(8):
                mm(pb[:, sub * 8:(sub + 1) * 8], hT[:, kc, sub * 128:(sub + 1) * 128], WK[:, kc, 1792:1800],
                   kc == 0, kc == 7, r=["WK", rh], w=["ps6"])
        tt("dve", zt[:].rearrange("p (s h) -> p s h", h=8), pb[:, 0:32].rearrange("p (s h) -> p s h", h=8),
           bfb[:].unsqueeze(1).to_broadcast([128, 4, 8]), ALU.add, r=["ps6", "bfb"], w=["zt"])
        act(zt[:], zt[:], AF.Exp, r=["zt"], w=["zt"], scale=-1.0)
        ts("dve", zt[:], zt[:], 1.0, None, ALU.add, r=["zt"], w=["zt"])
        act(zt[:], zt[:], AF.Ln, r=["zt"], w=["zt"])
        ts("dve", LF[:, G * 32:(G + 1) * 32], zt[:], -1.0, None, ALU.mult, r=["zt"], w=["LF"])

    TOT3 = TOT[:].rearrange("p (t h) -> p t h", h=8)
    INCL3 = INCL[:].rearrange("p (t h) -> p t h", h=8)
    CKW3 = CKW[:].rearrange("p (t h) -> p t h", h=8)

    def piece_cumsum():
        mm(ps[0][:], utri[:], LF[:], True, True, r=["utri", "LF"], w=["ps0"])
        mm(ps[1][:], onesm[:], LF[:], True, True, r=["onesm", "LF"], w=["ps1"])
        cp("act", CKW[:], ps[0][:], r=["ps0"], w=["CKW"])
        cp("dve", TOT[:], ps[1][:], r=["ps1"], w=["TOT"])
        for h in range(8):
            S.op("dve", lambda e, h=h: e.tensor_tensor_scan(out=INCL3[:, :, h], data0=onesm[:, 0:64], data1=TOT3[:, :, h],
                                                            initial=0.0, op0=ALU.mult, op1=ALU.add),
                 r=["TOT", "onesm"], w=["INCL"])
        tt("dve", INCL[:], INCL[:], TOT[:], ALU.subtract, r=["INCL", "TOT"], w=["INCL"])
        tt("dve", CKW[:], CKW[:], INCL[:], ALU.add, r=["CKW", "INCL"], w=["CKW"])

    def piece_bkb(s):
        tt("dve", prod[:].rearrange("p (t h) -> p t h", h=8), INCL3,
           oh[:, s, :].unsqueeze(2).to_broadcast([128, 64, 8]), ALU.mult, r=["INCL", "oh"], w=["prod"])
        S.op("dve", lambda e, s=s: e.tensor_reduce(out=RS[:, s * 8:(s + 1) * 8],
                                                   in_=prod[:].rearrange("p (t h) -> p h t", h=8),
                                                   axis=AX.X, op=ALU.add), r=["prod"], w=["RS"])
        o = BKB[:, s * 512:(s + 1) * 512].rearrange("p (h t) -> p h t", t=64)
        stt(o, CKW[:].rearrange("p (t h) -> p h t", h=8), -1.0, badd[:, s, :].unsqueeze(1).to_broadcast([128, 8, 64]),
            ALU.mult, ALU.add, r=["CKW", "badd"], w=["BKB"])
        tt("dve", o, o, RS[:, s * 8:(s + 1) * 8].unsqueeze(2).to_broadcast([128, 8, 64]), ALU.add, r=["BKB", "RS"], w=["BKB"])

    GC = 1.5957691216057308

    def piece_wload(kv):
        w1d = w1k_d if kv == 0 else w1v_d
        w2d = w2k_d if kv == 0 else w2v_d
        w1v = w1d.rearrange("(j d) n -> d j n", d=64)
        for half in range(2):
            for jq in range(4):
                dma("pool", W1[64 * half:64 * half + 64, jq * 8:(jq + 1) * 8, :], w1v[:, jq * 8:(jq + 1) * 8, :], w=["W1"])
        dma("pool", W2[:], w2d.rearrange("(hc p) n -> p hc n", p=128), w=["W2"])
        dma("pool", posT[:], (posk_d if kv == 0 else posv_d)[:], w=["posT"])

    def piece_posb(kv):
        for hc in range(2):
            for jj in range(32):
                mm(ps[0][:, hc:hc + 1], W1[0:64, jj, hc * 128:(hc + 1) * 128], posT[0:64, jj:jj + 1], jj == 0, jj == 31,
                   r=["W1", "posT"], w=["ps0"])
        cp("dve", posb[:], ps[0][:, 0:2], r=["ps0"], w=["posb"])

    def piece_mlp(kv, g, hc):
        TC = TCk if kv == 0 else TCv
        TC3 = TC[:].rearrange("p (n s) -> p n s", s=16)
        b0 = 64 * g
        pb, pr = ps[1], "ps1"
        for jj in range(32):
            rhs = TC3[b0:b0 + 64, 0:511, jj] if jj < 16 else TC3[b0:b0 + 64, 1:512, jj - 16]
            mm(pb[:, 0:511], W1[b0:b0 + 64, jj, hc * 128:(hc + 1) * 128], rhs, jj == 0, jj == 31, r=["W1", "TC"], w=[pr])
        act(Ucm[:, 0:511], pb[:, 0:511], AF.Identity, r=[pr, "posb"], w=["Ucm"], bias=posb[:, hc:hc + 1])
        tt("dve", Tcm[:, 0:511], Ucm[:, 0:511], Ucm[:, 0:511], ALU.mult, r=["Ucm"], w=["Tcm"])
        ts("dve", Tcm[:, 0:511], Tcm[:, 0:511], 0.044715, 1.0, ALU.mult, ALU.add, r=["Tcm"], w=["Tcm"])
        tt("dve", Tcm[:, 0:511], Tcm[:, 0:511], Ucm[:, 0:511], ALU.mult, r=["Tcm", "Ucm"], w=["Tcm"])
        act(Tcm[:, 0:511], Tcm[:, 0:511], AF.Exp, r=["Tcm"], w=["Tcm"], scale=-GC)
        ts("dve", Tcm[:, 0:511], Tcm[:, 0:511], 1.0, None, ALU.add, r=["Tcm"], w=["Tcm"])
        recip(Tcm[:, 0:511], Tcm[:, 0:511], r=["Tcm"], w=["Tcm"])
        tt("dve", HD[hc][:, 0:511], Ucm[:, 0:511], Tcm[:, 0:511], ALU.mult, r=["Tcm", "Ucm"], w=[f"HD{hc}"])
        if hc == 0:
            return
        if kv == 0:
            for h2 in range(2):
                mm(ps[0][0:64, 0:511], W2[:, h2, :], HD[h2][:, 0:511], h2 == 0, h2 == 1, r=["W2", f"HD{h2}"], w=["ps0"])
            qknorm(ps[0][0:64, 0:511], "ps0", 64, 511, gk2[0:64, 1:2], KC[g][0:64, 0:511], f"KC{g}")
        else:
            for cc in range(4):
                for h2 in range(2):
                    mm(ps[0][:, cc * 64:(cc + 1) * 64], HD[h2][:, cc * 128:(cc + 1) * 128], W2[:, h2, :], h2 == 0, h2 == 1,
                       r=["W2", f"HD{h2}"], w=["ps0"])
            cp("dve", VC[g][:, :, 0:64], ps[0][:, 0:256].rearrange("p (c d) -> p c d", d=64), r=["ps0"], w=[f"VC{g}"])

    sched1b = {
        15: [piece_cumsum, lambda: piece_posb(0)],
        16: [lambda: piece_mlp(0, 0, 0), lambda: piece_bkb(0), lambda: piece_bkb(1)],
        17: [lambda: piece_mlp(0, 0, 1), lambda: piece_bkb(2), lambda: piece_bkb(3)],
        18: [lambda: piece_mlp(0, 1, 0)],
        19: [lambda: piece_mlp(0, 1, 1), lambda: piece_wload(1)],
        21: [lambda: piece_posb(1), lambda: piece_mlp(1, 0, 0)],
        22: [lambda: piece_mlp(1, 0, 1)],
        23: [lambda: piece_mlp(1, 1, 0), lambda: piece_mlp(1, 1, 1)],
    }
    piece_wload(0)

    NG = len(groups)
    g_load(0)
    g_load(1)
    g_load(2)
    for f_ in g_stageA(None, 0):
        f_()
    for f_ in g_stageA(0, 1):
        f_()
    for k in range(NG):
        dfr = []
        if k + 1 < NG:
            dfr = g_stageA(k + 1, k + 2 if k + 2 < NG else None)
        g_stageB(k, dfr)
        if k + 3 < NG:
            g_load(k + 3)
        if stop_after >= 2:
            for pc in sched1b.get(k, []):
                pc()
    if "dbgA" in debug and stop_after == 2:
        dma("sp", dbgA[:, 0:2048], BKB[:], r=["BKB"], w=["dbgA"])
        dma("sp", dbgA[:, 2048:2560], CKW[:], r=["CKW"], w=["dbgA"])
    if "dbgB" in debug and stop_after == 2:
        for g in range(2):
            cp("dve", Ucm[:, :], KC[g][:, :], r=[f"KC{g}"], w=["Ucm"])
            dma("sp", dbgB[:, g * 512:(g + 1) * 512], Ucm[:], r=["Ucm"], w=["dbgB"])
            cp("dve", Tcm[:, :], VC[g][:].rearrange("p c d -> p (c d)"), r=[f"VC{g}"], w=["Tcm"])
            dma("sp", dbgB[:, 1024 + g * 512:1024 + (g + 1) * 512], Tcm[:], r=["Tcm"], w=["dbgB"])
    S.barrier()
    A.release(m1)
    nrm_nbuf[0] = 2
    if stop_after <= 2:
        return finish()

    HQ = A.alloc("HQ", [128, 8, 2048], BF16)
    WQn = A.alloc("WQn", [128, 8, 536], BF16)
    dma("pool", WQn[:, :, 0:512], w_in_v[:, :, 1544:2056], w=["WQn"])
    dma("pool", WQn[:, :, 512:536], w_in_v[:, :, 2824:2848], w=["WQn"])
    SGHL = A.alloc("SGHL", [64, 2048], BF16)
    memset("pool", SGHL[:], 0.0, w=["SGHL"])
    QT = [A.alloc(f"QT{i}", [128, 2048], BF16) for i in range(4)]
    Pb = [A.alloc(f"P{i}", [128, 512], BF16) for i in range(4)]
    AM = A.alloc("AM", [128, 4, 128], BF16)
    DG = A.alloc("DG", [128, 4, 512], BF16)
    etmp = [A.alloc(f"etmp{i}", [128, 512], F32) for i in range(2)]
    et2b = [A.alloc(f"et2_{i}", [128, 512], F32) for i in range(2)]
    dma("pool", AM[:], C["c_am"][:], w=["AM"])
    dma("pool", DG[:], C["c_dg"][:], w=["DG"])
    for i in range(4):
        memset("pool", QT[i][64:128, :], 0.0, w=[f"QT{i}"])
    mF = A.mark()
    AQH = A.alloc("AQH", [8, 2048], BF16)
    AQL = A.alloc("AQL", [8, 2048], BF16)
    WQf = A.alloc("WQf", [128, 8, 520], BF16)
    dma("pool", WQf[:, :, 0:512], w_in_v[:, :, 0:512], w=["WQf"])
    dma("pool", WQf[:, :, 512:520], w_in_v[:, :, 1536:1544], w=["WQf"])
    mT = A.mark()
    nbfc = A.alloc("nbfc", [128, 1], F32)
    dma("sp", nbfc[:], nbfc_d[:], w=["nbfc"])
    AQ = A.alloc("AQ", [8, 2048], F32)
    onesf = A.alloc("onesf", [128, 512], F32)
    memset("pool", onesf[:], 1.0, w=["onesf"])
    xq_t = [A.alloc(f"xq_t{i}", [128, 4, D], F32) for i in range(3)]
    junk = A.alloc("junk2", [128, D], BF16)
    ssq = [A.alloc(f"ssq{i}", [128, 4], F32) for i in range(3)]
    rsq = [A.alloc(f"rsq{i}", [128, 4], F32) for i in range(3)]
    zq = A.alloc("zq", [32, 512], F32)
    for s in range(3):
        dma("sp", xq_t[s][:], xq[s * 512:(s + 1) * 512, :].rearrange("(u p) d -> p u d", p=128), w=[f"xq_t{s}"])
    for s in range(4):
        i3 = s % 3
        norm_transpose(xq[s * 512:(s + 1) * 512, :], xq_t[i3], f"xq_t{i3}", HQ[:, :, s * 512:(s + 1) * 512], "HQ",
                       ssq[i3], rsq[i3], f"ssq{i3}", gmix, "gmix", None, load=False)
        if s == 0:
            dma("sp", xq_t[0][:], xq[3 * 512:4 * 512, :].rearrange("(u p) d -> p u d", p=128), w=["xq_t0"])
    for s in range(4):
        sl = slice(s * 512, (s + 1) * 512)
        pb = ps[6]
        for kc in range(8):
            mm(pb[0:8, :], WQf[:, kc, 512:520], HQ[:, kc, sl], kc == 0, kc == 7, r=["WQf", "HQ"], w=["ps6"])
        act(zq[0:8, :], pb[0:8, :], AF.Exp, r=["ps6", "nbfc"], w=["zq"], scale=-1.0, bias=nbfc[0:8, :])
        ts("dve", zq[0:8, :], zq[0:8, :], 1.0, None, ALU.add, r=["zq"], w=["zq"])
        act(zq[0:8, :], zq[0:8, :], AF.Ln, r=["zq"], w=["zq"])
        S.op("dve", lambda e, sl=sl: e.tensor_tensor_scan(out=AQ[0:8, sl], data0=onesf[0:8, :], data1=zq[0:8, :],
                                                          initial=0.0, op0=ALU.mult, op1=ALU.subtract),
             r=["zq", "onesf"], w=["AQ"])
        pb = ps[7]
        for kc in range(8):
            mm(pb[0:24, :], WQn[:, kc, 512:536], HQ[:, kc, sl], kc == 0, kc == 7, r=["WQn", "HQ"], w=["ps7"])
        act(zq[0:24, :], pb[0:24, :], AF.Exp, r=["ps7"], w=["zq"], scale=-1.0)
        ts("dve", zq[0:24, :], zq[0:24, :], 1.0, None, ALU.add, r=["zq"], w=["zq"])
        recip(zq[0:24, :], zq[0:24, :], r=["zq"], w=["zq"])
        cp("dve", SGHL[0:24, sl], zq[0:24, :], r=["zq"], w=["SGHL"])
        tt("dve", SGHL[32:56, sl], zq[0:24, :], SGHL[0:24, sl], ALU.subtract, r=["zq", "SGHL"], w=["SGHL"])
    cp("dve", AQH[:], AQ[:], r=["AQ"], w=["AQH"])
    tt("dve", AQL[:], AQ[:], AQH[:], ALU.subtract, r=["AQ", "AQH"], w=["AQL"])
    S.barrier()
    A.release(mT)

    tile_ctr = [0]
    slot_ctr = [0]

    def attn(qt, qres, tiles, oi, after_exps=None):
        n = len(tiles)
        base = tile_ctr[0]
        tile_ctr[0] += n

        def qk(i):
            t = tiles[i]
            b = (base + i) % 3
            ex = t["extras"]
            mm(ps[b][:], t["k"], qt, True, len(ex) == 0, r=t["kres"] + [qres], w=[f"ps{b}"])
            for ei, (l, rr, res) in enumerate(ex):
                mm(ps[b][:], l, rr, False, ei == len(ex) - 1, r=res, w=[f"ps{b}"])

        LA = 2
        for i in range(min(LA, n)):
            qk(i)
        for i in range(n):
            if i + LA < n:
                qk(i + LA)
            t = tiles[i]
            b = (base + i) % 3
            pi = (base + i) % 4
            act(Pb[pi][:], ps[b][:], AF.Exp, r=[f"ps{b}", t["bres"]], w=[f"P{pi}"], bias=t["bias"])
            mm(ps[oi][:], t["v"], Pb[pi][:], i == 0, i == n - 1, r=[t["vres"], f"P{pi}"], w=[f"ps{oi}"])
        if after_exps is not None:
            after_exps([Pb[(base + i) % 4] for i in range(n)], [f"P{(base + i) % 4}" for i in range(n)])

    def qproj_pair(Wt, wres, c0, gcol, qa, ra, qb_, rb):
        for s in range(4):
            sl = slice(s * 512, (s + 1) * 512)
            for kc in range(8):
                mm(ps[6][:], Wt[:, kc, c0:c0 + 128], HQ[:, kc, sl], kc == 0, kc == 7, r=[wres, "HQ"], w=["ps6"])
            qknorm(ps[6][:], "ps6", 128, 512, gcol, None, None, qscale=True, psum_idx=7,
                   split=(qa[0:64, sl], ra, qb_[0:64, sl], rb))

    KB = [A.alloc(f"KB{i}", [128, S_LEN], BF16) for i in range(2)]
    VB = [A.alloc(f"VB{i}", [128, NT, 128], BF16) for i in range(2)]
    OST = [A.alloc(f"OST{i}", [128, 2048], BF16) for i in range(2)]
    for i in range(2):
        memset("pool", KB[i][64:128, :], 0.0, w=[f"KBa{i}"])
        dma("pool", KB[i][64:65, :], C["c_onerow"][:, :], r=[f"KBa{i}"], w=[f"KBa{i}"])
        dma("pool", KB[i][96:97, :], C["c_onerow"][:, :], r=[f"KBa{i}"], w=[f"KBa{i}"])
    def fox_loads(h):
        kb, vb = KB[h % 2], VB[h % 2]
        for qd in range(4):
            dma("sp", kb[0:64, qd * 2048:(qd + 1) * 2048], KT_fox[h, :, qd * 2048:(qd + 1) * 2048],
                r=["KT_scr"], w=[f"KB{h % 2}_{qd}"])
            dma("sp", vb[:, qd * 16:(qd + 1) * 16, :], V_fox[h, :, qd * 16:(qd + 1) * 16, :],
                r=["V_scr"], w=[f"VB{h % 2}_{qd}"])

    fox_loads(0)
    fox_loads(1)
    for i in range(4):
        q0i = 2 * (i % 2)
        qproj_pair(WQf, "WQf", i * 128, gk2[:, 2:3], QT[q0i], f"QT{q0i}", QT[q0i + 1], f"QT{q0i + 1}")
        for par in range(2):
            qt, qr = QT[q0i + par], f"QT{q0i + par}"
            h = 2 * i + par
            dma("pool", qt[64:65, :], AQH[h:h + 1, :], r=["AQH"], w=[qr])
            dma("pool", qt[96:97, :], AQL[h:h + 1, :], r=["AQL"], w=[qr])
            kb, vb = KB[h % 2], VB[h % 2]
            for s in range(4):
                sl = slice(s * 512, (s + 1) * 512)
                tiles = []
                for t in range(16 * s + 16):
                    ex = []
                    if t >= 16 * s:
                        m_, tl = (t - 16 * s) // 4, (t - 16 * s) % 4
                        ex = [(AM[:, m_, :], DG[:, tl, :], ["AM", "DG"])]
                    tiles.append(dict(k=kb[0:97, t * 128:(t + 1) * 128], kres=[f"KB{h % 2}_{t // 16}", f"KBa{h % 2}"], extras=ex,
                                      bias=BKB[:, (s * 8 + h) * 64 + t:(s * 8 + h) * 64 + t + 1], bres="BKB",
                                      v=vb[:, t, :], vres=f"VB{h % 2}_{t // 16}"))
                oi = 3 + slot_ctr[0] % 2
                et = etmp[slot_ctr[0] % 2]
                er = f"etmp{slot_ctr[0] % 2}"
                slot_ctr[0] += 1
                attn(qt[0:97, sl], qr, tiles, oi)
                recip(et[0:64, :], ps[oi][64:128, :], r=[f"ps{oi}"], w=[er])
                tt("dve", OST[i % 2][par * 64:(par + 1) * 64, sl], ps[oi][0:64, :], et[0:64, :], ALU.mult,
                   r=[f"ps{oi}", er], w=[f"OST{i % 2}"])
            if h + 2 < 8:
                fox_loads(h + 2)
        dma("pool", OA_scr[i], OST[i % 2][:], r=[f"OST{i % 2}"], w=["OA_scr"])
    S.barrier()
    A.release(mF)
    if stop_after <= 3:
        return finish()

    dma("sp", BKB[:], C["c_kbs"][:], w=["BKB"])
    OBT = A.alloc("OBT", [128, 4, 2048], BF16)
    NST = [A.alloc(f"NST{g}", [128, 4, 512], BF16) for g in range(2)]
    SELG = A.alloc("SELG", [64, 24, 128], BF16)
    OV = A.alloc("OV", [128, 4, 128], BF16)
    KBW = A.alloc("KBW", [128, 256], F32)
    KBC = A.alloc("KBC", [128, 128], F32)
    X16 = A.alloc("X16", [128, S_LEN], BF16)
    dma("pool", SELG[:], C["c_selg"][:].rearrange("p (a b) -> p a b", b=128), w=["SELG"])
    dma("pool", OV[:], C["c_ov"][:].rearrange("p (a b) -> p a b", b=128), w=["OV"])
    dma("sp", KBW[:], C["c_kbw"][:], w=["KBW"])
    dma("sp", KBC[:], C["c_kbc"][:], w=["KBC"])
    CMv = X16[:].rearrange("p (s c q) -> p s c q", s=4, c=4)
    cm_flat = C["c_cm"].rearrange("p s c q -> p (s c q)")
    for qd in range(4):
        dma("pool", X16[:, qd * 2048:(qd + 1) * 2048], cm_flat[:, qd * 2048:(qd + 1) * 2048], w=["X16"])

    def nsa_qpair(i):
        q0i = 2 * (i % 2)
        qproj_pair(WQn, "WQn", i * 128, gk2[:, 3:4], QT[q0i], f"QT{q0i}", QT[q0i + 1], f"QT{q0i + 1}")
        for par in range(2):
            h = 2 * i + par
            dma("pool", QT[q0i + par][64:65, :], C["c_qaug"][h, 0:1, :], w=[f"QT{q0i + par}"])
            dma("pool", QT[q0i + par][96:97, :], C["c_qaug"][h, 1:2, :], w=[f"QT{q0i + par}"])

    def nsa_epilogue(h, br, s, oi):
        i, par = h // 2, h % 2
        sl = slice(s * 512, (s + 1) * 512)
        et = etmp[slot_ctr[0] % 2]
        er = f"etmp{slot_ctr[0] % 2}"
        et2 = et2b[slot_ctr[0] % 2]
        e2r = f"et2_{slot_ctr[0] % 2}"
        lo, hi = par * 64, par * 64 + 64
        if br == 0:
            act(et[0:64, :], ps[oi][64:128, :], AF.Ln, r=[f"ps{oi}", "tinyb"], w=[er], bias=tinyb[0:64, :])
            act(et[0:64, :], et[0:64, :], AF.Exp, r=[er], w=[er], scale=-1.0)
        else:
            ts("dve", et[0:64, :], ps[oi][64:128, :], 1e-30, None, ALU.max, r=[f"ps{oi}"], w=[er])
            recip(et[0:64, :], et[0:64, :], r=[er], w=[er])
        tt("dve", et2[lo:hi, :], ps[oi][0:64, :], et[0:64, :], ALU.mult, r=[f"ps{oi}", er], w=[e2r])
        mm(ps[5][:], SELG[:, h * 3 + br, :], SGHL[0:64, sl], True, True, r=["SELG", "SGHL"], w=["ps5"])
        if br == 0:
            tt("dve", OBT[lo:hi, i, sl], et2[lo:hi, :], ps[5][lo:hi, :], ALU.mult, r=[e2r, "ps5"], w=["OBT"])
        else:
            tt("dve", et2[lo:hi, :], et2[lo:hi, :], ps[5][lo:hi, :], ALU.mult, r=[e2r, "ps5"], w=[e2r])
            tt("pool", OBT[lo:hi, i, sl], OBT[lo:hi, i, sl], et2[lo:hi, :], ALU.add, r=[e2r, "OBT"], w=["OBT"])

    mC = A.mark()
    IMP = A.alloc("IMP", [128, 4, 512], F32)
    ASEL = A.alloc("ASEL", [128, 2048], BF16)
    BSEL = A.alloc("BSEL", [128, 2048], BF16)
    SC = A.alloc("SC", [128, 512], F32)
    SC2 = A.alloc("SC2", [128, 128], F32)
    tmpU = A.alloc("tmpU", [128, 512], F32)
    l4 = A.alloc("l4", [128, 4], F32)
    m8 = A.alloc("m8", [128, 16], F32)
    nsel = [A.alloc(f"nsel{i}", [128, 128], BF16) for i in range(2)]
    dma("pool", ASEL[:], C["c_asel"][:], w=["ASEL"])
    dma("pool", BSEL[:], C["c_bsel"][:], w=["BSEL"])
    for i in range(4):
        g = i // 2
        nsa_qpair(i)
        for par in range(2):
            h = 2 * i + par
            first_of_g = (h % 4 == 0)
            qt, qr = QT[2 * (i % 2) + par], f"QT{2 * (i % 2) + par}"
            for s in range(4):
                sl = slice(s * 512, (s + 1) * 512)
                tiles = [dict(k=KC[g][0:97, c * 128:(c + 1) * 128], kres=[f"KC{g}"],
                              extras=[(ident_b[:], CMv[:, s, c, :], ["ident_b", "X16"])],
                              bias=KBC[:, (h * 4 + s) * 4 + c:(h * 4 + s) * 4 + c + 1], bres="KBC",
                              v=VC[g][:, c, :], vres=f"VC{g}") for c in range(4)]
                oi = 3 + slot_ctr[0] % 2

                def imp_fn(Ps, Pres, s=s, first_of_g=first_of_g):
                    for sub in range(4):
                        for c in range(4):
                            mm(ps[6][:, sub * 128:(sub + 1) * 128], Ps[c][:, sub * 128:(sub + 1) * 128], OV[:, c, :],
                               c == 0, c == 3, r=[Pres[c], "OV"], w=["ps6"])
                    U3 = ps[6][:].rearrange("p (a j) -> p a j", j=128)
                    S.op("dve", lambda e: e.tensor_reduce(out=l4[:], in_=U3, axis=AX.X, op=ALU.add), r=["ps6"], w=["l4"])
                    ts("dve", l4[:], l4[:], 1e-30, None, ALU.max, r=["l4"], w=["l4"])
                    recip(l4[:], l4[:], r=["l4"], w=["l4"])
                    rlb = l4[:].unsqueeze(2).to_broadcast([128, 4, 128])
                    I3 = IMP[:, s, :].rearrange("p (a j) -> p a j", j=128)
                    if first_of_g:
                        tt("dve", I3, U3, rlb, ALU.mult, r=["ps6", "l4"], w=["IMP"])
                    else:
                        tt("dve", tmpU[:].rearrange("p (a j) -> p a j", j=128), U3, rlb, ALU.mult, r=["ps6", "l4"], w=["tmpU"])
                        tt("pool", IMP[:, s, :], IMP[:, s, :], tmpU[:], ALU.add, r=["tmpU", "IMP"], w=["IMP"])

                attn(qt[0:97, sl], qr, tiles, oi, after_exps=imp_fn)
                nsa_epilogue(h, 0, s, oi)
                slot_ctr[0] += 1
        if i % 2 == 1:
            for s in range(4):
                tt("dve", SC[:], IMP[:, s, :], ASEL[:, s * 512:(s + 1) * 512], ALU.mult, r=["IMP", "ASEL"], w=["SC"])
                tt("dve", SC[:], SC[:], BSEL[:, s * 512:(s + 1) * 512], ALU.add, r=["SC", "BSEL"], w=["SC"])
                for sub in range(4):
                    scs = SC[:, sub * 128:(sub + 1) * 128]
                    ns = nsel[sub % 2]
                    S.op("dve", lambda e, scs=scs: e.max(out=m8[:, 0:8], in_=scs), r=["SC"], w=["m8"])
                    S.op("dve", lambda e, scs=scs: e.match_replace(out=SC2[:], in_to_replace=m8[:, 0:8], in_values=scs,
                                                                  imm_value=-1e9), r=["SC", "m8"], w=["SC2"])
                    S.op("dve", lambda e: e.max(out=m8[:, 8:16], in_=SC2[:]), r=["SC2"], w=["m8"])
                    ts("dve", ns[:], scs, m8[:, 15:16], NEGM, ALU.is_lt, ALU.mult, r=["SC", "m8"], w=[f"nsel{sub % 2}"])
                    tr(ps7b[:, sub * 128:(sub + 1) * 128], ns[:], ident_b[:], r=[f"nsel{sub % 2}", "ident_b"], w=["ps7"])
                cp("act", NST[g][:, s, :], ps7b[:, 0:512], r=["ps7"], w=[f"NST{g}"])
    if "dbgA" in debug and stop_after == 4:
        for g in range(2):
            for s in range(4):
                cp("dve", SC[:], NST[g][:, s, :], r=[f"NST{g}"], w=["SC"])
                dma("sp", dbgA[:, (g * 4 + s) * 512:(g * 4 + s + 1) * 512], SC[:], r=["SC"], w=["dbgA"])
    S.barrier()
    A.release(mC)
    if stop_after <= 4:
        if "dbgB" in debug:
            for i in range(4):
                cp("dve", etmp[0][:], OBT[:, i, 0:512], r=["OBT"], w=["etmp0"])
                dma("sp", dbgB[:, i * 512:(i + 1) * 512], etmp[0][:], r=["etmp0"], w=["dbgB"])
        return finish()

    for qd in range(4):
        dma("pool", X16[:, qd * 2048:(qd + 1) * 2048], C["c_eall"][:, qd * 2048:(qd + 1) * 2048], w=["X16"])
    WM = A.alloc("WM", [128, 8, 512], BF16)
    dma("pool", WM[:], C["c_wm"][:], w=["WM"])
    KS = A.alloc("KS", [128, S_LEN], BF16)
    VS = A.alloc("VS", [128, NT, 128], BF16)
    KW = A.alloc("KW", [128, 4096], BF16)
    VW = A.alloc("VW", [128, 32, 128], BF16)
    for (kbuf, n, nm) in [(KS, S_LEN, "KSa"), (KW, 4096, "KWa")]:
        memset("pool", kbuf[64:128, :], 0.0, w=[nm])
        dma("pool", kbuf[64:65, :], C["c_onerow"][:, 0:n], r=[nm], w=[nm])
        dma("pool", kbuf[96:97, :], C["c_onerow"][:, 0:n], r=[nm], w=[nm])
    for g in range(2):
        for qd in range(4):
            dma("sp", KS[0:64, qd * 2048:(qd + 1) * 2048], KT_s[g, :, qd * 2048:(qd + 1) * 2048], r=["KT_scr"], w=[f"KS_{qd}"])
            dma("sp", VS[:, qd * 16:(qd + 1) * 16, :], V_s[g, :, qd * 16:(qd + 1) * 16, :], r=["V_scr"], w=[f"VS_{qd}"])
        dma("sp", KW[0:64, :], KT_w[g], r=["KT_scr"], w=["KW"])
        dma("sp", VW[:], V_w[g], r=["V_scr"], w=["VW"])
        for ip in range(2):
            i = 2 * g + ip
            nsa_qpair(i)
            for par in range(2):
                h = 2 * i + par
                qt, qr = QT[2 * (i % 2) + par], f"QT{2 * (i % 2) + par}"
                for s in range(4):
                    sl = slice(s * 512, (s + 1) * 512)
                    tiles = []
                    for t in range(16 * s + 16):
                        ex = [(X16[:, t * 128:(t + 1) * 128], NST[g][:, s, :], ["X16", f"NST{g}"])]
                        if t >= 16 * s:
                            m_, tl = (t - 16 * s) // 4, (t - 16 * s) % 4
                            ex.append((AM[:, m_, :], DG[:, tl, :], ["AM", "DG"]))
                        bi = (h * 4 + s) * 64 + t
                        tiles.append(dict(k=KS[0:97, t * 128:(t + 1) * 128], kres=[f"KS_{t // 16}", "KSa"], extras=ex,
                                          bias=BKB[:, bi:bi + 1], bres="BKB", v=VS[:, t, :], vres=f"VS_{t // 16}"))
                    oi = 3 + slot_ctr[0] % 2
                    attn(qt[0:97, sl], qr, tiles, oi)
                    nsa_epilogue(h, 1, s, oi)
                    slot_ctr[0] += 1
                for s in range(4):
                    sl = slice(s * 512, (s + 1) * 512)
                    tiles = []
                    for t in range(8):
                        bi = (h * 4 + s) * 8 + t
                        tiles.append(dict(k=KW[0:97, (8 * s + t) * 128:(8 * s + t + 1) * 128], kres=["KW", "KWa"],
                                          extras=[(ident_b[:], WM[:, t, :], ["ident_b", "WM"])],
                                          bias=KBW[:, bi:bi + 1], bres="KBW", v=VW[:, 8 * s + t, :], vres="VW"))
                    oi = 3 + slot_ctr[0] % 2
                    attn(qt[0:97, sl], qr, tiles, oi)
                    nsa_epilogue(h, 2, s, oi)
                    slot_ctr[0] += 1
    if "dbgB" in debug and stop_after == 5:
        for i in range(4):
            for s in range(4):
                cp("dve", etmp[0][:], OBT[:, i, s * 512:(s + 1) * 512], r=["OBT"], w=["etmp0"])
                dma("sp", dbgB_big[:, (i * 4 + s) * 512:(i * 4 + s + 1) * 512], etmp[0][:], r=["etmp0"], w=["dbgB_big"])
    for i in range(4):
        dma("sp", OB_scr[i], OBT[:, i, :], r=["OBT"], w=["OB_scr"])
    S.barrier()
    A.release(m_base)
    if stop_after <= 5:
        return finish()

    ACC = A.alloc("ACC", [128, 16, D], F32)
    gffn = A.alloc("gffn", [128, 8], F32)
    dma("sp", gffn[:], gffn_d[:], w=["gffn"])
    m5 = A.mark()
    WA = A.alloc("WA", [128, 4, D], BF16)
    WB = A.alloc("WB", [128, 4, D], BF16)
    WO = A.alloc("WO", [128, 8, D], BF16)
    WG = A.alloc("WG", [128, 8, 2048], BF16)
    def wg_load(qd):
        dma("pool", WG[:, :, qd * 512:(qd + 1) * 512], w_in_v[:, :, 2848 + qd * 512:2848 + (qd + 1) * 512], w=[f"WG{qd}"])
    wg_load(0)
    dma("pool", WA[:], wfu_d.rearrange("(c p) n -> p c n", p=128), w=["WA"])
    wg_load(2)
    dma("pool", WB[:], wnu_d.rearrange("(c p) n -> p c n", p=128), w=["WB"])
    wg_load(1)
    wg_load(3)
    dma("pool", WO[:], wout_d.rearrange("(c p) n -> p c n", p=128), w=["WO"])
    xt3 = A.alloc("xt3", [128, 4, D], F32)
    xraw = A.alloc("xraw", [128, 4, D], F32)
    junk = A.alloc("junk3", [128, D], BF16)
    ss3 = A.alloc("ss3", [128, 4], F32)
    rs3 = A.alloc("rs3", [128, 4], F32)
    HQs = A.alloc("HQs", [128, 8, 512], BF16)
    OAs = A.alloc("OAs", [128, 4, 512], BF16)
    OBs = A.alloc("OBs", [128, 4, 512], BF16)
    MIX = A.alloc("MIX", [128, 8, 512], BF16)
    sgt = [A.alloc(f"sgt{i}", [128, 512], F32) for i in range(2)]
    mxa = A.alloc("mxa", [128, 512], F32)
    dma("sp", xt3[:], xq[0:512, :].rearrange("(u p) d -> p u d", p=128), w=["xt3"])
    norm_scale(xt3, "xt3", ss3, rs3, "ss3")
    for s in range(4):
        sl = slice(s * 512, (s + 1) * 512)
        dma("sp", xraw[:], xq[sl, :].rearrange("(u p) d -> p u d", p=128), w=["xraw"])
        dma("sp", OAs[:], OA_scr.rearrange("c p t -> p c t")[:, :, sl], r=["OA_scr"], w=["OAs"])
        dma("sp", OBs[:], OB_scr.rearrange("c p t -> p c t")[:, :, sl], r=["OB_scr"], w=["OBs"])
        transpose_gain(xt3, "xt3", HQs, "HQs", gmix, "gmix")
        if s < 3:
            dma("sp", xt3[:], xq[(s + 1) * 512:(s + 2) * 512, :].rearrange("(u p) d -> p u d", p=128), w=["xt3"])
            norm_scale(xt3, "xt3", ss3, rs3, "ss3")
        for m in range(8):
            for ab in range(2):
                c0 = ab * 1024 + m * 128
                wgr = f"WG{c0 // 512}"
                pg, pgr = ps[2 + 2 * ab], f"ps{2 + 2 * ab}"
                po, por = ps[3 + 2 * ab], f"ps{3 + 2 * ab}"
                for kc in range(8):
                    mm(pg[:], WG[:, kc, c0:c0 + 128], HQs[:, kc, :], kc == 0, kc == 7, r=[wgr, "HQs"], w=[pgr])
                Wx, wxr, Ox, oxr = (WA, "WA", OAs, "OAs") if ab == 0 else (WB, "WB", OBs, "OBs")
                for c in range(4):
                    mm(po[:], Wx[:, c, m * 128:(m + 1) * 128], Ox[:, c, :], c == 0, c == 3, r=[wxr, oxr], w=[por])
                act(sgt[ab][:], pg[:], AF.Sigmoid, r=[pgr], w=[f"sgt{ab}"])
                if ab == 0:
                    tt("dve", mxa[:], po[:], sgt[0][:], ALU.mult, r=[por, "sgt0"], w=["mxa"])
                else:
                    tt("dve", sgt[1][:], po[:], sgt[1][:], ALU.mult, r=[por, "sgt1"], w=["sgt1"])
                    tt("pool", MIX[:, m, :], mxa[:], sgt[1][:], ALU.add, r=["mxa", "sgt1"], w=["MIX"])
        for u in range(4):
            for hf in range(2):
                bi = 6 + (u * 2 + hf) % 2
                for kc in range(8):
                    mm(ps[bi][:], MIX[:, kc, u * 128:(u + 1) * 128], WO[:, kc, hf * 512:(hf + 1) * 512], kc == 0, kc == 7,
                       r=["MIX", "WO"], w=[f"ps{bi}"])
                tt("dve", ACC[:, s * 4 + u, hf * 512:(hf + 1) * 512], ps[bi][:], xraw[:, u, hf * 512:(hf + 1) * 512], ALU.add,
                   r=[f"ps{bi}", "xraw"], w=[f"ACC{s}"])
    S.barrier()
    A.release(m5)

    TT = A.alloc("TT", [128, 8, 2048], BF16)
    COMB = A.alloc("COMB", [128, 16, 16], F32)
    WGb = [A.alloc(f"WGb{i}", [128, 8, 512], BF16) for i in range(2)]
    WUb = [A.alloc(f"WUb{i}", [128, 8, 512], BF16) for i in range(2)]
    WDb = [A.alloc(f"WDb{i}", [128, 4, D], BF16) for i in range(2)]

    def moe_wload(e_):
        bi = e_ % 2
        wgv = wg_d[e_].rearrange("(c p) n -> p c n", p=128)
        wuv = wu_d[e_].rearrange("(c p) n -> p c n", p=128)
        wdv = wd_d[e_].rearrange("(c p) n -> p c n", p=128)
        for hlf in range(2):
            dma("pool", WGb[bi][:, hlf * 4:(hlf + 1) * 4, :], wgv[:, hlf * 4:(hlf + 1) * 4, :], w=[f"WGb{bi}"])
            dma("pool", WUb[bi][:, hlf * 4:(hlf + 1) * 4, :], wuv[:, hlf * 4:(hlf + 1) * 4, :], w=[f"WUb{bi}"])
            dma("pool", WDb[bi][:, hlf * 2:(hlf + 1) * 2, :], wdv[:, hlf * 2:(hlf + 1) * 2, :], w=[f"WDb{bi}"])

    moe_wload(0)
    moe_wload(1)
    m6 = A.mark()
    WRf = A.alloc("WRf", [128, 8, 20], F32)
    WRh = A.alloc("WRh", [128, 8, 20], BF16)
    WRl = A.alloc("WRl", [128, 8, 20], BF16)
    brt = A.alloc("brt", [128, 20], F32)
    dma("sp", WRf[:], wr_d.rearrange("(c p) n -> p c n", p=128), w=["WRf"])
    dma("sp", brt[:], br_d[:], w=["brt"])
    cp("dve", WRh[:], WRf[:], r=["WRf"], w=["WRh"])
    tt("dve", WRl[:], WRf[:], WRh[:], ALU.subtract, r=["WRf", "WRh"], w=["WRl"])
    xn2 = [A.alloc(f"xn2_{i}", [128, 4, D], F32) for i in range(2)]
    junk = A.alloc("junk4", [128, D], BF16)
    ss4 = [A.alloc(f"ss4_{i}", [128, 4], F32) for i in range(2)]
    rs4 = [A.alloc(f"rs4_{i}", [128, 4], F32) for i in range(2)]
    tlo = [A.alloc(f"tlo{i}", [128, 8, 512], BF16) for i in range(2)]
    LALL = A.alloc("LALL", [128, 16, 20], F32)
    for s in range(4):
        sl = slice(s * 512, (s + 1) * 512)
        i2 = s % 2
        xn, ssx, rsx, tl = xn2[i2], ss4[i2], rs4[i2], tlo[i2]
        xr_, sr_, tr_ = f"xn2_{i2}", f"ss4_{i2}", f"tlo{i2}"
        for u in range(4):
            act(junk[:], ACC[:, s * 4 + u, :], AF.Square, r=[f"ACC{s}"], w=["junk", sr_], accum_out=ssx[:, u:u + 1])
        ts("dve", rsx[:], ssx[:], 1.0 / D, 1e-6, ALU.mult, ALU.add, r=[sr_], w=[sr_ + "r"])
        act(rsx[:], rsx[:], AF.Sqrt, r=[sr_ + "r"], w=[sr_ + "r"])
        recip(rsx[:], rsx[:], r=[sr_ + "r"], w=[sr_ + "r"])
        for u in range(4):
            ts("dve", xn[:, u, :], ACC[:, s * 4 + u, :], rsx[:, u:u + 1], None, ALU.mult,
               r=[f"ACC{s}", sr_ + "r"], w=[xr_])
        for kc in range(8):
            pb, pr = ps[kc % 2], f"ps{kc % 2}"
            for u in range(4):
                tr(pb[:, u * 128:(u + 1) * 128], xn[:, u, kc * 128:(kc + 1) * 128], ident_f[:], r=[xr_, "ident_f"], w=[pr])
            S.op("act", lambda e, o=TT[:, kc, sl], i=pb[:], sc=gffn[:, kc:kc + 1]:
                 e.activation(out=o, in_=i, func=AF.Copy, scale=sc), r=[pr, "gffn"], w=[f"TT{s}"])
            stt(tl[:, kc, :], pb[:], gffn[:, kc:kc + 1], TT[:, kc, sl], ALU.mult, ALU.subtract, r=[pr, "gffn", f"TT{s}"], w=[tr_])
        for u in range(4):
            tok = slice(s * 512 + u * 128, s * 512 + (u + 1) * 128)
            pl = ps[2 + u % 2]
            plr = f"ps{2 + u % 2}"
            k = 0
            for (lh, wr_, wrr) in [("TT", WRh, "WRh"), ("tlo", WRh, "WRh"), ("TT", WRl, "WRl")]:
                for kc in range(8):
                    lhsT = TT[:, kc, tok] if lh == "TT" else tl[:, kc, u * 128:(u + 1) * 128]
                    mm(pl[:, 0:20], lhsT, wr_[:, kc, :], k == 0, k == 23, r=[f"TT{s}" if lh == "TT" else tr_, wrr], w=[plr])
                    k += 1
            tt("dve", LALL[:, s * 4 + u, :], pl[:, 0:20], brt[:], ALU.add, r=[plr, "brt"], w=["LALL"])
    rr = ["RTB"]
    gl = LALL[:, :, 0:4]
    el4 = LALL[:, :, 4:20].rearrange("p a (g i) -> p a g i", i=4)
    def R(name, n):
        return A.alloc(name, [128, 16, n], F32)
    gmax, ohg, gsh, sumg, pgc = R("r_gmax", 1), R("r_ohg", 4), R("r_gsh", 4), R("r_sumg", 1), R("r_pg", 1)
    tmp16, ein, m1_, msk, e2, m2_ = R("r_tmp16", 16), R("r_ein", 4), R("r_m1", 1), R("r_msk", 4), R("r_e2", 4), R("r_m2", 1)
    esh, den, w4 = R("r_esh", 4), R("r_den", 1), R("r_w4", 4)
    def bc(ap1, n):
        return ap1.to_broadcast([128, 16, n])
    def red(out, in_, op):
        S.op("dve", lambda e: e.tensor_reduce(out=out, in_=in_, axis=AX.X, op=op), r=rr + ["LALL"], w=rr)
    red(gmax[:, :, 0], gl, ALU.max)
    tt("dve", ohg[:], gl, bc(gmax[:], 4), ALU.is_ge, r=rr + ["LALL"], w=rr)
    tt("dve", gsh[:], gl, bc(gmax[:], 4), ALU.subtract, r=rr + ["LALL"], w=rr)
    act(gsh[:], gsh[:], AF.Exp, r=rr, w=rr)
    red(sumg[:, :, 0], gsh[:], ALU.add)
    recip(pgc[:], sumg[:], r=rr, w=rr)
    t4 = tmp16[:].rearrange("p a (g i) -> p a g i", i=4)
    tt("dve", t4, el4, ohg[:].unsqueeze(3).to_broadcast([128, 16, 4, 4]), ALU.mult, r=rr + ["LALL"], w=rr)
    red(ein[:], tmp16[:].rearrange("p a (g i) -> p a i g", i=4), ALU.add)
    red(m1_[:, :, 0], ein[:], ALU.max)
    tt("dve", msk[:], ein[:], bc(m1_[:], 4), ALU.is_ge, r=rr, w=rr)
    stt(e2[:], msk[:], -1e30, ein[:], ALU.mult, ALU.add, r=rr, w=rr)
    red(m2_[:, :, 0], e2[:], ALU.max)
    tt("dve", msk[:], ein[:], bc(m2_[:], 4), ALU.is_ge, r=rr, w=rr)
    tt("dve", esh[:], ein[:], bc(m1_[:], 4), ALU.subtract, r=rr, w=rr)
    act(esh[:], esh[:], AF.Exp, r=rr, w=rr)
    tt("dve", esh[:], esh[:], msk[:], ALU.mult, r=rr, w=rr)
    red(den[:, :, 0], esh[:], ALU.add)
    recip(den[:], den[:], r=rr, w=rr)
    tt("dve", den[:], den[:], pgc[:], ALU.mult, r=rr, w=rr)
    tt("dve", w4[:], esh[:], bc(den[:], 4), ALU.mult, r=rr, w=rr)
    for g in range(4):
        tt("dve", COMB[:, :, 4 * g:4 * g + 4], w4[:], bc(ohg[:, :, g:g + 1], 4), ALU.mult, r=rr, w=["COMB"])
    if "dbgB_big" in debug and stop_after == 6:
        for u in range(8):
            dma("sp", dbgB_big[:, u * 1024:(u + 1) * 1024], ACC[:, u, :], r=[f"ACC{u // 4}"], w=["dbgB_big"])
        dma("sp", dbgA[:, 0:256], COMB[:].rearrange("p a b -> p (a b)"), r=["COMB"], w=["dbgA"])
    S.barrier()
    A.release(m6)
    if stop_after <= 6:
        return finish()

    hid = [A.alloc(f"hid{i}", [128, 512], BF16) for i in range(8)]
    sgm = [A.alloc(f"sgm{i}", [128, 512], F32) for i in range(2)]
    hctr = [0]
    for e_ in range(16):
        bi = e_ % 2
        if e_ >= 2:
            moe_wload(e_)
        for tg in range(4):
            tsl = slice(tg * 512, (tg + 1) * 512)
            hs = []
            for fc in range(4):
                pgi, pui = (fc % 2) * 2, (fc % 2) * 2 + 1
                for kc in range(8):
                    mm(ps[pgi][:], WGb[bi][:, kc, fc * 128:(fc + 1) * 128], TT[:, kc, tsl], kc == 0, kc == 7,
                       r=[f"WGb{bi}", f"TT{tg}"], w=[f"ps{pgi}"])
                for kc in range(8):
                    mm(ps[pui][:], WUb[bi][:, kc, fc * 128:(fc + 1) * 128], TT[:, kc, tsl], kc == 0, kc == 7,
                       r=[f"WUb{bi}", f"TT{tg}"], w=[f"ps{pui}"])
                sg_ = sgm[fc % 2]
                hh_ = hid[hctr[0] % 8]
                hr = f"hid{hctr[0] % 8}"
                hctr[0] += 1
                act(sg_[:], ps[pgi][:], AF.Silu, r=[f"ps{pgi}"], w=[f"sgm{fc % 2}"])
                tt("dve", hh_[:], ps[pui][:], sg_[:], ALU.mult, r=[f"ps{pui}", f"sgm{fc % 2}"], w=[hr])
                hs.append((hh_, hr))
            for u in range(4):
                for hf in range(2):
                    yi = 4 + (u * 2 + hf) % 4
                    for fc in range(4):
                        mm(ps[yi][:], hs[fc][0][:, u * 128:(u + 1) * 128], WDb[bi][:, fc, hf * 512:(hf + 1) * 512],
                           fc == 0, fc == 3, r=[hs[fc][1], f"WDb{bi}"], w=[f"ps{yi}"])
                    a_ = ACC[:, tg * 4 + u, hf * 512:(hf + 1) * 512]
                    stt(a_, ps[yi][:], COMB[:, tg * 4 + u, e_:e_ + 1], a_, ALU.mult, ALU.add,
                        r=[f"ps{yi}", "COMB", f"ACC{tg}"], w=[f"ACC{tg}"])
    for u in range(16):
        dma("sp", out_d[u * 128:(u + 1) * 128, :], ACC[:, u, :], r=[f"ACC{u // 4}"], w=["out"])
    return finish()


def make_in_maps(inputs):
    f = np.float32
    g = lambda k: np.asarray(inputs[k], f)
    x = g("x")
    maps = []
    shared = {}
    shared["w_in"] = np.ascontiguousarray(g("w_in")[0])
    shared["gmix"] = np.ascontiguousarray(g("norm_mix_g")[0].reshape(8, 128).T)
    shared["gffn"] = np.ascontiguousarray(g("norm_ffn_g")[0].reshape(8, 128).T)
    shared["gk2"] = np.ascontiguousarray(np.stack([np.tile(g("fox_k_g")[0], 2), np.tile(g("nsa_k_g")[0], 2),
                                                    np.tile(g("fox_q_g")[0], 2), np.tile(g("nsa_q_g")[0], 2)], 1))
    shared["bfb"] = np.ascontiguousarray(np.tile(g("b_forget")[0][None, :], (128, 1)))
    nb = np.zeros((128, 1), f)
    nb[:8, 0] = -g("b_forget")[0]
    shared["nbfc"] = nb
    shared["cmp_k_w1"] = np.ascontiguousarray(g("cmp_k_w1")[0])
    shared["cmp_v_w1"] = np.ascontiguousarray(g("cmp_v_w1")[0])
    shared["cmp_k_w2"] = np.ascontiguousarray(g("cmp_k_w2")[0])
    shared["cmp_v_w2"] = np.ascontiguousarray(g("cmp_v_w2")[0])
    shared["poskT"] = np.ascontiguousarray(np.tile(g("cmp_k_pos")[0].T, (2, 1)))
    shared["posvT"] = np.ascontiguousarray(np.tile(g("cmp_v_pos")[0].T, (2, 1)))
    shared["w_fox_up"] = np.ascontiguousarray(g("w_fox_up")[0])
    shared["w_nsa_up"] = np.ascontiguousarray(g("w_nsa_up")[0])
    shared["w_out"] = np.ascontiguousarray(g("w_out")[0])
    shared["w_rt"] = np.ascontiguousarray(np.concatenate([g("w_group")[0], g("w_router")[0]], 1))
    shared["b_rt"] = np.ascontiguousarray(np.tile(np.concatenate([g("b_group")[0], g("b_router")[0]])[None, :], (128, 1)))
    shared["w_gate"] = np.ascontiguousarray(g("w_gate")[0])
    shared["w_up"] = np.ascontiguousarray(g("w_up")[0])
    shared["w_down"] = np.ascontiguousarray(g("w_down")[0])
    consts = [host_consts(j) for j in range(4)]
    for c in range(8):
        b, j = c // 4, c % 4
        m = dict(shared)
        m["xb"] = np.ascontiguousarray(x[b])
        m["xq"] = np.ascontiguousarray(np.concatenate([x[b, 512 * (4 * s + j):512 * (4 * s + j + 1)] for s in range(4)], 0))
        xw = np.zeros((4096, D), f)
        for s in range(4):
            q0 = 512 * (4 * s + j)
            lo = q0 - 512
            if lo >= 0:
                xw[1024 * s:1024 * (s + 1)] = x[b, lo:lo + 1024]
            else:
                xw[1024 * s + 512:1024 * (s + 1)] = x[b, 0:512]
        m["xw"] = xw
        for k, v in consts[j].items():
            m[k] = np.ascontiguousarray(v.reshape(CONST_SHAPES[k]))
        maps.append(m)
    return maps


def kernel(**inputs):
    nc = build()
    maps = make_in_maps(inputs)
    res = run_bass_kernel_spmd(nc, maps, core_ids=list(range(8)))
    out = np.zeros((2, S_LEN, D), np.float32)
    for c in range(8):
        b, j = c // 4, c % 4
        o = res.results[c]["out"]
        for s in range(4):
            out[b, 512 * (4 * s + j):512 * (4 * s + j + 1)] = o[512 * s:512 * (s + 1)]
    return out
```
